# Optimizing a Trainium2 kernel written in Bass

```python
import math
import jax, jax.numpy as jnp
from jax import lax
import numpy as np

D_MODEL = 1024
BATCH = 32
SEQ = 2048
DEPTH = 2

CONV_CH = D_MODEL // 2
CONV_WIDTH = 31
SB_HEADS = 8
SB_HEAD_DIM = 64
SB_WIDTH = SB_HEADS * SB_HEAD_DIM
Q_BLOCK = 128
EV_IN = 2 * CONV_CH + 3 * SB_WIDTH
EV_OUT_IN = CONV_CH + SB_WIDTH
LRU_WIDTH = D_MODEL
LRU_BLOCKS = 8
LRU_BLOCK_SIZE = LRU_WIDTH // LRU_BLOCKS
LRU_CONV_WIDTH = 4
LRU_C = 8.0
N_GROUPS = 4
EXPERTS_PER_GROUP = 4
N_EXPERTS = N_GROUPS * EXPERTS_PER_GROUP
TOP_K = 2
GROUP_SCORE_K = 2
D_FF_EXPERT = 512
MOE_BLOCK = 128

N_EVEN = (DEPTH + 1) // 2
N_ODD = DEPTH // 2
EPS = 1e-6

kernel_name = 'hybrid_conformer_stickbreak_rglru_moe'


def rms_norm(x, g):
    xf = x.astype(jnp.float32)
    y = xf * lax.rsqrt(jnp.mean(xf * xf, axis=-1, keepdims=True) + EPS)
    return (y * g.astype(jnp.float32)).astype(x.dtype)


def layer_norm(x, g, b):
    xf = x.astype(jnp.float32)
    mu = jnp.mean(xf, axis=-1, keepdims=True)
    xc = xf - mu
    var = jnp.mean(xc * xc, axis=-1, keepdims=True)
    y = xc * lax.rsqrt(var + EPS) * g.astype(jnp.float32) + b.astype(jnp.float32)
    return y.astype(x.dtype)


def causal_dwconv(x, w, b):
    width = w.shape[0]
    y = lax.conv_general_dilated(
        x, w[:, None, :].astype(x.dtype), window_strides=(1,),
        padding=[(width - 1, 0)], dimension_numbers=('NWC', 'WIO', 'NWC'),
        feature_group_count=x.shape[-1])
    return y + b.astype(x.dtype)


def stick_breaking_attention(q, k, v):
    seq = q.shape[2]
    scale = 1.0 / math.sqrt(q.shape[-1])
    outs = []
    for blk in range(seq // Q_BLOCK):
        q0 = blk * Q_BLOCK
        end = q0 + Q_BLOCK
        qb = q[:, :, q0:end]
        kb = k[:, :, :end]
        vb = v[:, :, :end]
        z = jnp.einsum('bhqd,bhkd->bhqk', qb, kb).astype(jnp.float32) * scale
        qpos = q0 + jnp.arange(Q_BLOCK)[:, None]
        kpos = jnp.arange(end)[None, :]
        causal = kpos < qpos
        log_1m = jnp.where(causal, jax.nn.log_sigmoid(-z), 0.0)
        between = lax.cumsum(log_1m, axis=3, reverse=True) - log_1m
        wts = jnp.where(causal, jnp.exp(jax.nn.log_sigmoid(z) + between), 0.0)
        outs.append(jnp.einsum('bhqk,bhkd->bhqd', wts.astype(v.dtype), vb))
    return jnp.concatenate(outs, axis=2)


def even_mixer(h, in_w, dw_w, dw_b, ln_g, ln_b, q_g, k_g, out_w):
    bsz, seq, _ = h.shape
    proj = h @ in_w
    a_val, a_gate, q, k, v = jnp.split(
        proj, [CONV_CH, 2 * CONV_CH, 2 * CONV_CH + SB_WIDTH, 2 * CONV_CH + 2 * SB_WIDTH], axis=-1)
    u = a_val * jax.nn.sigmoid(a_gate)
    u = causal_dwconv(u, dw_w, dw_b)
    u = jax.nn.silu(layer_norm(u, ln_g, ln_b))
    def heads(t):
        return t.reshape(bsz, seq, SB_HEADS, SB_HEAD_DIM)
    qh = rms_norm(heads(q), q_g).transpose(0, 2, 1, 3)
    kh = rms_norm(heads(k), k_g).transpose(0, 2, 1, 3)
    vh = heads(v).transpose(0, 2, 1, 3)
    o = stick_breaking_attention(qh, kh, vh).transpose(0, 2, 1, 3).reshape(bsz, seq, SB_WIDTH)
    return jnp.concatenate([u, o], axis=-1) @ out_w


def block_diag_linear(x, w, b):
    nb, bs, _ = w.shape
    xb = x.reshape(x.shape[:-1] + (nb, bs))
    return jnp.einsum('bsnk,nkj->bsnj', xb, w).reshape(x.shape) + b


def lru_combine(earlier, later):
    a1, b1 = earlier
    a2, b2 = later
    return a1 * a2, a2 * b1 + b2


def odd_mixer(h, in_w, conv_w, conv_b, rg_w, rg_b, ig_w, ig_b, lam, out_w):
    proj = h @ in_w
    y_branch, x_branch = jnp.split(proj, 2, axis=-1)
    xc = causal_dwconv(x_branch, conv_w, conv_b)
    r = jax.nn.sigmoid(block_diag_linear(xc, rg_w, rg_b))
    i = jax.nn.sigmoid(block_diag_linear(xc, ig_w, ig_b))
    log_a = LRU_C * r.astype(jnp.float32) * jax.nn.log_sigmoid(lam.astype(jnp.float32))
    a = jnp.exp(log_a)
    mult = jnp.sqrt(-jnp.expm1(2.0 * log_a))
    b_in = mult * (i * xc).astype(jnp.float32)
    _, hseq = lax.associative_scan(lru_combine, (a, b_in), axis=1)
    return (jax.nn.gelu(y_branch) * hseq.astype(h.dtype)) @ out_w


def moe(h2, router_w, router_b, w1, w3, w2):
    n_tok, d = h2.shape
    logits = h2.astype(jnp.float32) @ router_w.astype(jnp.float32)
    scores = jax.nn.sigmoid(logits)
    biased = (scores + router_b.astype(jnp.float32)).reshape(n_tok, N_GROUPS, EXPERTS_PER_GROUP)
    group_score = jnp.sum(lax.top_k(biased, GROUP_SCORE_K)[0], axis=-1)
    gidx = jnp.argmax(group_score, axis=-1)
    in_group = jnp.take_along_axis(biased, gidx[:, None, None], axis=1)[:, 0]
    _, loc = lax.top_k(in_group, TOP_K)
    eid = gidx[:, None] * EXPERTS_PER_GROUP + loc
    sel = jnp.take_along_axis(scores, eid, axis=1)
    gates = (sel / jnp.sum(sel, axis=-1, keepdims=True)).astype(h2.dtype)
    n_asg = n_tok * TOP_K
    flat_e = eid.reshape(n_asg)
    flat_tok = jnp.repeat(jnp.arange(n_tok, dtype=jnp.int32), TOP_K)
    flat_g = gates.reshape(n_asg)
    order = jnp.argsort(flat_e)
    se = flat_e[order]
    stok = flat_tok[order]
    sg = flat_g[order]
    counts = jnp.bincount(flat_e, length=N_EXPERTS)
    starts = jnp.cumsum(counts) - counts
    padded = (counts + MOE_BLOCK - 1) // MOE_BLOCK * MOE_BLOCK
    pad_end = jnp.cumsum(padded)
    pad_start = pad_end - padded
    dest = pad_start[se] + (jnp.arange(n_asg) - starts[se])
    n_blocks = -(-n_asg // MOE_BLOCK) + N_EXPERTS
    buf_len = n_blocks * MOE_BLOCK
    buf_tok = jnp.zeros((buf_len,), jnp.int32).at[dest].set(stok)
    buf_g = jnp.zeros((buf_len,), h2.dtype).at[dest].set(sg)
    blk_e = jnp.minimum(
        jnp.searchsorted(pad_end, jnp.arange(n_blocks) * MOE_BLOCK, side='right'), N_EXPERTS - 1)

    def run_block(args):
        tok, g, e = args
        xb = h2[tok]
        y = (jax.nn.silu(xb @ w1[e]) * (xb @ w3[e])) @ w2[e]
        return y * g[:, None]

    ys = lax.map(run_block, (buf_tok.reshape(n_blocks, MOE_BLOCK),
                             buf_g.reshape(n_blocks, MOE_BLOCK), blk_e))
    return jnp.zeros_like(h2).at[buf_tok].add(ys.reshape(buf_len, d))


def setup_inputs(seed: int = 0) -> dict:
    key = jax.random.key(seed)
    ks = jax.random.split(key, 28)
    f32 = jnp.float32

    def nrm(k, shape, scale):
        return jax.random.normal(k, shape, f32) * scale

    def gain(k, shape):
        return 1.0 + 0.05 * jax.random.normal(k, shape, f32)

    u = jax.random.uniform(ks[24], (N_ODD, LRU_WIDTH), f32, 0.9, 0.999)
    s = u ** (1.0 / LRU_C)
    lam = jnp.log(s) - jnp.log1p(-s)
    return {
        'x': nrm(ks[0], (BATCH, SEQ, D_MODEL), 1.0),
        'c': nrm(ks[1], (BATCH, D_MODEL), 1.0),
        'mod_w': nrm(ks[2], (DEPTH, D_MODEL, 6 * D_MODEL), 0.5 * D_MODEL ** -0.5),
        'mod_b': nrm(ks[3], (DEPTH, 6 * D_MODEL), 0.02),
        'mix_norm_g': gain(ks[4], (DEPTH, D_MODEL)),
        'ffn_norm_g': gain(ks[5], (DEPTH, D_MODEL)),
        'ev_in_w': nrm(ks[6], (N_EVEN, D_MODEL, EV_IN), D_MODEL ** -0.5),
        'ev_dw_w': nrm(ks[7], (N_EVEN, CONV_WIDTH, CONV_CH), CONV_WIDTH ** -0.5),
        'ev_dw_b': nrm(ks[8], (N_EVEN, CONV_CH), 0.02),
        'ev_ln_g': gain(ks[9], (N_EVEN, CONV_CH)),
        'ev_ln_b': nrm(ks[10], (N_EVEN, CONV_CH), 0.02),
        'ev_q_g': gain(ks[11], (N_EVEN, SB_HEAD_DIM)),
        'ev_k_g': gain(ks[12], (N_EVEN, SB_HEAD_DIM)),
        'ev_out_w': nrm(ks[13], (N_EVEN, EV_OUT_IN, D_MODEL), EV_OUT_IN ** -0.5),
        'od_in_w': nrm(ks[14], (N_ODD, D_MODEL, 2 * LRU_WIDTH), D_MODEL ** -0.5),
        'od_conv_w': nrm(ks[15], (N_ODD, LRU_CONV_WIDTH, LRU_WIDTH), LRU_CONV_WIDTH ** -0.5),
        'od_conv_b': nrm(ks[16], (N_ODD, LRU_WIDTH), 0.02),
        'od_rg_w': nrm(ks[17], (N_ODD, LRU_BLOCKS, LRU_BLOCK_SIZE, LRU_BLOCK_SIZE), LRU_BLOCK_SIZE ** -0.5),
        'od_rg_b': nrm(ks[18], (N_ODD, LRU_WIDTH), 0.02),
        'od_ig_w': nrm(ks[19], (N_ODD, LRU_BLOCKS, LRU_BLOCK_SIZE, LRU_BLOCK_SIZE), LRU_BLOCK_SIZE ** -0.5),
        'od_ig_b': nrm(ks[20], (N_ODD, LRU_WIDTH), 0.02),
        'od_lam': lam,
        'od_out_w': nrm(ks[21], (N_ODD, LRU_WIDTH, D_MODEL), LRU_WIDTH ** -0.5),
        'router_w': nrm(ks[22], (D_MODEL, N_EXPERTS), D_MODEL ** -0.5),
        'router_b': nrm(ks[23], (N_EXPERTS,), 0.01),
        'ex_w1': nrm(ks[25], (DEPTH, N_EXPERTS, D_MODEL, D_FF_EXPERT), D_MODEL ** -0.5),
        'ex_w3': nrm(ks[26], (DEPTH, N_EXPERTS, D_MODEL, D_FF_EXPERT), D_MODEL ** -0.5),
        'ex_w2': nrm(ks[27], (DEPTH, N_EXPERTS, D_FF_EXPERT, D_MODEL), D_FF_EXPERT ** -0.5),
    }


def reference(x, c, mod_w, mod_b, mix_norm_g, ffn_norm_g, ev_in_w, ev_dw_w, ev_dw_b,
              ev_ln_g, ev_ln_b, ev_q_g, ev_k_g, ev_out_w, od_in_w, od_conv_w, od_conv_b,
              od_rg_w, od_rg_b, od_ig_w, od_ig_b, od_lam, od_out_w, router_w, router_b,
              ex_w1, ex_w3, ex_w2):
    bsz, seq, d = x.shape
    cond = jax.nn.silu(c)
    for layer in range(DEPTH):
        mod = cond @ mod_w[layer] + mod_b[layer]
        sh1, sc1, g1, sh2, sc2, g2 = [m[:, None, :] for m in jnp.split(mod, 6, axis=-1)]
        h = rms_norm(x, mix_norm_g[layer]) * (1 + sc1) + sh1
        if layer % 2 == 0:
            e = layer // 2
            mix = even_mixer(h, ev_in_w[e], ev_dw_w[e], ev_dw_b[e], ev_ln_g[e], ev_ln_b[e],
                             ev_q_g[e], ev_k_g[e], ev_out_w[e])
        else:
            o = layer // 2
            mix = odd_mixer(h, od_in_w[o], od_conv_w[o], od_conv_b[o], od_rg_w[o], od_rg_b[o],
                            od_ig_w[o], od_ig_b[o], od_lam[o], od_out_w[o])
        x = x + g1 * mix
        h = rms_norm(x, ffn_norm_g[layer]) * (1 + sc2) + sh2
        ffn = moe(h.reshape(bsz * seq, d), router_w, router_b,
                  ex_w1[layer], ex_w3[layer], ex_w2[layer]).reshape(bsz, seq, d)
        x = x + g2 * ffn
    return x
```

```python
import numpy as np
from contextlib import ExitStack
import concourse.bass as bass
import concourse.mybir as mybir
from concourse.bass_utils import run_bass_kernel_spmd

F32 = mybir.dt.float32
BF16 = mybir.dt.bfloat16
AF = mybir.ActivationFunctionType
ALU = mybir.AluOpType
AX = mybir.AxisListType

D = 1024
S = 2048
NCH = 8
TT = 512
NT = 4
NE = 16
EPS = 1e-6
N_CORES = 8
SEQ_PER_CORE = 4


def _fm(v):
    v = np.asarray(v, np.float32)
    return np.ascontiguousarray(v.reshape(-1, 128).T)


PV_FIELDS = [("mixg", 16), ("ffng", 16), ("modb", 96), ("dwb", 4), ("lng", 4), ("lnb", 4), ("dww", 124),
             ("ocw", 32), ("ocb", 8), ("rgb", 8), ("igb", 8), ("lam", 8), ("qg", 1), ("kg", 1), ("rb", 16), ("cT", 32)]
PV_OFF = {}
_o = 0
for _n, _w in PV_FIELDS:
    PV_OFF[_n] = (_o, _w)
    _o += _w
NV = _o

CST_FIELDS = [("ident", 128), ("ustrict", 128), ("bdones", 128), ("ones", 128), ("mask", 2048), ("sel", 2048)]
CST_OFF = {}
_o = 0
for _n, _w in CST_FIELDS:
    CST_OFF[_n] = (_o, _w)
    _o += _w
NCST = _o


def make_consts():
    c = np.zeros((128, NCST), np.float32)
    j = np.arange(128)[:, None]
    s = np.arange(128)[None, :]
    c[:, CST_OFF["ident"][0]:][:, :128] = (j == s)
    c[:, CST_OFF["ustrict"][0]:][:, :128] = (j > s)
    c[:, CST_OFF["bdones"][0]:][:, :128] = ((j // 64) == (s // 64))
    c[:, CST_OFF["ones"][0]:][:, :128] = 1.0
    t = np.arange(512)[None, :]
    for d in range(4):
        o = CST_OFF["mask"][0] + d * 512
        c[:, o:o + 512] = ((128 * d + j) < t)
    o = CST_OFF["sel"][0]
    for e in range(16):
        c[e, o + e * 128:o + (e + 1) * 128] = 1.0
    return c


def make_pvec(inp, b0, nseq):
    pv = np.zeros((128, NV), np.float32)

    def put(name, arr):
        o, w = PV_OFF[name]
        assert arr.shape == (128, w), (name, arr.shape, w)
        pv[:, o:o + w] = arr

    put("mixg", np.concatenate([_fm(inp["mix_norm_g"][l]) for l in range(2)], axis=1))
    put("ffng", np.concatenate([_fm(inp["ffn_norm_g"][l]) for l in range(2)], axis=1))
    put("modb", np.concatenate([_fm(inp["mod_b"][l]) for l in range(2)], axis=1))
    put("dwb", _fm(inp["ev_dw_b"][0]))
    put("lng", _fm(inp["ev_ln_g"][0]))
    put("lnb", _fm(inp["ev_ln_b"][0]))
    dw = np.asarray(inp["ev_dw_w"][0], np.float32)
    put("dww", np.ascontiguousarray(dw.reshape(31, 4, 128).transpose(2, 1, 0).reshape(128, 124)))
    cw = np.asarray(inp["od_conv_w"][0], np.float32)
    put("ocw", np.ascontiguousarray(cw.reshape(4, 8, 128).transpose(2, 1, 0).reshape(128, 32)))
    put("ocb", _fm(inp["od_conv_b"][0]))
    put("rgb", _fm(inp["od_rg_b"][0]))
    put("igb", _fm(inp["od_ig_b"][0]))
    put("lam", _fm(inp["od_lam"][0]))
    put("qg", np.tile(np.asarray(inp["ev_q_g"][0], np.float32), 2)[:, None])
    put("kg", np.tile(np.asarray(inp["ev_k_g"][0], np.float32), 2)[:, None])
    put("rb", np.tile(np.asarray(inp["router_b"], np.float32)[None, :], (128, 1)))
    cc = np.zeros((4, 1024), np.float32)
    cc[:nseq] = np.asarray(inp["c"][b0:b0 + nseq], np.float32)
    put("cT", np.ascontiguousarray(cc.reshape(4, 8, 128).transpose(2, 1, 0).reshape(128, 32)))
    return pv


class Sched:
    def __init__(self, nc, es):
        self.nc = nc
        self.es = es
        self.engines = {"pe": nc.tensor, "act": nc.scalar, "dve": nc.vector, "pool": nc.gpsimd, "sp": nc.sync}
        self.sems = {}
        self.count = {}
        self.seen = {e: {} for e in self.engines}
        self.lastw = {}
        self.readers = {}
        self.n_inst = 0
        self.n_wait = 0
        for e in ("pe", "act", "dve", "pool"):
            self._sem(e)

    def _sem(self, name):
        if name not in self.sems:
            self.sems[name] = self.es.enter_context(self.nc.semaphore(name))
            self.count[name] = 0
        return self.sems[name]

    def _wait(self, eng, sem, val):
        if val <= 0 or self.seen[eng].get(sem, 0) >= val:
            return
        self.engines[eng].wait_ge(self.sems[sem], val)
        self.seen[eng][sem] = val
        self.n_wait += 1

    def _deps(self, eng, reads, writes):
        need = {}
        for k in reads:
            ev = self.lastw.get(k)
            if ev is not None and ev[1] > need.get(ev[0], 0):
                need[ev[0]] = ev[1]
        for k in writes:
            ev = self.lastw.get(k)
            if ev is not None and ev[1] > need.get(ev[0], 0):
                need[ev[0]] = ev[1]
            rd = self.readers.get(k)
            if rd:
                for s_, v_ in rd.items():
                    if v_ > need.get(s_, 0):
                        need[s_] = v_
        for s_, v_ in need.items():
            if s_ == eng:
                if v_ > self.count[eng]:
                    continue
                if eng == "pe":
                    continue
            self._wait(eng, s_, v_)

    def _record(self, ev, reads, writes):
        for k in reads:
            rd = self.readers.setdefault(k, {})
            if ev[1] > rd.get(ev[0], 0):
                rd[ev[0]] = ev[1]
        for k in writes:
            self.lastw[k] = ev
            self.readers[k] = {}

    def op(self, eng, fn, reads=(), writes=(), inc=True):
        self._deps(eng, reads, writes)
        inst = fn(self.engines[eng])
        self.n_inst += 1
        if inc:
            inst.then_inc(self.sems[eng], 1)
            self.count[eng] += 1
            ev = (eng, self.count[eng])
        else:
            ev = (eng, self.count[eng] + 1)
        self._record(ev, reads, writes)
        return inst

    def dma(self, q, sem, pairs, reads=(), writes=()):
        self._sem(sem)
        self._deps(q, reads, writes)
        self._wait(q, sem, self.count[sem])
        for out, in_ in pairs:
            self.engines[q].dma_start(out=out, in_=in_).then_inc(self.sems[sem], 16)
            self.count[sem] += 16
            self.n_inst += 1
        ev = (sem, self.count[sem])
        self._record(ev, reads, writes)

    def wait_all(self, eng):
        for s_, v_ in self.count.items():
            self._wait(eng, s_, v_)


def build_program(nseq=SEQ_PER_CORE, phases=("m0", "e0", "m1", "e1")):
    nc = bass.Bass("TRN2", target_bir_lowering=False)
    dr = {}

    def din(name, shape):
        dr[name] = nc.dram_tensor(name, list(shape), F32, kind="ExternalInput").ap()
        return dr[name]

    xT_d = din("xT", [nseq, D, S])
    pvec_d = din("pvec", [128, NV])
    cst_d = din("cst", [128, NCST])
    modw_d = din("mod_w", [2, D, 6 * D])
    evin_d = din("ev_in_w", [D, 2560])
    evout_d = din("ev_out_w", [D, D])
    odin_d = din("od_in_w", [D, 2048])
    odout_d = din("od_out_w", [D, D])
    rgw_d = din("od_rg_w", [8, 128, 128])
    igw_d = din("od_ig_w", [8, 128, 128])
    rw_d = din("router_w", [D, NE])
    w1_d = din("ex_w1", [2, NE, D, 512])
    w3_d = din("ex_w3", [2, NE, D, 512])
    w2_d = din("ex_w2", [2, NE, 512, D])
    outT_d = nc.dram_tensor("outT", [nseq, D, S], F32, kind="ExternalOutput").ap()

    _uid = [0]

    def SBT(name, shape, dt):
        _uid[0] += 1
        return nc.sbuf_tensor(f"{name}_{_uid[0]}", shape, dt)

    with ExitStack() as es:
        sc = Sched(nc, es)

        def sb(name, shape, dt):
            return es.enter_context(SBT(name, list(shape), dt))

        xT = sb("xT_s", [128, NCH, S], F32)
        hT = sb("hT_s", [128, NCH, S], BF16)
        pv = sb("pv_s", [128, NV], F32)
        ident = sb("ident", [128, 128], F32)
        ones_bf = sb("ones_bf", [128, 128], BF16)
        ustr_bf = sb("ustr_bf", [128, 128], BF16)
        bdon_bf = sb("bdon_bf", [128, 128], BF16)
        sel = sb("sel", [16, NE, 128], F32)
        rw = sb("rw", [128, NCH, NE], F32)
        modT = [sb(f"modT{l}", [128, 48, 4], F32) for l in range(2)]
        A1 = [sb(f"A1_{l}", [128, NCH, 4], F32) for l in range(2)]
        A2 = [sb(f"A2_{l}", [128, NCH, 4], F32) for l in range(2)]
        cl8 = sb("cl8", [128, NCH], F32)
        ps = [es.enter_context(nc.psum_tensor(f"ps{i}", [128, 512], F32)) for i in range(8)]

        def pvs(name, i=0, n=None):
            o, w = PV_OFF[name]
            n = 1 if n is None else n
            return pv[:, o + i:o + i + n]

        def K(*a):
            return a

        o_, _ = CST_OFF["ident"]
        sc.dma("sp", "d_c0", [(pv[:], pvec_d[:, :]), (ident[:], cst_d[:, o_:o_ + 128])], writes=[K("pv"), K("ident")])
        o_, _ = CST_OFF["sel"]
        sc.dma("sp", "d_c1", [(sel[:], cst_d[0:16, o_:o_ + 2048].rearrange("p (e m) -> p e m", e=NE)),
                              (rw[:], rw_d.rearrange("(c p) e -> p c e", p=128))], writes=[K("sel"), K("rw")])
        oo, ou, ob, om = CST_OFF["ones"][0], CST_OFF["ustrict"][0], CST_OFF["bdones"][0], CST_OFF["mask"][0]
        sc.dma("pool", "d_c2", [(ones_bf[:], cst_d[:, oo:oo + 128]), (ustr_bf[:], cst_d[:, ou:ou + 128]),
                                (bdon_bf[:], cst_d[:, ob:ob + 128])],
               writes=[K("ones"), K("ustr"), K("bdon")])

        with ExitStack() as es2:
            cond = es2.enter_context(SBT("cond", [128, NCH, 4], F32))
            mw = [es2.enter_context(SBT(f"mw{i}", [128, NCH, 512], F32)) for i in range(2)]
            o_, _ = PV_OFF["cT"]
            sc.op("act", lambda e: e.activation(out=cond[:].rearrange("p c b -> p (c b)"), in_=pv[:, o_:o_ + 32], func=AF.Silu),
                  reads=[K("pv")], writes=[K("cond")])
            gi = 0
            for l in range(2):
                for g in range(12):
                    slot = gi % 2
                    gi += 1
                    sc.dma("sp", f"d_mw{slot}", [(mw[slot][:], modw_d[l, :, g * 512:(g + 1) * 512].rearrange("(c p) f -> p c f", p=128))],
                           writes=[K("mw", slot)])
                    for fc in range(4):
                        f = g * 4 + fc
                        for kc in range(NCH):
                            sc.op("pe", lambda e: e.matmul(ps[0][:, f * 4:(f + 1) * 4], lhsT=mw[slot][:, kc, fc * 128:(fc + 1) * 128],
                                                           rhs=cond[:, kc, :], start=(kc == 0), stop=(kc == NCH - 1)),
                                  reads=[K("mw", slot), K("cond")], writes=[K("ps", 0)], inc=(kc == NCH - 1))
                o_, _ = PV_OFF["modb"]
                sc.op("dve", lambda e: e.tensor_tensor(out=modT[l][:], in0=ps[0][:, 0:192].rearrange("p (f b) -> p f b", b=4),
                                                       in1=pv[:, o_ + l * 48:o_ + (l + 1) * 48].unsqueeze(2).broadcast_to([128, 48, 4]),
                                                       op=ALU.add),
                      reads=[K("ps", 0), K("pv")], writes=[K("modT", l)])
                og, _ = PV_OFF["mixg"]
                sc.op("dve", lambda e: e.scalar_tensor_tensor(out=A1[l][:], in0=modT[l][:, 8:16, :], scalar=1.0, op0=ALU.add,
                                                              in1=pv[:, og + l * 8:og + (l + 1) * 8].unsqueeze(2).broadcast_to([128, 8, 4]),
                                                              op1=ALU.mult),
                      reads=[K("modT", l), K("pv")], writes=[K("A1", l)])
                og, _ = PV_OFF["ffng"]
                sc.op("dve", lambda e: e.scalar_tensor_tensor(out=A2[l][:], in0=modT[l][:, 32:40, :], scalar=1.0, op0=ALU.add,
                                                              in1=pv[:, og + l * 8:og + (l + 1) * 8].unsqueeze(2).broadcast_to([128, 8, 4]),
                                                              op1=ALU.mult),
                      reads=[K("modT", l), K("pv")], writes=[K("A2", l)])
            ol, _ = PV_OFF["lam"]
            sc.op("act", lambda e: e.activation(out=cl8[:], in_=pv[:, ol:ol + 8], func=AF.Exp, scale=-1.0), reads=[K("pv")], writes=[K("cl8")])
            sc.op("act", lambda e: e.activation(out=cl8[:], in_=cl8[:], func=AF.Ln, bias=1.0), reads=[K("cl8")], writes=[K("cl8")])
            sc.op("dve", lambda e: e.tensor_scalar(out=cl8[:], in0=cl8[:], scalar1=-8.0, scalar2=None, op0=ALU.mult),
                  reads=[K("cl8")], writes=[K("cl8")])
            for e_ in ("pe", "act", "dve", "pool", "sp"):
                sc.wait_all(e_)

        xkeys = [K("x", c, t) for c in range(NCH) for t in range(NT)]
        hkeys = [K("h", c, t) for c in range(NCH) for t in range(NT)]

        def phase_norm(b, Aap, Sap, router, lgT=None):
            with ExitStack() as esn:
                rstd = esn.enter_context(SBT("rstd", [128, NT, TT], F32))
                sqb = [esn.enter_context(SBT(f"sqb{i}", [128, TT], BF16)) for i in range(2)]
                tmp = [esn.enter_context(SBT(f"ntmp{i}", [128, TT], F32)) for i in range(2)]
                h32 = esn.enter_context(SBT("h32", [128, NCH, TT], F32)) if router else None
                i2 = 0
                for t in range(NT):
                    tl = slice(t * TT, (t + 1) * TT)
                    for c in range(NCH):
                        s_ = i2 % 2
                        i2 += 1
                        if c % 2 == 0:
                            sc.op("act", lambda e: e.activation(out=sqb[s_][:], in_=xT[:, c, tl], func=AF.Square),
                                  reads=[K("x", c, t)], writes=[K("sqb", s_)])
                        else:
                            sc.op("pool", lambda e: e.tensor_tensor(out=sqb[s_][:], in0=xT[:, c, tl], in1=xT[:, c, tl], op=ALU.mult),
                                  reads=[K("x", c, t)], writes=[K("sqb", s_)])
                        sc.op("pe", lambda e: e.matmul(ps[t][:], lhsT=ones_bf[:], rhs=sqb[s_][:], start=(c == 0), stop=(c == NCH - 1)),
                              reads=[K("ones"), K("sqb", s_)], writes=[K("ps", t)], inc=True)
                for t in range(NT):
                    sc.op("act", lambda e: e.activation(out=rstd[:, t, :], in_=ps[t][:], func=AF.Ln, scale=1.0 / D, bias=EPS),
                          reads=[K("ps", t)], writes=[K("rstd", t)])
                for t in range(NT):
                    sc.op("act", lambda e: e.activation(out=rstd[:, t, :], in_=rstd[:, t, :], func=AF.Exp, scale=-0.5),
                          reads=[K("rstd", t)], writes=[K("rstd", t)])
                i2 = 0
                for t in range(NT):
                    tl = slice(t * TT, (t + 1) * TT)
                    for c in range(NCH):
                        s_ = i2 % 2
                        i2 += 1
                        sc.op("pool", lambda e: e.tensor_tensor(out=tmp[s_][:], in0=xT[:, c, tl], in1=rstd[:, t, :], op=ALU.mult),
                              reads=[K("x", c, t), K("rstd", t)], writes=[K("ntmp", s_)])
                        if router:
                            sc.op("act", lambda e: e.activation(out=h32[:, c, :], in_=tmp[s_][:], func=AF.Identity,
                                                                scale=Aap[:, c, b:b + 1], bias=Sap[:, c, b:b + 1]),
                                  reads=[K("ntmp", s_), K("modall")], writes=[K("h32", c)])
                            sc.op("dve", lambda e: e.tensor_copy(out=hT[:, c, tl], in_=h32[:, c, :]),
                                  reads=[K("h32", c)], writes=[K("h", c, t)])
                        else:
                            sc.op("act", lambda e: e.activation(out=hT[:, c, tl], in_=tmp[s_][:], func=AF.Identity,
                                                                scale=Aap[:, c, b:b + 1], bias=Sap[:, c, b:b + 1]),
                                  reads=[K("ntmp", s_), K("modall")], writes=[K("h", c, t)])
                    if router:
                        for c in range(NCH):
                            sc.op("pe", lambda e: e.matmul(ps[4][0:16, :], lhsT=rw[:, c, :], rhs=h32[:, c, :], start=(c == 0), stop=(c == NCH - 1)),
                                  reads=[K("rw"), K("h32", c)], writes=[K("ps", 4)], inc=(c == NCH - 1))
                        sc.op("act", lambda e: e.activation(out=lgT[:, tl], in_=ps[4][0:16, :], func=AF.Identity),
                              reads=[K("ps", 4)], writes=[K("lgT", t)])
                for e_ in ("pe", "act", "dve", "pool", "sp"):
                    sc.wait_all(e_)

        def phase_route(lgT, gT):
            with ExitStack() as esr:
                def t_(name, shape):
                    return esr.enter_context(SBT(name, list(shape), F32))
                scr = t_("r_sc", [128, 256])
                bi = t_("r_bi", [128, 256])
                p6 = t_("r_p6", [128, 64, 6])
                gs = t_("r_gs", [128, 64])
                gmax = t_("r_gmax", [128, 16])
                goh = t_("r_goh", [128, 64])
                m1 = t_("r_m1", [128, 64])
                e1 = t_("r_e1", [128, 256])
                b2 = t_("r_b2", [128, 256])
                m2 = t_("r_m2", [128, 64])
                sl = t_("r_sl", [128, 256])
                den = t_("r_den", [128, 16])
                gates = t_("r_gates", [128, 256])
                for tc in range(16):
                    sc.op("pe", lambda e: e.transpose(out=ps[5][:, tc * 16:(tc + 1) * 16], in_=lgT[:, tc * 128:(tc + 1) * 128], identity=ident[0:16, 0:16]),
                          reads=[K("lgT", tc // 4), K("ident")], writes=[K("ps", 5)], inc=(tc == 15))
                sc.op("act", lambda e: e.activation(out=scr[:], in_=ps[5][:, 0:256], func=AF.Sigmoid), reads=[K("ps", 5)], writes=[K("r_sc")])
                o_, _ = PV_OFF["rb"]
                V = sc.op
                V("dve", lambda e: e.tensor_tensor(out=bi[:].rearrange("p (t e) -> p t e", e=16), in0=scr[:].rearrange("p (t e) -> p t e", e=16),
                                                   in1=pv[:, o_:o_ + 16].unsqueeze(1).broadcast_to([128, 16, 16]), op=ALU.add),
                  reads=[K("r_sc"), K("pv")], writes=[K("r_bi")])
                bi4 = bi[:].rearrange("p (g k) -> p g k", k=4)
                V("dve", lambda e: e.tensor_tensor(out=p6[:, :, 0:3], in0=bi4[:, :, 0:3], in1=bi4[:, :, 1:4], op=ALU.add), reads=[K("r_bi")], writes=[K("r_p6a")])
                V("dve", lambda e: e.tensor_tensor(out=p6[:, :, 3:5], in0=bi4[:, :, 0:2], in1=bi4[:, :, 2:4], op=ALU.add), reads=[K("r_bi")], writes=[K("r_p6b")])
                V("dve", lambda e: e.tensor_tensor(out=p6[:, :, 5:6], in0=bi4[:, :, 0:1], in1=bi4[:, :, 3:4], op=ALU.add), reads=[K("r_bi")], writes=[K("r_p6c")])
                V("dve", lambda e: e.tensor_reduce(out=gs[:], in_=p6[:], axis=AX.X, op=ALU.max), reads=[K("r_p6a"), K("r_p6b"), K("r_p6c")], writes=[K("r_gs")])
                V("dve", lambda e: e.tensor_reduce(out=gmax[:], in_=gs[:].rearrange("p (t g) -> p t g", g=4), axis=AX.X, op=ALU.max), reads=[K("r_gs")], writes=[K("r_gmax")])
                V("dve", lambda e: e.tensor_tensor(out=goh[:].rearrange("p (t g) -> p t g", g=4), in0=gs[:].rearrange("p (t g) -> p t g", g=4),
                                                   in1=gmax[:].unsqueeze(2).broadcast_to([128, 16, 4]), op=ALU.is_equal), reads=[K("r_gs"), K("r_gmax")], writes=[K("r_goh")])
                V("dve", lambda e: e.tensor_reduce(out=m1[:], in_=bi4, axis=AX.X, op=ALU.max), reads=[K("r_bi")], writes=[K("r_m1")])
                V("dve", lambda e: e.tensor_tensor(out=e1[:].rearrange("p (g k) -> p g k", k=4), in0=bi4, in1=m1[:].unsqueeze(2).broadcast_to([128, 64, 4]), op=ALU.is_equal),
                  reads=[K("r_bi"), K("r_m1")], writes=[K("r_e1")])
                V("dve", lambda e: e.scalar_tensor_tensor(out=b2[:], in0=e1[:], scalar=-1.0e9, op0=ALU.mult, in1=bi[:], op1=ALU.add), reads=[K("r_e1"), K("r_bi")], writes=[K("r_b2")])
                V("dve", lambda e: e.tensor_reduce(out=m2[:], in_=b2[:].rearrange("p (g k) -> p g k", k=4), axis=AX.X, op=ALU.max), reads=[K("r_b2")], writes=[K("r_m2")])
                V("dve", lambda e: e.tensor_tensor(out=sl[:].rearrange("p (g k) -> p g k", k=4), in0=bi4, in1=m2[:].unsqueeze(2).broadcast_to([128, 64, 4]), op=ALU.is_ge),
                  reads=[K("r_bi"), K("r_m2")], writes=[K("r_sl")])
                V("dve", lambda e: e.tensor_tensor(out=sl[:].rearrange("p (g k) -> p g k", k=4), in0=sl[:].rearrange("p (g k) -> p g k", k=4),
                                                   in1=goh[:].unsqueeze(2).broadcast_to([128, 64, 4]), op=ALU.mult), reads=[K("r_sl"), K("r_goh")], writes=[K("r_sl")])
                V("dve", lambda e: e.tensor_tensor(out=sl[:], in0=sl[:], in1=scr[:], op=ALU.mult), reads=[K("r_sl"), K("r_sc")], writes=[K("r_sl")])
                V("dve", lambda e: e.tensor_reduce(out=den[:], in_=sl[:].rearrange("p (t e) -> p t e", e=16), axis=AX.X, op=ALU.add), reads=[K("r_sl")], writes=[K("r_den")])
                V("dve", lambda e: e.reciprocal(out=den[:], in_=den[:]), reads=[K("r_den")], writes=[K("r_den")])
                V("dve", lambda e: e.tensor_tensor(out=gates[:].rearrange("p (t e) -> p t e", e=16), in0=sl[:].rearrange("p (t e) -> p t e", e=16),
                                                   in1=den[:].unsqueeze(2).broadcast_to([128, 16, 16]), op=ALU.mult), reads=[K("r_sl"), K("r_den")], writes=[K("r_gates")])
                for tc in range(16):
                    bk = tc // 4
                    sc.op("pe", lambda e: e.transpose(out=ps[bk][0:16, (tc % 4) * 128:(tc % 4 + 1) * 128], in_=gates[:, tc * 16:(tc + 1) * 16], identity=ident[:]),
                          reads=[K("r_gates"), K("ident")], writes=[K("ps", bk)], inc=(tc % 4 == 3))
                for bk in range(4):
                    sc.op("act", lambda e: e.activation(out=gT[:, bk * TT:(bk + 1) * TT], in_=ps[bk][0:16, :], func=AF.Identity),
                          reads=[K("ps", bk)], writes=[K("gT", bk)])
                for e_ in ("pe", "act", "dve", "pool", "sp"):
                    sc.wait_all(e_)

        def phase_moe(l, b):
            with ExitStack() as esm:
                lgT = esm.enter_context(SBT("lgT", [16, S], F32))
                gT = esm.enter_context(SBT("gT", [16, S], F32))
                phase_norm(b, A2[l], modT[l][:, 24:32, :], True, lgT)
                phase_route(lgT, gT)
                w1s = [esm.enter_context(SBT(f"w1s{i}", [128, NCH, 512], BF16)) for i in range(2)]
                w3s = [esm.enter_context(SBT(f"w3s{i}", [128, NCH, 512], BF16)) for i in range(2)]
                w2s = [esm.enter_context(SBT(f"w2s{i}", [128, 4, D], BF16)) for i in range(2)]
                gbc = [esm.enter_context(SBT(f"gbc{i}", [128, TT], F32)) for i in range(2)]
                s1 = [esm.enter_context(SBT(f"s1_{i}", [128, TT], F32)) for i in range(2)]
                sg = [esm.enter_context(SBT(f"sg_{i}", [128, TT], F32)) for i in range(2)]
                actT = [esm.enter_context(SBT(f"actT{i}", [128, 4, TT], BF16)) for i in range(2)]

                def load_w(e):
                    s_ = e % 2
                    sc.dma("pool", f"d_w{s_}",
                           [(w1s[s_][:], w1_d[l, e].rearrange("(c p) f -> p c f", p=128)),
                            (w3s[s_][:], w3_d[l, e].rearrange("(c p) f -> p c f", p=128)),
                            (w2s[s_][:], w2_d[l, e].rearrange("(c p) f -> p c f", p=128))],
                           writes=[K("wexp", s_)])

                load_w(0)
                it = 0
                fi = 0
                for e in range(NE):
                    ws = e % 2
                    if e + 1 < NE:
                        load_w(e + 1)
                    for t in range(NT):
                        tl = slice(t * TT, (t + 1) * TT)
                        a_ = it % 2
                        it += 1
                        sc.op("pe", lambda en: en.matmul(ps[6][:], lhsT=sel[:, e, :], rhs=gT[:, tl], start=True, stop=True),
                              reads=[K("sel"), K("gT", t)], writes=[K("ps", 6)])
                        sc.op("act", lambda en: en.activation(out=gbc[a_][:], in_=ps[6][:], func=AF.Identity), reads=[K("ps", 6)], writes=[K("gbc", a_)])
                        for fc in range(4):
                            f_ = fi % 2
                            fi += 1
                            for kc in range(NCH):
                                sc.op("pe", lambda en: en.matmul(ps[0 + f_][:], lhsT=w1s[ws][:, kc, fc * 128:(fc + 1) * 128], rhs=hT[:, kc, tl],
                                                                 start=(kc == 0), stop=(kc == NCH - 1)),
                                      reads=[K("wexp", ws), K("h", kc, t)], writes=[K("ps", 0 + f_)], inc=(kc == NCH - 1))
                            for kc in range(NCH):
                                sc.op("pe", lambda en: en.matmul(ps[2 + f_][:], lhsT=w3s[ws][:, kc, fc * 128:(fc + 1) * 128], rhs=hT[:, kc, tl],
                                                                 start=(kc == 0), stop=(kc == NCH - 1)),
                                      reads=[K("wexp", ws), K("h", kc, t)], writes=[K("ps", 2 + f_)], inc=(kc == NCH - 1))
                            sc.op("act", lambda en: en.activation(out=s1[f_][:], in_=ps[0 + f_][:], func=AF.Silu), reads=[K("ps", 0 + f_)], writes=[K("s1", f_)])
                            sc.op("pool", lambda en: en.tensor_tensor(out=sg[f_][:], in0=s1[f_][:], in1=gbc[a_][:], op=ALU.mult),
                                  reads=[K("s1", f_), K("gbc", a_)], writes=[K("sg", f_)])
                            sc.op("dve", lambda en: en.tensor_tensor(out=actT[a_][:, fc, :], in0=ps[2 + f_][:], in1=sg[f_][:], op=ALU.mult),
                                  reads=[K("ps", 2 + f_), K("sg", f_)], writes=[K("actT", a_, fc)])
                        for dc in range(NCH):
                            o_ = 4 + dc % 2
                            for fc in range(4):
                                sc.op("pe", lambda en: en.matmul(ps[o_][:], lhsT=w2s[ws][:, fc, dc * 128:(dc + 1) * 128], rhs=actT[a_][:, fc, :],
                                                                 start=(fc == 0), stop=(fc == 3)),
                                      reads=[K("wexp", ws), K("actT", a_, fc)], writes=[K("ps", o_)], inc=(fc == 3))
                            sc.op("dve", lambda en: en.scalar_tensor_tensor(out=xT[:, dc, tl], in0=ps[o_][:], scalar=modT[l][:, 40 + dc, b:b + 1], op0=ALU.mult,
                                                                            in1=xT[:, dc, tl], op1=ALU.add),
                                  reads=[K("ps", o_), K("x", dc, t), K("modall")], writes=[K("x", dc, t)])
                for e_ in ("pe", "act", "dve", "pool", "sp"):
                    sc.wait_all(e_)

        def barrier():
            for e_ in ("pe", "act", "dve", "pool", "sp"):
                sc.wait_all(e_)

        def out_proj(l, b, w_d, zT_):
            with ExitStack() as eso:
                ow = eso.enter_context(SBT("ow", [128, NCH, D], BF16))
                sc.dma("pool", "d_ow", [(ow[:, 0:4, :], w_d[0:512, :].rearrange("(c p) f -> p c f", p=128)),
                                        (ow[:, 4:8, :], w_d[512:1024, :].rearrange("(c p) f -> p c f", p=128))], writes=[K("ow")])
                i_ = 0
                for dc in range(NCH):
                    for t in range(NT):
                        tl = slice(t * TT, (t + 1) * TT)
                        o_ = i_ % 4
                        i_ += 1
                        for kc in range(NCH):
                            sc.op("pe", lambda en: en.matmul(ps[o_][:], lhsT=ow[:, kc, dc * 128:(dc + 1) * 128], rhs=zT_[:, kc, tl],
                                                             start=(kc == 0), stop=(kc == NCH - 1)),
                                  reads=[K("ow"), K("h", kc, t)], writes=[K("ps", o_)], inc=(kc == NCH - 1))
                        sc.op("dve", lambda en: en.scalar_tensor_tensor(out=xT[:, dc, tl], in0=ps[o_][:], scalar=modT[l][:, 16 + dc, b:b + 1], op0=ALU.mult,
                                                                        in1=xT[:, dc, tl], op1=ALU.add),
                              reads=[K("ps", o_), K("x", dc, t)], writes=[K("x", dc, t)])
                barrier()

        def phase_m1(b):
            l = 1
            phase_norm(b, A1[l], modT[l][:, 0:8, :], False)
            HS = S // 2
            with ExitStack() as esz:
                zT = esz.enter_context(SBT("zT", [128, NCH, S], BF16))
                with ExitStack() as es1:
                    def t_(name, shape, dt=F32):
                        return es1.enter_context(SBT(name, list(shape), dt))
                    wyx = [t_(f"wyx{i}", [128, NCH, 256], BF16) for i in range(2)]
                    rgw = t_("rgw", [128, NCH, 128], BF16)
                    igw = t_("igw", [128, NCH, 128], BF16)
                    xbr = t_("xbr", [128, 3 + S])
                    Bgy = t_("Bgy", [128, HS])
                    Bxc = t_("Bxc", [128, HS])
                    Br = t_("Br", [128, HS])
                    Bi = t_("Bi", [128, HS])
                    Bm = t_("Bm", [128, HS])
                    xcb = t_("xcb", [128, HS], BF16)
                    carry = t_("carry", [128, 1])
                    sc.dma("pool", "d_gw", [(rgw[:], rgw_d.rearrange("n k j -> k n j")), (igw[:], igw_d.rearrange("n k j -> k n j"))], writes=[K("gw")])
                    sc.op("pool", lambda en: en.memset(xbr[:, 0:3], 0.0), writes=[K("xbrpad")])
                    ocw, _ = PV_OFF["ocw"]

                    def load_wyx(c):
                        s_ = c % 2
                        sc.dma("pool", f"d_wyx{s_}", [(wyx[s_][:, :, 0:128], odin_d[:, c * 128:(c + 1) * 128].rearrange("(k p) f -> p k f", p=128)),
                                                      (wyx[s_][:, :, 128:256], odin_d[:, 1024 + c * 128:1024 + (c + 1) * 128].rearrange("(k p) f -> p k f", p=128))],
                               writes=[K("wyx", s_)])
                    load_wyx(0)
                    for c in range(NCH):
                        ws = c % 2
                        if c + 1 < NCH:
                            load_wyx(c + 1)
                        for hf in range(2):
                            for tt in range(2):
                                t = hf * 2 + tt
                                tl = slice(t * TT, (t + 1) * TT)
                                hl = slice(tt * TT, (tt + 1) * TT)
                                for kc in range(NCH):
                                    sc.op("pe", lambda en: en.matmul(ps[tt][:], lhsT=wyx[ws][:, kc, 0:128], rhs=hT[:, kc, tl], start=(kc == 0), stop=(kc == NCH - 1)),
                                          reads=[K("wyx", ws), K("h", kc, t)], writes=[K("ps", tt)], inc=(kc == NCH - 1))
                                for kc in range(NCH):
                                    sc.op("pe", lambda en: en.matmul(ps[2 + tt][:], lhsT=wyx[ws][:, kc, 128:256], rhs=hT[:, kc, tl], start=(kc == 0), stop=(kc == NCH - 1)),
                                          reads=[K("wyx", ws), K("h", kc, t)], writes=[K("ps", 2 + tt)], inc=(kc == NCH - 1))
                                sc.op("act", lambda en: en.activation(out=Bgy[:, hl], in_=ps[tt][:], func=AF.Gelu_apprx_tanh), reads=[K("ps", tt)], writes=[K("Bgy", tt)])
                                sc.op("act", lambda en: en.activation(out=xbr[:, 3 + t * TT:3 + (t + 1) * TT], in_=ps[2 + tt][:], func=AF.Identity),
                                      reads=[K("ps", 2 + tt)], writes=[K("xbr", t)])
                            for tt in range(2):
                                t = hf * 2 + tt
                                hl = slice(tt * TT, (tt + 1) * TT)
                                rk = [K("xbr", t), K("xbrpad")] + ([K("xbr", t - 1)] if t > 0 else [])
                                sc.op("dve", lambda en: en.tensor_scalar(out=Bxc[:, hl], in0=xbr[:, t * TT + 3:t * TT + 3 + TT], scalar1=pv[:, ocw + c * 4 + 3:ocw + c * 4 + 4],
                                                                         scalar2=pvs("ocb", c), op0=ALU.mult, op1=ALU.add),
                                      reads=rk, writes=[K("Bxc", tt)])
                                for k in range(3):
                                    sc.op("dve", lambda en: en.scalar_tensor_tensor(out=Bxc[:, hl], in0=xbr[:, t * TT + k:t * TT + k + TT], scalar=pv[:, ocw + c * 4 + k:ocw + c * 4 + k + 1],
                                                                                    op0=ALU.mult, in1=Bxc[:, hl], op1=ALU.add),
                                          reads=rk + [K("Bxc", tt)], writes=[K("Bxc", tt)])
                                sc.op("pool", lambda en: en.tensor_copy(out=xcb[:, hl], in_=Bxc[:, hl]), reads=[K("Bxc", tt)], writes=[K("xcb", tt)])
                                sc.op("pe", lambda en: en.matmul(ps[4 + tt][:], lhsT=rgw[:, c, :], rhs=xcb[:, hl], start=True, stop=True),
                                      reads=[K("gw"), K("xcb", tt)], writes=[K("ps", 4 + tt)])
                                sc.op("pe", lambda en: en.matmul(ps[6 + tt][:], lhsT=igw[:, c, :], rhs=xcb[:, hl], start=True, stop=True),
                                      reads=[K("gw"), K("xcb", tt)], writes=[K("ps", 6 + tt)])
                            for tt in range(2):
                                hl = slice(tt * TT, (tt + 1) * TT)
                                sc.op("act", lambda en: en.activation(out=Br[:, hl], in_=ps[4 + tt][:], func=AF.Sigmoid, bias=pvs("rgb", c)), reads=[K("ps", 4 + tt)], writes=[K("Br", tt)])
                                sc.op("act", lambda en: en.activation(out=Bi[:, hl], in_=ps[6 + tt][:], func=AF.Sigmoid, bias=pvs("igb", c)), reads=[K("ps", 6 + tt)], writes=[K("Bi", tt)])
                            for tt in range(2):
                                hl = slice(tt * TT, (tt + 1) * TT)
                                sc.op("act", lambda en: en.activation(out=Br[:, hl], in_=Br[:, hl], func=AF.Exp, scale=cl8[:, c:c + 1]), reads=[K("Br", tt)], writes=[K("Br", tt)])
                                sc.op("pool", lambda en: en.tensor_tensor(out=Bm[:, hl], in0=Br[:, hl], in1=Br[:, hl], op=ALU.mult), reads=[K("Br", tt)], writes=[K("Bm", tt)])
                                sc.op("pool", lambda en: en.tensor_tensor(out=Bi[:, hl], in0=Bi[:, hl], in1=Bxc[:, hl], op=ALU.mult), reads=[K("Bi", tt), K("Bxc", tt)], writes=[K("Bi", tt)])
                            for tt in range(2):
                                hl = slice(tt * TT, (tt + 1) * TT)
                                sc.op("act", lambda en: en.activation(out=Bm[:, hl], in_=Bm[:, hl], func=AF.Ln, scale=-1.0, bias=1.0), reads=[K("Bm", tt)], writes=[K("Bm", tt)])
                            for tt in range(2):
                                hl = slice(tt * TT, (tt + 1) * TT)
                                sc.op("act", lambda en: en.activation(out=Bm[:, hl], in_=Bm[:, hl], func=AF.Exp, scale=0.5), reads=[K("Bm", tt)], writes=[K("Bm", tt)])
                            for tt in range(2):
                                t = hf * 2 + tt
                                tl = slice(t * TT, (t + 1) * TT)
                                hl = slice(tt * TT, (tt + 1) * TT)
                                sc.op("pool", lambda en: en.tensor_tensor(out=Bi[:, hl], in0=Bi[:, hl], in1=Bm[:, hl], op=ALU.mult), reads=[K("Bi", tt), K("Bm", tt)], writes=[K("Bi", tt)])
                                if t == 0:
                                    sc.op("dve", lambda en: en.tensor_tensor_scan(out=Bxc[:, hl], data0=Br[:, hl], data1=Bi[:, hl], initial=0.0, op0=ALU.mult, op1=ALU.add),
                                          reads=[K("Br", tt), K("Bi", tt), K("Bxc", tt)], writes=[K("Bxc", tt)])
                                else:
                                    sc.op("dve", lambda en: en.tensor_tensor_scan(out=Bxc[:, hl], data0=Br[:, hl], data1=Bi[:, hl], initial=carry[:, 0:1], op0=ALU.mult, op1=ALU.add),
                                          reads=[K("Br", tt), K("Bi", tt), K("Bxc", tt), K("carry")], writes=[K("Bxc", tt)])
                                sc.op("dve", lambda en: en.tensor_copy(out=carry[:, 0:1], in_=Bxc[:, tt * TT + TT - 1:tt * TT + TT]), reads=[K("Bxc", tt)], writes=[K("carry")])
                                sc.op("pool", lambda en: en.tensor_tensor(out=zT[:, c, tl], in0=Bgy[:, hl], in1=Bxc[:, hl], op=ALU.mult),
                                      reads=[K("Bgy", tt), K("Bxc", tt)], writes=[K("z", c, t)])
                    barrier()
                out_proj(l, b, odout_d, zT)

        def phase_m0(b):
            l = 0
            phase_norm(b, A1[l], modT[l][:, 0:8, :], False)
            UP = 30
            with ExitStack() as es0:
                def t0_(name, shape, dt=F32):
                    return es0.enter_context(SBT(name, list(shape), dt))
                u_pad = t0_("u_pad", [128, 4, UP + S], BF16)
                mask_bf = t0_("mask_bf", [128, 4, 512], BF16)
                ustr_f = t0_("ustr_f", [128, 128])
                ones_f = t0_("ones_f", [128, 128])
                om, ou, oo = CST_OFF["mask"][0], CST_OFF["ustrict"][0], CST_OFF["ones"][0]
                sc.dma("pool", "d_c3", [(mask_bf[:], cst_d[:, om:om + 2048].rearrange("p (d t) -> p d t", d=4))], writes=[K("mask")])
                sc.dma("sp", "d_c4", [(ustr_f[:], cst_d[:, ou:ou + 128]), (ones_f[:], cst_d[:, oo:oo + 128])], writes=[K("ustr_f"), K("ones_f")])
                sc.op("pool", lambda en: en.memset(u_pad[:, :, 0:UP], 0.0), writes=[K("upad")])
                esq = ExitStack()
                qT = esq.enter_context(SBT("qT", [128, 4, S], BF16))
                kT = esq.enter_context(SBT("kT", [128, 4, S], BF16))
                v_sb = esq.enter_context(SBT("v_sb", [128, 16, 512], BF16))
                with ExitStack() as esa:
                    def ta_(name, shape, dt=F32):
                        return esa.enter_context(SBT(name, list(shape), dt))
                    wb = [ta_(f"wb{i}", [128, NCH, 512], BF16) for i in range(2)]
                    sig = [ta_(f"sig{i}", [128, TT]) for i in range(2)]
                    qsq = [ta_(f"qsq{i}", [128, TT], BF16) for i in range(2)]
                    rq = [ta_(f"rq{i}", [128, TT]) for i in range(2)]

                    def load_g(g, slot):
                        sc.dma("pool", f"d_wb{slot}", [(wb[slot][:], evin_d[:, g * 512:(g + 1) * 512].rearrange("(k p) f -> p k f", p=128))], writes=[K("wb", slot)])
                    load_g(0, 0)
                    load_g(1, 1)
                    i_ = 0
                    for fc in range(4):
                        for t in range(NT):
                            tl = slice(t * TT, (t + 1) * TT)
                            p_ = i_ % 2
                            i_ += 1
                            for kc in range(NCH):
                                sc.op("pe", lambda en: en.matmul(ps[p_][:], lhsT=wb[0][:, kc, fc * 128:(fc + 1) * 128], rhs=hT[:, kc, tl], start=(kc == 0), stop=(kc == NCH - 1)),
                                      reads=[K("wb", 0), K("h", kc, t)], writes=[K("ps", p_)], inc=(kc == NCH - 1))
                            for kc in range(NCH):
                                sc.op("pe", lambda en: en.matmul(ps[2 + p_][:], lhsT=wb[1][:, kc, fc * 128:(fc + 1) * 128], rhs=hT[:, kc, tl], start=(kc == 0), stop=(kc == NCH - 1)),
                                      reads=[K("wb", 1), K("h", kc, t)], writes=[K("ps", 2 + p_)], inc=(kc == NCH - 1))
                            sc.op("act", lambda en: en.activation(out=sig[p_][:], in_=ps[2 + p_][:], func=AF.Sigmoid), reads=[K("ps", 2 + p_)], writes=[K("sig", p_)])
                            sc.op("dve", lambda en: en.tensor_tensor(out=u_pad[:, fc, UP + t * TT:UP + (t + 1) * TT], in0=ps[p_][:], in1=sig[p_][:], op=ALU.mult),
                                  reads=[K("ps", p_), K("sig", p_)], writes=[K("u", fc, t)])
                    for gi_, (g, dstT, gname) in enumerate([(2, qT, "qg"), (3, kT, "kg")]):
                        slot = gi_ % 2
                        load_g(g, slot)
                        for fc in range(4):
                            for t in range(NT):
                                tl = slice(t * TT, (t + 1) * TT)
                                p_ = i_ % 2
                                i_ += 1
                                for kc in range(NCH):
                                    sc.op("pe", lambda en: en.matmul(ps[4 + p_][:], lhsT=wb[slot][:, kc, fc * 128:(fc + 1) * 128], rhs=hT[:, kc, tl], start=(kc == 0), stop=(kc == NCH - 1)),
                                          reads=[K("wb", slot), K("h", kc, t)], writes=[K("ps", 4 + p_)], inc=(kc == NCH - 1))
                                sc.op("act", lambda en: en.activation(out=qsq[p_][:], in_=ps[4 + p_][:], func=AF.Square), reads=[K("ps", 4 + p_)], writes=[K("qsq", p_)])
                                sc.op("pe", lambda en: en.matmul(ps[6 + p_][:], lhsT=bdon_bf[:], rhs=qsq[p_][:], start=True, stop=True),
                                      reads=[K("bdon"), K("qsq", p_)], writes=[K("ps", 6 + p_)])
                                sc.op("act", lambda en: en.activation(out=rq[p_][:], in_=ps[6 + p_][:], func=AF.Ln, scale=1.0 / 64.0, bias=EPS), reads=[K("ps", 6 + p_)], writes=[K("rq", p_)])
                                sc.op("act", lambda en: en.activation(out=rq[p_][:], in_=rq[p_][:], func=AF.Exp, scale=-0.5), reads=[K("rq", p_)], writes=[K("rq", p_)])
                                sc.op("dve", lambda en: en.scalar_tensor_tensor(out=dstT[:, fc, tl], in0=ps[4 + p_][:], scalar=pvs(gname), op0=ALU.mult, in1=rq[p_][:], op1=ALU.mult),
                                      reads=[K("ps", 4 + p_), K("rq", p_)], writes=[K(gname, fc, t)])
                    load_g(4, 0)
                    for tc in range(16):
                        p_ = tc % 2
                        for kc in range(NCH):
                            sc.op("pe", lambda en: en.matmul(ps[p_][:], lhsT=hT[:, kc, tc * 128:(tc + 1) * 128], rhs=wb[0][:, kc, :], start=(kc == 0), stop=(kc == NCH - 1)),
                                  reads=[K("wb", 0), K("h", kc, tc // 4)], writes=[K("ps", p_)], inc=(kc == NCH - 1))
                        if tc % 2 == 0:
                            sc.op("act", lambda en: en.activation(out=v_sb[:, tc, :], in_=ps[p_][:], func=AF.Identity), reads=[K("ps", p_)], writes=[K("v", tc)])
                        else:
                            sc.op("dve", lambda en: en.tensor_copy(out=v_sb[:, tc, :], in_=ps[p_][:]), reads=[K("ps", p_)], writes=[K("v", tc)])
                    barrier()
                with ExitStack() as esb:
                    def tb_(name, shape, dt=F32):
                        return esb.enter_context(SBT(name, list(shape), dt))
                    spb = [tb_(f"spb{i}", [128, TT]) for i in range(2)]
                    spm = [tb_(f"spm{i}", [128, TT]) for i in range(2)]
                    Tb = [tb_(f"Tb{i}", [128, TT]) for i in range(2)]
                    eeb = [tb_(f"eeb{i}", [128, TT]) for i in range(2)]
                    wtb = [tb_(f"wtb{i}", [128, TT], BF16) for i in range(2)]
                    Sacc = [tb_(f"Sacc{i}", [128, TT]) for i in range(2)]
                    units = []
                    for hp in range(4):
                        for qt in range(NT):
                            nk = 4 * (qt + 1)
                            for sc_ in range(nk - 1, -1, -1):
                                for hh in range(2):
                                    units.append((hp, qt, sc_, hh, nk))

                    def front(ui):
                        hp, qt, sc_, hh, nk = units[ui]
                        u2 = ui % 2
                        hr = slice(hh * 64, (hh + 1) * 64)
                        ql = slice(qt * TT, (qt + 1) * TT)
                        kl = slice(sc_ * 128, (sc_ + 1) * 128)
                        d = sc_ - 4 * qt
                        if sc_ == nk - 1:
                            sc.op("pool", lambda en: en.memset(Sacc[hh][:], 0.0), writes=[K("Sacc", hh)])
                        sc.op("pe", lambda en: en.matmul(ps[u2][:], lhsT=kT[hr, hp, kl], rhs=qT[hr, hp, ql], start=True, stop=True),
                              reads=[K("kg", hp, sc_ // 4), K("qg", hp, qt)], writes=[K("ps", u2)])
                        sc.op("act", lambda en: en.activation(out=spb[u2][:], in_=ps[u2][:], func=AF.Exp, scale=0.125), reads=[K("ps", u2)], writes=[K("spb", u2)])
                        sc.op("act", lambda en: en.activation(out=spb[u2][:], in_=spb[u2][:], func=AF.Ln, bias=1.0), reads=[K("spb", u2)], writes=[K("spb", u2)])
                        if d >= 0:
                            sc.op("pool", lambda en: en.tensor_tensor(out=spm[u2][:], in0=spb[u2][:], in1=mask_bf[:, d, :], op=ALU.mult),
                                  reads=[K("spb", u2), K("mask")], writes=[K("spm", u2)])
                            src, skey = spm[u2], K("spm", u2)
                        else:
                            src, skey = spb[u2], K("spb", u2)
                        sc.op("pe", lambda en: en.matmul(ps[2 + u2][:], lhsT=ustr_f[:], rhs=src[:], start=True, stop=True),
                              reads=[K("ustr_f"), skey], writes=[K("ps", 2 + u2)])
                        if sc_ > 0:
                            sc.op("pe", lambda en: en.matmul(ps[4 + u2][:], lhsT=ones_f[:], rhs=src[:], start=True, stop=True),
                                  reads=[K("ones_f"), skey], writes=[K("ps", 4 + u2)])

                    def back(ui):
                        hp, qt, sc_, hh, nk = units[ui]
                        u2 = ui % 2
                        ql = slice(qt * TT, (qt + 1) * TT)
                        d = sc_ - 4 * qt
                        pvb = 6 + ((hp * NT + qt) % 2)
                        h = 2 * hp + hh
                        sc.op("dve", lambda en: en.tensor_tensor(out=Tb[u2][:], in0=ps[2 + u2][:], in1=Sacc[hh][:], op=ALU.add),
                              reads=[K("ps", 2 + u2), K("Sacc", hh)], writes=[K("Tb", u2)])
                        if sc_ > 0:
                            sc.op("dve", lambda en: en.tensor_tensor(out=Sacc[hh][:], in0=ps[4 + u2][:], in1=Sacc[hh][:], op=ALU.add),
                                  reads=[K("ps", 4 + u2), K("Sacc", hh)], writes=[K("Sacc", hh)])
                        sc.op("pool", lambda en: en.tensor_tensor(out=Tb[u2][:], in0=Tb[u2][:], in1=spb[u2][:], op=ALU.add),
                              reads=[K("Tb", u2), K("spb", u2)], writes=[K("Tb", u2)])
                        sc.op("dve", lambda en: en.scalar_tensor_tensor(out=eeb[u2][:], in0=ps[u2][:], scalar=0.125, op0=ALU.mult, in1=Tb[u2][:], op1=ALU.subtract),
                              reads=[K("ps", u2), K("Tb", u2)], writes=[K("eeb", u2)])
                        sc.op("act", lambda en: en.activation(out=wtb[u2][:], in_=eeb[u2][:], func=AF.Exp), reads=[K("eeb", u2)], writes=[K("wtb", u2)])
                        if d >= 0:
                            sc.op("pool", lambda en: en.tensor_tensor(out=wtb[u2][:], in0=wtb[u2][:], in1=mask_bf[:, d, :], op=ALU.mult),
                                  reads=[K("wtb", u2), K("mask")], writes=[K("wtb", u2)])
                        sc.op("pe", lambda en: en.matmul(ps[pvb][hh * 64:(hh + 1) * 64, :], lhsT=v_sb[:, sc_, h * 64:(h + 1) * 64], rhs=wtb[u2][:],
                                                         start=(sc_ == nk - 1), stop=(sc_ == 0)),
                              reads=[K("v", sc_), K("wtb", u2)], writes=[K("pv", pvb, hh)], inc=True)
                        if sc_ == 0 and hh == 1:
                            sc.op("act", lambda en: en.activation(out=hT[:, 4 + hp, ql], in_=ps[pvb][:], func=AF.Identity),
                                  reads=[K("pv", pvb, 0), K("pv", pvb, 1)], writes=[K("h", 4 + hp, qt)])

                    nu = len(units)
                    for ui in range(nu + 1):
                        if ui < nu:
                            front(ui)
                        if ui >= 1:
                            back(ui - 1)
                    barrier()
                esq.close()
                with ExitStack() as esc:
                    def tc_(name, shape, dt=F32):
                        return esc.enter_context(SBT(name, list(shape), dt))
                    HS = S // 2
                    y = tc_("cv_y", [128, 4, HS])
                    ysq = [tc_(f"ysq{i}", [128, TT], BF16) for i in range(2)]
                    ybf = [tc_(f"ybf{i}", [128, TT], BF16) for i in range(2)]
                    mean = tc_("cv_mean", [128, TT])
                    msq = tc_("cv_msq", [128, TT])
                    rs = tc_("cv_rs", [128, TT])
                    t1 = [tc_(f"cv_t1{i}", [128, TT]) for i in range(2)]
                    odw, _ = PV_OFF["dww"]
                    for hf in range(2):
                        for fc in range(4):
                            rk = [K("upad")] + [K("u", fc, t) for t in range(0, hf * 2 + 2)]
                            sc.op("dve", lambda en: en.tensor_scalar(out=y[:, fc, :], in0=u_pad[:, fc, hf * HS:hf * HS + HS], scalar1=pv[:, odw + fc * 31:odw + fc * 31 + 1],
                                                                     scalar2=pvs("dwb", fc), op0=ALU.mult, op1=ALU.add),
                                  reads=rk + [K("y", fc, 0), K("y", fc, 1)], writes=[K("y", fc, 0), K("y", fc, 1)])
                            for k in range(1, 31):
                                sc.op("dve", lambda en: en.scalar_tensor_tensor(out=y[:, fc, :], in0=u_pad[:, fc, hf * HS + k:hf * HS + k + HS],
                                                                                scalar=pv[:, odw + fc * 31 + k:odw + fc * 31 + k + 1], op0=ALU.mult, in1=y[:, fc, :], op1=ALU.add),
                                      reads=[K("y", fc, 0), K("y", fc, 1)], writes=[K("y", fc, 0), K("y", fc, 1)])
                        for tt in range(2):
                            t = hf * 2 + tt
                            tl = slice(t * TT, (t + 1) * TT)
                            hl = slice(tt * TT, (tt + 1) * TT)
                            for fc in range(4):
                                s_ = fc % 2
                                sc.op("act", lambda en: en.activation(out=ysq[s_][:], in_=y[:, fc, hl], func=AF.Square), reads=[K("y", fc, tt)], writes=[K("ysq", s_)])
                                sc.op("pool", lambda en: en.tensor_copy(out=ybf[s_][:], in_=y[:, fc, hl]), reads=[K("y", fc, tt)], writes=[K("ybf", s_)])
                                sc.op("pe", lambda en: en.matmul(ps[0][:], lhsT=ones_bf[:], rhs=ybf[s_][:], start=(fc == 0), stop=(fc == 3)),
                                      reads=[K("ones"), K("ybf", s_)], writes=[K("ps", 0)], inc=True)
                                sc.op("pe", lambda en: en.matmul(ps[1][:], lhsT=ones_bf[:], rhs=ysq[s_][:], start=(fc == 0), stop=(fc == 3)),
                                      reads=[K("ones"), K("ysq", s_)], writes=[K("ps", 1)], inc=True)
                            sc.op("dve", lambda en: en.tensor_scalar(out=mean[:], in0=ps[0][:], scalar1=1.0 / 512.0, scalar2=None, op0=ALU.mult), reads=[K("ps", 0)], writes=[K("mean")])
                            sc.op("pool", lambda en: en.tensor_tensor(out=msq[:], in0=mean[:], in1=mean[:], op=ALU.mult), reads=[K("mean")], writes=[K("msq")])
                            sc.op("dve", lambda en: en.scalar_tensor_tensor(out=rs[:], in0=ps[1][:], scalar=1.0 / 512.0, op0=ALU.mult, in1=msq[:], op1=ALU.subtract),
                                  reads=[K("ps", 1), K("msq")], writes=[K("rs")])
                            sc.op("act", lambda en: en.activation(out=rs[:], in_=rs[:], func=AF.Ln, bias=EPS), reads=[K("rs")], writes=[K("rs")])
                            sc.op("act", lambda en: en.activation(out=rs[:], in_=rs[:], func=AF.Exp, scale=-0.5), reads=[K("rs")], writes=[K("rs")])
                            for fc in range(4):
                                s_ = fc % 2
                                sc.op("pool", lambda en: en.tensor_tensor(out=t1[s_][:], in0=y[:, fc, hl], in1=mean[:], op=ALU.subtract), reads=[K("y", fc, tt), K("mean")], writes=[K("t1", s_)])
                                sc.op("pool", lambda en: en.tensor_tensor(out=t1[s_][:], in0=t1[s_][:], in1=rs[:], op=ALU.mult), reads=[K("t1", s_), K("rs")], writes=[K("t1", s_)])
                                sc.op("act", lambda en: en.activation(out=hT[:, fc, tl], in_=t1[s_][:], func=AF.Silu, scale=pvs("lng", fc), bias=pvs("lnb", fc)),
                                      reads=[K("t1", s_)], writes=[K("h", fc, t)])
                    barrier()
            out_proj(l, b, evout_d, hT)

        for si in range(nseq):
            b = si
            for c in range(NCH):
                sc.dma("sp", f"d_x{c}", [(xT[:, c, :], xT_d[si, c * 128:(c + 1) * 128, :])], writes=[K("x", c, t) for t in range(NT)])
            for ph in phases:
                l = int(ph[1])
                if ph[0] == "e":
                    phase_moe(l, b)
                elif ph == "m0":
                    phase_m0(b)
                elif ph == "m1":
                    phase_m1(b)
            for c in range(NCH):
                sc.dma("sp", f"d_o{c}", [(outT_d[si, c * 128:(c + 1) * 128, :], xT[:, c, :])], reads=[K("x", c, t) for t in range(NT)])
        for e_ in ("sp",):
            sc.wait_all(e_)
        print(f"[build] instructions={sc.n_inst} waits={sc.n_wait} counts={ {k: v for k, v in sc.count.items() if k in ('pe','act','dve','pool')} }")
    return nc


def kernel(**inputs):
    inp = {k: np.asarray(v) for k, v in inputs.items()}
    x = inp["x"]
    B = x.shape[0]
    nseq = B // N_CORES
    nc = build_program(nseq=nseq)
    cst = make_consts()
    shared = {
        "cst": cst,
        "mod_w": np.ascontiguousarray(inp["mod_w"], np.float32),
        "ev_in_w": np.ascontiguousarray(inp["ev_in_w"][0], np.float32),
        "ev_out_w": np.ascontiguousarray(inp["ev_out_w"][0], np.float32),
        "od_in_w": np.ascontiguousarray(inp["od_in_w"][0], np.float32),
        "od_out_w": np.ascontiguousarray(inp["od_out_w"][0], np.float32),
        "od_rg_w": np.ascontiguousarray(inp["od_rg_w"][0], np.float32),
        "od_ig_w": np.ascontiguousarray(inp["od_ig_w"][0], np.float32),
        "router_w": np.ascontiguousarray(inp["router_w"], np.float32),
        "ex_w1": np.ascontiguousarray(inp["ex_w1"], np.float32),
        "ex_w3": np.ascontiguousarray(inp["ex_w3"], np.float32),
        "ex_w2": np.ascontiguousarray(inp["ex_w2"], np.float32),
    }
    in_maps = []
    for core in range(N_CORES):
        b0 = core * nseq
        m = dict(shared)
        m["xT"] = np.ascontiguousarray(np.transpose(x[b0:b0 + nseq], (0, 2, 1)))
        m["pvec"] = make_pvec(inp, b0, nseq)
        in_maps.append(m)
    res = run_bass_kernel_spmd(nc, in_maps, core_ids=list(range(N_CORES)))
    out = np.empty_like(x)
    for core in range(N_CORES):
        b0 = core * nseq
        out[b0:b0 + nseq] = np.transpose(res.results[core]["outT"], (0, 2, 1))
    return out
```

```python
import numpy as np
from contextlib import ExitStack
import concourse.bass as bass
import concourse.mybir as mybir
from concourse.bass_utils import run_bass_kernel_spmd

F32 = mybir.dt.float32
BF16 = mybir.dt.bfloat16
AF = mybir.ActivationFunctionType
ALU = mybir.AluOpType
AX = mybir.AxisListType

D = 1024
S = 2048
NCH = 8
TT = 512
NT = 4
NE = 16
EPS = 1e-6
N_CORES = 8
SEQ_PER_CORE = 4


def _fm(v):
    v = np.asarray(v, np.float32)
    return np.ascontiguousarray(v.reshape(-1, 128).T)


PV_FIELDS = [("mixg", 16), ("ffng", 16), ("modb", 96), ("dwb", 4), ("lng", 4), ("lnb", 4), ("dww", 124),
             ("ocw", 32), ("ocb", 8), ("rgb", 8), ("igb", 8), ("lam", 8), ("qg", 1), ("kg", 1), ("rb", 16), ("cT", 32)]
PV_OFF = {}
_o = 0
for _n, _w in PV_FIELDS:
    PV_OFF[_n] = (_o, _w)
    _o += _w
NV = _o

CST_FIELDS = [("ident", 128), ("ustrict", 128), ("bdones", 128), ("ones", 128), ("mask", 2048), ("sel", 2048), ("nmask", 2048)]
CST_OFF = {}
_o = 0
for _n, _w in CST_FIELDS:
    CST_OFF[_n] = (_o, _w)
    _o += _w
NCST = _o


def make_consts():
    c = np.zeros((128, NCST), np.float32)
    j = np.arange(128)[:, None]
    s = np.arange(128)[None, :]
    c[:, CST_OFF["ident"][0]:][:, :128] = (j == s)
    c[:, CST_OFF["ustrict"][0]:][:, :128] = (j > s)
    c[:, CST_OFF["bdones"][0]:][:, :128] = ((j // 64) == (s // 64))
    c[:, CST_OFF["ones"][0]:][:, :128] = 1.0
    t = np.arange(512)[None, :]
    for d in range(4):
        o = CST_OFF["mask"][0] + d * 512
        c[:, o:o + 512] = ((128 * d + j) < t)
    o = CST_OFF["sel"][0]
    for e in range(16):
        c[e, o + e * 128:o + (e + 1) * 128] = 1.0
    for d in range(4):
        o = CST_OFF["nmask"][0] + d * 512
        c[:, o:o + 512] = np.where((128 * d + j) < t, 0.0, 30000.0)
    return c


def make_pvec(inp, b0, nseq):
    pv = np.zeros((128, NV), np.float32)

    def put(name, arr):
        o, w = PV_OFF[name]
        assert arr.shape == (128, w), (name, arr.shape, w)
        pv[:, o:o + w] = arr

    put("mixg", np.concatenate([_fm(inp["mix_norm_g"][l]) for l in range(2)], axis=1))
    put("ffng", np.concatenate([_fm(inp["ffn_norm_g"][l]) for l in range(2)], axis=1))
    put("modb", np.concatenate([_fm(inp["mod_b"][l]) for l in range(2)], axis=1))
    put("dwb", _fm(inp["ev_dw_b"][0]))
    put("lng", _fm(inp["ev_ln_g"][0]))
    put("lnb", _fm(inp["ev_ln_b"][0]))
    dw = np.asarray(inp["ev_dw_w"][0], np.float32)
    put("dww", np.ascontiguousarray(dw.reshape(31, 4, 128).transpose(2, 1, 0).reshape(128, 124)))
    cw = np.asarray(inp["od_conv_w"][0], np.float32)
    put("ocw", np.ascontiguousarray(cw.reshape(4, 8, 128).transpose(2, 1, 0).reshape(128, 32)))
    put("ocb", _fm(inp["od_conv_b"][0]))
    put("rgb", _fm(inp["od_rg_b"][0]))
    put("igb", _fm(inp["od_ig_b"][0]))
    put("lam", _fm(inp["od_lam"][0]))
    put("qg", np.tile(np.asarray(inp["ev_q_g"][0], np.float32), 2)[:, None])
    put("kg", np.tile(np.asarray(inp["ev_k_g"][0], np.float32), 2)[:, None])
    put("rb", np.tile(np.asarray(inp["router_b"], np.float32)[None, :], (128, 1)))
    cc = np.zeros((4, 1024), np.float32)
    cc[:nseq] = np.asarray(inp["c"][b0:b0 + nseq], np.float32)
    put("cT", np.ascontiguousarray(cc.reshape(4, 8, 128).transpose(2, 1, 0).reshape(128, 32)))
    return pv


class Sched:
    def __init__(self, nc, es):
        self.nc = nc
        self.es = es
        self.engines = {"pe": nc.tensor, "act": nc.scalar, "dve": nc.vector, "pool": nc.gpsimd, "sp": nc.sync}
        self.sems = {}
        self.count = {}
        self.seen = {e: {} for e in self.engines}
        self.lastw = {}
        self.readers = {}
        self.n_inst = 0
        self.n_wait = 0
        for e in ("pe", "act", "dve", "pool"):
            self._sem(e)

    def _sem(self, name):
        if name not in self.sems:
            self.sems[name] = self.es.enter_context(self.nc.semaphore(name))
            self.count[name] = 0
        return self.sems[name]

    def _wait(self, eng, sem, val):
        if val <= 0 or self.seen[eng].get(sem, 0) >= val:
            return
        self.engines[eng].wait_ge(self.sems[sem], val)
        self.seen[eng][sem] = val
        self.n_wait += 1

    def _deps(self, eng, reads, writes):
        need = {}
        for k in reads:
            ev = self.lastw.get(k)
            if ev is not None and ev[1] > need.get(ev[0], 0):
                need[ev[0]] = ev[1]
        for k in writes:
            ev = self.lastw.get(k)
            if ev is not None and ev[1] > need.get(ev[0], 0):
                need[ev[0]] = ev[1]
            rd = self.readers.get(k)
            if rd:
                for s_, v_ in rd.items():
                    if v_ > need.get(s_, 0):
                        need[s_] = v_
        for s_, v_ in need.items():
            if s_ == eng:
                if v_ > self.count[eng]:
                    continue
                if eng == "pe":
                    continue
            self._wait(eng, s_, v_)

    def _record(self, ev, reads, writes):
        for k in reads:
            rd = self.readers.setdefault(k, {})
            if ev[1] > rd.get(ev[0], 0):
                rd[ev[0]] = ev[1]
        for k in writes:
            self.lastw[k] = ev
            self.readers[k] = {}

    def op(self, eng, fn, reads=(), writes=(), inc=True):
        self._deps(eng, reads, writes)
        inst = fn(self.engines[eng])
        self.n_inst += 1
        if inc:
            inst.then_inc(self.sems[eng], 1)
            self.count[eng] += 1
            ev = (eng, self.count[eng])
        else:
            ev = (eng, self.count[eng] + 1)
        self._record(ev, reads, writes)
        return inst

    def dma(self, q, sem, pairs, reads=(), writes=()):
        self._sem(sem)
        self._deps(q, reads, writes)
        self._wait(q, sem, self.count[sem])
        for out, in_ in pairs:
            self.engines[q].dma_start(out=out, in_=in_).then_inc(self.sems[sem], 16)
            self.count[sem] += 16
            self.n_inst += 1
        ev = (sem, self.count[sem])
        self._record(ev, reads, writes)

    def wait_all(self, eng):
        for s_, v_ in self.count.items():
            self._wait(eng, s_, v_)


def build_program(nseq=SEQ_PER_CORE, phases=("m0", "e0", "m1", "e1")):
    nc = bass.Bass("TRN2", target_bir_lowering=False)
    dr = {}

    def din(name, shape):
        dr[name] = nc.dram_tensor(name, list(shape), F32, kind="ExternalInput").ap()
        return dr[name]

    xT_d = din("xT", [nseq, D, S])
    pvec_d = din("pvec", [128, NV])
    cst_d = din("cst", [128, NCST])
    modw_d = din("mod_w", [2, D, 6 * D])
    evin_d = din("ev_in_w", [D, 2560])
    evout_d = din("ev_out_w", [D, D])
    odin_d = din("od_in_w", [D, 2048])
    odout_d = din("od_out_w", [D, D])
    rgw_d = din("od_rg_w", [8, 128, 128])
    igw_d = din("od_ig_w", [8, 128, 128])
    rw_d = din("router_w", [D, NE])
    w1_d = din("ex_w1", [2, NE, D, 512])
    w3_d = din("ex_w3", [2, NE, D, 512])
    w2_d = din("ex_w2", [2, NE, 512, D])
    outT_d = nc.dram_tensor("outT", [nseq, D, S], F32, kind="ExternalOutput").ap()

    _uid = [0]

    def SBT(name, shape, dt):
        _uid[0] += 1
        return nc.sbuf_tensor(f"{name}_{_uid[0]}", shape, dt)

    with ExitStack() as es:
        sc = Sched(nc, es)

        def sb(name, shape, dt):
            return es.enter_context(SBT(name, list(shape), dt))

        xT = sb("xT_s", [128, NCH, S], F32)
        hT = sb("hT_s", [128, NCH, S], BF16)
        pv = sb("pv_s", [128, NV], F32)
        ident = sb("ident", [128, 128], F32)
        ones_bf = sb("ones_bf", [128, 128], BF16)
        ustr_bf = sb("ustr_bf", [128, 128], BF16)
        bdon_bf = sb("bdon_bf", [128, 128], BF16)
        sel = sb("sel", [16, NE, 128], F32)
        rw = sb("rw", [128, NCH, NE], F32)
        modT = [sb(f"modT{l}", [128, 48, 4], F32) for l in range(2)]
        A1 = [sb(f"A1_{l}", [128, NCH, 4], F32) for l in range(2)]
        A2 = [sb(f"A2_{l}", [128, NCH, 4], F32) for l in range(2)]
        cl8 = sb("cl8", [128, NCH], F32)
        ps = [es.enter_context(nc.psum_tensor(f"ps{i}", [128, 512], F32)) for i in range(8)]

        def pvs(name, i=0, n=None):
            o, w = PV_OFF[name]
            n = 1 if n is None else n
            return pv[:, o + i:o + i + n]

        def K(*a):
            return a

        o_, _ = CST_OFF["ident"]
        sc.dma("sp", "d_c0", [(pv[:], pvec_d[:, :]), (ident[:], cst_d[:, o_:o_ + 128])], writes=[K("pv"), K("ident")])
        o_, _ = CST_OFF["sel"]
        sc.dma("sp", "d_c1", [(sel[:], cst_d[0:16, o_:o_ + 2048].rearrange("p (e m) -> p e m", e=NE)),
                              (rw[:], rw_d.rearrange("(c p) e -> p c e", p=128))], writes=[K("sel"), K("rw")])
        oo, ou, ob, om = CST_OFF["ones"][0], CST_OFF["ustrict"][0], CST_OFF["bdones"][0], CST_OFF["mask"][0]
        sc.dma("pool", "d_c2", [(ones_bf[:], cst_d[:, oo:oo + 128]), (ustr_bf[:], cst_d[:, ou:ou + 128]),
                                (bdon_bf[:], cst_d[:, ob:ob + 128])],
               writes=[K("ones"), K("ustr"), K("bdon")])

        with ExitStack() as es2:
            cond = es2.enter_context(SBT("cond", [128, NCH, 4], F32))
            mw = [es2.enter_context(SBT(f"mw{i}", [128, NCH, 512], F32)) for i in range(2)]
            o_, _ = PV_OFF["cT"]
            sc.op("act", lambda e: e.activation(out=cond[:].rearrange("p c b -> p (c b)"), in_=pv[:, o_:o_ + 32], func=AF.Silu),
                  reads=[K("pv")], writes=[K("cond")])
            gi = 0
            for l in range(2):
                for g in range(12):
                    slot = gi % 2
                    gi += 1
                    sc.dma("sp", f"d_mw{slot}", [(mw[slot][:], modw_d[l, :, g * 512:(g + 1) * 512].rearrange("(c p) f -> p c f", p=128))],
                           writes=[K("mw", slot)])
                    for fc in range(4):
                        f = g * 4 + fc
                        for kc in range(NCH):
                            sc.op("pe", lambda e: e.matmul(ps[0][:, f * 4:(f + 1) * 4], lhsT=mw[slot][:, kc, fc * 128:(fc + 1) * 128],
                                                           rhs=cond[:, kc, :], start=(kc == 0), stop=(kc == NCH - 1)),
                                  reads=[K("mw", slot), K("cond")], writes=[K("ps", 0)], inc=(kc == NCH - 1))
                o_, _ = PV_OFF["modb"]
                sc.op("dve", lambda e: e.tensor_tensor(out=modT[l][:], in0=ps[0][:, 0:192].rearrange("p (f b) -> p f b", b=4),
                                                       in1=pv[:, o_ + l * 48:o_ + (l + 1) * 48].unsqueeze(2).broadcast_to([128, 48, 4]),
                                                       op=ALU.add),
                      reads=[K("ps", 0), K("pv")], writes=[K("modT", l)])
                og, _ = PV_OFF["mixg"]
                sc.op("dve", lambda e: e.scalar_tensor_tensor(out=A1[l][:], in0=modT[l][:, 8:16, :], scalar=1.0, op0=ALU.add,
                                                              in1=pv[:, og + l * 8:og + (l + 1) * 8].unsqueeze(2).broadcast_to([128, 8, 4]),
                                                              op1=ALU.mult),
                      reads=[K("modT", l), K("pv")], writes=[K("A1", l)])
                og, _ = PV_OFF["ffng"]
                sc.op("dve", lambda e: e.scalar_tensor_tensor(out=A2[l][:], in0=modT[l][:, 32:40, :], scalar=1.0, op0=ALU.add,
                                                              in1=pv[:, og + l * 8:og + (l + 1) * 8].unsqueeze(2).broadcast_to([128, 8, 4]),
                                                              op1=ALU.mult),
                      reads=[K("modT", l), K("pv")], writes=[K("A2", l)])
            ol, _ = PV_OFF["lam"]
            sc.op("act", lambda e: e.activation(out=cl8[:], in_=pv[:, ol:ol + 8], func=AF.Exp, scale=-1.0), reads=[K("pv")], writes=[K("cl8")])
            sc.op("act", lambda e: e.activation(out=cl8[:], in_=cl8[:], func=AF.Ln, bias=1.0), reads=[K("cl8")], writes=[K("cl8")])
            sc.op("dve", lambda e: e.tensor_scalar(out=cl8[:], in0=cl8[:], scalar1=-8.0, scalar2=None, op0=ALU.mult),
                  reads=[K("cl8")], writes=[K("cl8")])
            for e_ in ("pe", "act", "dve", "pool", "sp"):
                sc.wait_all(e_)

        xkeys = [K("x", c, t) for c in range(NCH) for t in range(NT)]
        hkeys = [K("h", c, t) for c in range(NCH) for t in range(NT)]

        def phase_norm(b, Aap, Sap, router, lgT=None):
            with ExitStack() as esn:
                rstd = esn.enter_context(SBT("rstd", [128, NT, TT], F32))
                sqb = [esn.enter_context(SBT(f"sqb{i}", [128, TT], BF16)) for i in range(2)]
                tmp = [esn.enter_context(SBT(f"ntmp{i}", [128, TT], F32)) for i in range(2)]
                h32 = esn.enter_context(SBT("h32", [128, NCH, TT], F32)) if router else None
                i2 = 0
                for t in range(NT):
                    tl = slice(t * TT, (t + 1) * TT)
                    for c in range(NCH):
                        s_ = i2 % 2
                        i2 += 1
                        if c % 2 == 0:
                            sc.op("act", lambda e: e.activation(out=sqb[s_][:], in_=xT[:, c, tl], func=AF.Square),
                                  reads=[K("x", c, t)], writes=[K("sqb", s_)])
                        else:
                            sc.op("pool", lambda e: e.tensor_tensor(out=sqb[s_][:], in0=xT[:, c, tl], in1=xT[:, c, tl], op=ALU.mult),
                                  reads=[K("x", c, t)], writes=[K("sqb", s_)])
                        sc.op("pe", lambda e: e.matmul(ps[t][:], lhsT=ones_bf[:], rhs=sqb[s_][:], start=(c == 0), stop=(c == NCH - 1)),
                              reads=[K("ones"), K("sqb", s_)], writes=[K("ps", t)], inc=True)
                for t in range(NT):
                    sc.op("act", lambda e: e.activation(out=rstd[:, t, :], in_=ps[t][:], func=AF.Ln, scale=1.0 / D, bias=EPS),
                          reads=[K("ps", t)], writes=[K("rstd", t)])
                for t in range(NT):
                    sc.op("act", lambda e: e.activation(out=rstd[:, t, :], in_=rstd[:, t, :], func=AF.Exp, scale=-0.5),
                          reads=[K("rstd", t)], writes=[K("rstd", t)])
                i2 = 0
                for t in range(NT):
                    tl = slice(t * TT, (t + 1) * TT)
                    for c in range(NCH):
                        s_ = i2 % 2
                        i2 += 1
                        sc.op("pool", lambda e: e.tensor_tensor(out=tmp[s_][:], in0=xT[:, c, tl], in1=rstd[:, t, :], op=ALU.mult),
                              reads=[K("x", c, t), K("rstd", t)], writes=[K("ntmp", s_)])
                        if router:
                            sc.op("act", lambda e: e.activation(out=h32[:, c, :], in_=tmp[s_][:], func=AF.Identity,
                                                                scale=Aap[:, c, b:b + 1], bias=Sap[:, c, b:b + 1]),
                                  reads=[K("ntmp", s_), K("modall")], writes=[K("h32", c)])
                            sc.op("dve", lambda e: e.tensor_copy(out=hT[:, c, tl], in_=h32[:, c, :]),
                                  reads=[K("h32", c)], writes=[K("h", c, t)])
                        else:
                            sc.op("act", lambda e: e.activation(out=hT[:, c, tl], in_=tmp[s_][:], func=AF.Identity,
                                                                scale=Aap[:, c, b:b + 1], bias=Sap[:, c, b:b + 1]),
                                  reads=[K("ntmp", s_), K("modall")], writes=[K("h", c, t)])
                    if router:
                        for c in range(NCH):
                            sc.op("pe", lambda e: e.matmul(ps[4][0:16, :], lhsT=rw[:, c, :], rhs=h32[:, c, :], start=(c == 0), stop=(c == NCH - 1)),
                                  reads=[K("rw"), K("h32", c)], writes=[K("ps", 4)], inc=(c == NCH - 1))
                        sc.op("act", lambda e: e.activation(out=lgT[:, tl], in_=ps[4][0:16, :], func=AF.Identity),
                              reads=[K("ps", 4)], writes=[K("lgT", t)])
                for e_ in ("pe", "act", "dve", "pool", "sp"):
                    sc.wait_all(e_)

        def phase_route(lgT, gT):
            with ExitStack() as esr:
                def t_(name, shape):
                    return esr.enter_context(SBT(name, list(shape), F32))
                scr = t_("r_sc", [128, 256])
                bi = t_("r_bi", [128, 256])
                p6 = t_("r_p6", [128, 64, 6])
                gs = t_("r_gs", [128, 64])
                gmax = t_("r_gmax", [128, 16])
                goh = t_("r_goh", [128, 64])
                m1 = t_("r_m1", [128, 64])
                e1 = t_("r_e1", [128, 256])
                b2 = t_("r_b2", [128, 256])
                m2 = t_("r_m2", [128, 64])
                sl = t_("r_sl", [128, 256])
                den = t_("r_den", [128, 16])
                gates = t_("r_gates", [128, 256])
                for tc in range(16):
                    sc.op("pe", lambda e: e.transpose(out=ps[5][:, tc * 16:(tc + 1) * 16], in_=lgT[:, tc * 128:(tc + 1) * 128], identity=ident[0:16, 0:16]),
                          reads=[K("lgT", tc // 4), K("ident")], writes=[K("ps", 5)], inc=(tc == 15))
                sc.op("act", lambda e: e.activation(out=scr[:], in_=ps[5][:, 0:256], func=AF.Sigmoid), reads=[K("ps", 5)], writes=[K("r_sc")])
                o_, _ = PV_OFF["rb"]
                V = sc.op
                V("dve", lambda e: e.tensor_tensor(out=bi[:].rearrange("p (t e) -> p t e", e=16), in0=scr[:].rearrange("p (t e) -> p t e", e=16),
                                                   in1=pv[:, o_:o_ + 16].unsqueeze(1).broadcast_to([128, 16, 16]), op=ALU.add),
                  reads=[K("r_sc"), K("pv")], writes=[K("r_bi")])
                bi4 = bi[:].rearrange("p (g k) -> p g k", k=4)
                V("dve", lambda e: e.tensor_tensor(out=p6[:, :, 0:3], in0=bi4[:, :, 0:3], in1=bi4[:, :, 1:4], op=ALU.add), reads=[K("r_bi")], writes=[K("r_p6a")])
                V("dve", lambda e: e.tensor_tensor(out=p6[:, :, 3:5], in0=bi4[:, :, 0:2], in1=bi4[:, :, 2:4], op=ALU.add), reads=[K("r_bi")], writes=[K("r_p6b")])
                V("dve", lambda e: e.tensor_tensor(out=p6[:, :, 5:6], in0=bi4[:, :, 0:1], in1=bi4[:, :, 3:4], op=ALU.add), reads=[K("r_bi")], writes=[K("r_p6c")])
                V("dve", lambda e: e.tensor_reduce(out=gs[:], in_=p6[:], axis=AX.X, op=ALU.max), reads=[K("r_p6a"), K("r_p6b"), K("r_p6c")], writes=[K("r_gs")])
                V("dve", lambda e: e.tensor_reduce(out=gmax[:], in_=gs[:].rearrange("p (t g) -> p t g", g=4), axis=AX.X, op=ALU.max), reads=[K("r_gs")], writes=[K("r_gmax")])
                V("dve", lambda e: e.tensor_tensor(out=goh[:].rearrange("p (t g) -> p t g", g=4), in0=gs[:].rearrange("p (t g) -> p t g", g=4),
                                                   in1=gmax[:].unsqueeze(2).broadcast_to([128, 16, 4]), op=ALU.is_equal), reads=[K("r_gs"), K("r_gmax")], writes=[K("r_goh")])
                V("dve", lambda e: e.tensor_reduce(out=m1[:], in_=bi4, axis=AX.X, op=ALU.max), reads=[K("r_bi")], writes=[K("r_m1")])
                V("dve", lambda e: e.tensor_tensor(out=e1[:].rearrange("p (g k) -> p g k", k=4), in0=bi4, in1=m1[:].unsqueeze(2).broadcast_to([128, 64, 4]), op=ALU.is_equal),
                  reads=[K("r_bi"), K("r_m1")], writes=[K("r_e1")])
                V("dve", lambda e: e.scalar_tensor_tensor(out=b2[:], in0=e1[:], scalar=-1.0e9, op0=ALU.mult, in1=bi[:], op1=ALU.add), reads=[K("r_e1"), K("r_bi")], writes=[K("r_b2")])
                V("dve", lambda e: e.tensor_reduce(out=m2[:], in_=b2[:].rearrange("p (g k) -> p g k", k=4), axis=AX.X, op=ALU.max), reads=[K("r_b2")], writes=[K("r_m2")])
                V("dve", lambda e: e.tensor_tensor(out=sl[:].rearrange("p (g k) -> p g k", k=4), in0=bi4, in1=m2[:].unsqueeze(2).broadcast_to([128, 64, 4]), op=ALU.is_ge),
                  reads=[K("r_bi"), K("r_m2")], writes=[K("r_sl")])
                V("dve", lambda e: e.tensor_tensor(out=sl[:].rearrange("p (g k) -> p g k", k=4), in0=sl[:].rearrange("p (g k) -> p g k", k=4),
                                                   in1=goh[:].unsqueeze(2).broadcast_to([128, 64, 4]), op=ALU.mult), reads=[K("r_sl"), K("r_goh")], writes=[K("r_sl")])
                V("dve", lambda e: e.tensor_tensor(out=sl[:], in0=sl[:], in1=scr[:], op=ALU.mult), reads=[K("r_sl"), K("r_sc")], writes=[K("r_sl")])
                V("dve", lambda e: e.tensor_reduce(out=den[:], in_=sl[:].rearrange("p (t e) -> p t e", e=16), axis=AX.X, op=ALU.add), reads=[K("r_sl")], writes=[K("r_den")])
                V("dve", lambda e: e.reciprocal(out=den[:], in_=den[:]), reads=[K("r_den")], writes=[K("r_den")])
                V("dve", lambda e: e.tensor_tensor(out=gates[:].rearrange("p (t e) -> p t e", e=16), in0=sl[:].rearrange("p (t e) -> p t e", e=16),
                                                   in1=den[:].unsqueeze(2).broadcast_to([128, 16, 16]), op=ALU.mult), reads=[K("r_sl"), K("r_den")], writes=[K("r_gates")])
                for tc in range(16):
                    bk = tc // 4
                    sc.op("pe", lambda e: e.transpose(out=ps[bk][0:16, (tc % 4) * 128:(tc % 4 + 1) * 128], in_=gates[:, tc * 16:(tc + 1) * 16], identity=ident[:]),
                          reads=[K("r_gates"), K("ident")], writes=[K("ps", bk)], inc=(tc % 4 == 3))
                for bk in range(4):
                    sc.op("act", lambda e: e.activation(out=gT[:, bk * TT:(bk + 1) * TT], in_=ps[bk][0:16, :], func=AF.Identity),
                          reads=[K("ps", bk)], writes=[K("gT", bk)])
                for e_ in ("pe", "act", "dve", "pool", "sp"):
                    sc.wait_all(e_)

        def phase_moe(l, b):
            with ExitStack() as esm:
                lgT = esm.enter_context(SBT("lgT", [16, S], F32))
                gT = esm.enter_context(SBT("gT", [16, S], F32))
                phase_norm(b, A2[l], modT[l][:, 24:32, :], True, lgT)
                phase_route(lgT, gT)
                w1s = [esm.enter_context(SBT(f"w1s{i}", [128, NCH, 512], BF16)) for i in range(2)]
                w3s = [esm.enter_context(SBT(f"w3s{i}", [128, NCH, 512], BF16)) for i in range(2)]
                w2s = [esm.enter_context(SBT(f"w2s{i}", [128, 4, D], BF16)) for i in range(2)]
                gbc = [esm.enter_context(SBT(f"gbc{i}", [128, TT], F32)) for i in range(2)]
                s1 = [esm.enter_context(SBT(f"s1_{i}", [128, TT], F32)) for i in range(2)]
                sg = [esm.enter_context(SBT(f"sg_{i}", [128, TT], F32)) for i in range(2)]
                actT = [esm.enter_context(SBT(f"actT{i}", [128, 4, TT], BF16)) for i in range(2)]

                def load_w(e):
                    s_ = e % 2
                    sc.dma("pool", f"d_w{s_}",
                           [(w1s[s_][:], w1_d[l, e].rearrange("(c p) f -> p c f", p=128)),
                            (w3s[s_][:], w3_d[l, e].rearrange("(c p) f -> p c f", p=128)),
                            (w2s[s_][:], w2_d[l, e].rearrange("(c p) f -> p c f", p=128))],
                           writes=[K("wexp", s_)])

                load_w(0)
                it = 0
                fi = 0
                for e in range(NE):
                    ws = e % 2
                    if e + 1 < NE:
                        load_w(e + 1)
                    for t in range(NT):
                        tl = slice(t * TT, (t + 1) * TT)
                        a_ = it % 2
                        it += 1
                        sc.op("pe", lambda en: en.matmul(ps[6][:], lhsT=sel[:, e, :], rhs=gT[:, tl], start=True, stop=True),
                              reads=[K("sel"), K("gT", t)], writes=[K("ps", 6)])
                        sc.op("act", lambda en: en.activation(out=gbc[a_][:], in_=ps[6][:], func=AF.Identity), reads=[K("ps", 6)], writes=[K("gbc", a_)])
                        for fc in range(4):
                            f_ = fi % 2
                            fi += 1
                            for kc in range(NCH):
                                sc.op("pe", lambda en: en.matmul(ps[0 + f_][:], lhsT=w1s[ws][:, kc, fc * 128:(fc + 1) * 128], rhs=hT[:, kc, tl],
                                                                 start=(kc == 0), stop=(kc == NCH - 1)),
                                      reads=[K("wexp", ws), K("h", kc, t)], writes=[K("ps", 0 + f_)], inc=(kc == NCH - 1))
                            for kc in range(NCH):
                                sc.op("pe", lambda en: en.matmul(ps[2 + f_][:], lhsT=w3s[ws][:, kc, fc * 128:(fc + 1) * 128], rhs=hT[:, kc, tl],
                                                                 start=(kc == 0), stop=(kc == NCH - 1)),
                                      reads=[K("wexp", ws), K("h", kc, t)], writes=[K("ps", 2 + f_)], inc=(kc == NCH - 1))
                            sc.op("act", lambda en: en.activation(out=s1[f_][:], in_=ps[0 + f_][:], func=AF.Silu), reads=[K("ps", 0 + f_)], writes=[K("s1", f_)])
                            sc.op("pool", lambda en: en.tensor_tensor(out=sg[f_][:], in0=s1[f_][:], in1=gbc[a_][:], op=ALU.mult),
                                  reads=[K("s1", f_), K("gbc", a_)], writes=[K("sg", f_)])
                            sc.op("dve", lambda en: en.tensor_tensor(out=actT[a_][:, fc, :], in0=ps[2 + f_][:], in1=sg[f_][:], op=ALU.mult),
                                  reads=[K("ps", 2 + f_), K("sg", f_)], writes=[K("actT", a_, fc)])
                        for dc in range(NCH):
                            o_ = 4 + dc % 2
                            for fc in range(4):
                                sc.op("pe", lambda en: en.matmul(ps[o_][:], lhsT=w2s[ws][:, fc, dc * 128:(dc + 1) * 128], rhs=actT[a_][:, fc, :],
                                                                 start=(fc == 0), stop=(fc == 3)),
                                      reads=[K("wexp", ws), K("actT", a_, fc)], writes=[K("ps", o_)], inc=(fc == 3))
                            sc.op("dve", lambda en: en.scalar_tensor_tensor(out=xT[:, dc, tl], in0=ps[o_][:], scalar=modT[l][:, 40 + dc, b:b + 1], op0=ALU.mult,
                                                                            in1=xT[:, dc, tl], op1=ALU.add),
                                  reads=[K("ps", o_), K("x", dc, t), K("modall")], writes=[K("x", dc, t)])
                for e_ in ("pe", "act", "dve", "pool", "sp"):
                    sc.wait_all(e_)

        def barrier():
            for e_ in ("pe", "act", "dve", "pool", "sp"):
                sc.wait_all(e_)

        def out_proj(l, b, w_d, zT_):
            with ExitStack() as eso:
                ow = eso.enter_context(SBT("ow", [128, NCH, D], BF16))
                sc.dma("pool", "d_ow", [(ow[:, 0:4, :], w_d[0:512, :].rearrange("(c p) f -> p c f", p=128)),
                                        (ow[:, 4:8, :], w_d[512:1024, :].rearrange("(c p) f -> p c f", p=128))], writes=[K("ow")])
                i_ = 0
                for dc in range(NCH):
                    for t in range(NT):
                        tl = slice(t * TT, (t + 1) * TT)
                        o_ = i_ % 4
                        i_ += 1
                        for kc in range(NCH):
                            sc.op("pe", lambda en: en.matmul(ps[o_][:], lhsT=ow[:, kc, dc * 128:(dc + 1) * 128], rhs=zT_[:, kc, tl],
                                                             start=(kc == 0), stop=(kc == NCH - 1)),
                                  reads=[K("ow"), K("h", kc, t)], writes=[K("ps", o_)], inc=(kc == NCH - 1))
                        sc.op("dve", lambda en: en.scalar_tensor_tensor(out=xT[:, dc, tl], in0=ps[o_][:], scalar=modT[l][:, 16 + dc, b:b + 1], op0=ALU.mult,
                                                                        in1=xT[:, dc, tl], op1=ALU.add),
                              reads=[K("ps", o_), K("x", dc, t)], writes=[K("x", dc, t)])
                barrier()

        def phase_m1(b):
            l = 1
            phase_norm(b, A1[l], modT[l][:, 0:8, :], False)
            HS = S // 2
            with ExitStack() as esz:
                zT = esz.enter_context(SBT("zT", [128, NCH, S], BF16))
                with ExitStack() as es1:
                    def t_(name, shape, dt=F32):
                        return es1.enter_context(SBT(name, list(shape), dt))
                    wyx = [t_(f"wyx{i}", [128, NCH, 256], BF16) for i in range(2)]
                    rgw = t_("rgw", [128, NCH, 128], BF16)
                    igw = t_("igw", [128, NCH, 128], BF16)
                    xbr = t_("xbr", [128, 3 + S])
                    Bgy = t_("Bgy", [128, HS])
                    Bxc = t_("Bxc", [128, HS])
                    Br = t_("Br", [128, HS])
                    Bi = t_("Bi", [128, HS])
                    Bm = t_("Bm", [128, HS])
                    xcb = t_("xcb", [128, HS], BF16)
                    carry = t_("carry", [128, 1])
                    sc.dma("pool", "d_gw", [(rgw[:], rgw_d.rearrange("n k j -> k n j")), (igw[:], igw_d.rearrange("n k j -> k n j"))], writes=[K("gw")])
                    sc.op("pool", lambda en: en.memset(xbr[:, 0:3], 0.0), writes=[K("xbrpad")])
                    ocw, _ = PV_OFF["ocw"]

                    def load_wyx(c):
                        s_ = c % 2
                        sc.dma("pool", f"d_wyx{s_}", [(wyx[s_][:, :, 0:128], odin_d[:, c * 128:(c + 1) * 128].rearrange("(k p) f -> p k f", p=128)),
                                                      (wyx[s_][:, :, 128:256], odin_d[:, 1024 + c * 128:1024 + (c + 1) * 128].rearrange("(k p) f -> p k f", p=128))],
                               writes=[K("wyx", s_)])
                    load_wyx(0)
                    for c in range(NCH):
                        ws = c % 2
                        if c + 1 < NCH:
                            load_wyx(c + 1)
                        for hf in range(2):
                            for tt in range(2):
                                t = hf * 2 + tt
                                tl = slice(t * TT, (t + 1) * TT)
                                hl = slice(tt * TT, (tt + 1) * TT)
                                for kc in range(NCH):
                                    sc.op("pe", lambda en: en.matmul(ps[tt][:], lhsT=wyx[ws][:, kc, 0:128], rhs=hT[:, kc, tl], start=(kc == 0), stop=(kc == NCH - 1)),
                                          reads=[K("wyx", ws), K("h", kc, t)], writes=[K("ps", tt)], inc=(kc == NCH - 1))
                                for kc in range(NCH):
                                    sc.op("pe", lambda en: en.matmul(ps[2 + tt][:], lhsT=wyx[ws][:, kc, 128:256], rhs=hT[:, kc, tl], start=(kc == 0), stop=(kc == NCH - 1)),
                                          reads=[K("wyx", ws), K("h", kc, t)], writes=[K("ps", 2 + tt)], inc=(kc == NCH - 1))
                                sc.op("act", lambda en: en.activation(out=Bgy[:, hl], in_=ps[tt][:], func=AF.Gelu_apprx_tanh), reads=[K("ps", tt)], writes=[K("Bgy", tt)])
                                sc.op("act", lambda en: en.activation(out=xbr[:, 3 + t * TT:3 + (t + 1) * TT], in_=ps[2 + tt][:], func=AF.Identity),
                                      reads=[K("ps", 2 + tt)], writes=[K("xbr", t)])
                            for tt in range(2):
                                t = hf * 2 + tt
                                hl = slice(tt * TT, (tt + 1) * TT)
                                rk = [K("xbr", t), K("xbrpad")] + ([K("xbr", t - 1)] if t > 0 else [])
                                sc.op("dve", lambda en: en.tensor_scalar(out=Bxc[:, hl], in0=xbr[:, t * TT + 3:t * TT + 3 + TT], scalar1=pv[:, ocw + c * 4 + 3:ocw + c * 4 + 4],
                                                                         scalar2=pvs("ocb", c), op0=ALU.mult, op1=ALU.add),
                                      reads=rk, writes=[K("Bxc", tt)])
                                for k in range(3):
                                    sc.op("dve", lambda en: en.scalar_tensor_tensor(out=Bxc[:, hl], in0=xbr[:, t * TT + k:t * TT + k + TT], scalar=pv[:, ocw + c * 4 + k:ocw + c * 4 + k + 1],
                                                                                    op0=ALU.mult, in1=Bxc[:, hl], op1=ALU.add),
                                          reads=rk + [K("Bxc", tt)], writes=[K("Bxc", tt)])
                                sc.op("pool", lambda en: en.tensor_copy(out=xcb[:, hl], in_=Bxc[:, hl]), reads=[K("Bxc", tt)], writes=[K("xcb", tt)])
                                sc.op("pe", lambda en: en.matmul(ps[4 + tt][:], lhsT=rgw[:, c, :], rhs=xcb[:, hl], start=True, stop=True),
                                      reads=[K("gw"), K("xcb", tt)], writes=[K("ps", 4 + tt)])
                                sc.op("pe", lambda en: en.matmul(ps[6 + tt][:], lhsT=igw[:, c, :], rhs=xcb[:, hl], start=True, stop=True),
                                      reads=[K("gw"), K("xcb", tt)], writes=[K("ps", 6 + tt)])
                            for tt in range(2):
                                hl = slice(tt * TT, (tt + 1) * TT)
                                sc.op("act", lambda en: en.activation(out=Br[:, hl], in_=ps[4 + tt][:], func=AF.Sigmoid, bias=pvs("rgb", c)), reads=[K("ps", 4 + tt)], writes=[K("Br", tt)])
                                sc.op("act", lambda en: en.activation(out=Bi[:, hl], in_=ps[6 + tt][:], func=AF.Sigmoid, bias=pvs("igb", c)), reads=[K("ps", 6 + tt)], writes=[K("Bi", tt)])
                            for tt in range(2):
                                hl = slice(tt * TT, (tt + 1) * TT)
                                sc.op("act", lambda en: en.activation(out=Br[:, hl], in_=Br[:, hl], func=AF.Exp, scale=cl8[:, c:c + 1]), reads=[K("Br", tt)], writes=[K("Br", tt)])
                                sc.op("pool", lambda en: en.tensor_tensor(out=Bm[:, hl], in0=Br[:, hl], in1=Br[:, hl], op=ALU.mult), reads=[K("Br", tt)], writes=[K("Bm", tt)])
                                sc.op("pool", lambda en: en.tensor_tensor(out=Bi[:, hl], in0=Bi[:, hl], in1=Bxc[:, hl], op=ALU.mult), reads=[K("Bi", tt), K("Bxc", tt)], writes=[K("Bi", tt)])
                            for tt in range(2):
                                hl = slice(tt * TT, (tt + 1) * TT)
                                sc.op("act", lambda en: en.activation(out=Bm[:, hl], in_=Bm[:, hl], func=AF.Ln, scale=-1.0, bias=1.0), reads=[K("Bm", tt)], writes=[K("Bm", tt)])
                            for tt in range(2):
                                hl = slice(tt * TT, (tt + 1) * TT)
                                sc.op("act", lambda en: en.activation(out=Bm[:, hl], in_=Bm[:, hl], func=AF.Exp, scale=0.5), reads=[K("Bm", tt)], writes=[K("Bm", tt)])
                            for tt in range(2):
                                t = hf * 2 + tt
                                tl = slice(t * TT, (t + 1) * TT)
                                hl = slice(tt * TT, (tt + 1) * TT)
                                sc.op("pool", lambda en: en.tensor_tensor(out=Bi[:, hl], in0=Bi[:, hl], in1=Bm[:, hl], op=ALU.mult), reads=[K("Bi", tt), K("Bm", tt)], writes=[K("Bi", tt)])
                                if t == 0:
                                    sc.op("dve", lambda en: en.tensor_tensor_scan(out=Bxc[:, hl], data0=Br[:, hl], data1=Bi[:, hl], initial=0.0, op0=ALU.mult, op1=ALU.add),
                                          reads=[K("Br", tt), K("Bi", tt), K("Bxc", tt)], writes=[K("Bxc", tt)])
                                else:
                                    sc.op("dve", lambda en: en.tensor_tensor_scan(out=Bxc[:, hl], data0=Br[:, hl], data1=Bi[:, hl], initial=carry[:, 0:1], op0=ALU.mult, op1=ALU.add),
                                          reads=[K("Br", tt), K("Bi", tt), K("Bxc", tt), K("carry")], writes=[K("Bxc", tt)])
                                sc.op("dve", lambda en: en.tensor_copy(out=carry[:, 0:1], in_=Bxc[:, tt * TT + TT - 1:tt * TT + TT]), reads=[K("Bxc", tt)], writes=[K("carry")])
                                sc.op("pool", lambda en: en.tensor_tensor(out=zT[:, c, tl], in0=Bgy[:, hl], in1=Bxc[:, hl], op=ALU.mult),
                                      reads=[K("Bgy", tt), K("Bxc", tt)], writes=[K("z", c, t)])
                    barrier()
                out_proj(l, b, odout_d, zT)

        def phase_m0(b):
            l = 0
            phase_norm(b, A1[l], modT[l][:, 0:8, :], False)
            UP = 30
            with ExitStack() as es0:
                def t0_(name, shape, dt=F32):
                    return es0.enter_context(SBT(name, list(shape), dt))
                u_pad = t0_("u_pad", [128, 4, UP + S], BF16)
                mask_bf = t0_("mask_bf", [128, 4, 512], BF16)
                om, ou, oo = CST_OFF["mask"][0], CST_OFF["ustrict"][0], CST_OFF["ones"][0]
                nmask_bf = t0_("nmask_bf", [128, 4, 512], BF16)
                ident_bf = t0_("ident_bf", [128, 128], BF16)
                onm, oid = CST_OFF["nmask"][0], CST_OFF["ident"][0]
                sc.dma("pool", "d_c3", [(mask_bf[:], cst_d[:, om:om + 2048].rearrange("p (d t) -> p d t", d=4)),
                                        (nmask_bf[:], cst_d[:, onm:onm + 2048].rearrange("p (d t) -> p d t", d=4)),
                                        (ident_bf[:], cst_d[:, oid:oid + 128])], writes=[K("mask")])
                sc.op("pool", lambda en: en.memset(u_pad[:, :, 0:UP], 0.0), writes=[K("upad")])
                esq = ExitStack()
                qT = esq.enter_context(SBT("qT", [128, 4, S], BF16))
                kT = esq.enter_context(SBT("kT", [128, 4, S], BF16))
                v_sb = esq.enter_context(SBT("v_sb", [128, 16, 512], BF16))
                with ExitStack() as esa:
                    def ta_(name, shape, dt=F32):
                        return esa.enter_context(SBT(name, list(shape), dt))
                    wb = [ta_(f"wb{i}", [128, NCH, 512], BF16) for i in range(2)]
                    sig = [ta_(f"sig{i}", [128, TT]) for i in range(2)]
                    qsq = [ta_(f"qsq{i}", [128, TT], BF16) for i in range(2)]
                    rq = [ta_(f"rq{i}", [128, TT]) for i in range(2)]

                    def load_g(g, slot):
                        sc.dma("pool", f"d_wb{slot}", [(wb[slot][:], evin_d[:, g * 512:(g + 1) * 512].rearrange("(k p) f -> p k f", p=128))], writes=[K("wb", slot)])
                    load_g(0, 0)
                    load_g(1, 1)
                    i_ = 0
                    for fc in range(4):
                        for t in range(NT):
                            tl = slice(t * TT, (t + 1) * TT)
                            p_ = i_ % 2
                            i_ += 1
                            for kc in range(NCH):
                                sc.op("pe", lambda en: en.matmul(ps[p_][:], lhsT=wb[0][:, kc, fc * 128:(fc + 1) * 128], rhs=hT[:, kc, tl], start=(kc == 0), stop=(kc == NCH - 1)),
                                      reads=[K("wb", 0), K("h", kc, t)], writes=[K("ps", p_)], inc=(kc == NCH - 1))
                            for kc in range(NCH):
                                sc.op("pe", lambda en: en.matmul(ps[2 + p_][:], lhsT=wb[1][:, kc, fc * 128:(fc + 1) * 128], rhs=hT[:, kc, tl], start=(kc == 0), stop=(kc == NCH - 1)),
                                      reads=[K("wb", 1), K("h", kc, t)], writes=[K("ps", 2 + p_)], inc=(kc == NCH - 1))
                            sc.op("act", lambda en: en.activation(out=sig[p_][:], in_=ps[2 + p_][:], func=AF.Sigmoid), reads=[K("ps", 2 + p_)], writes=[K("sig", p_)])
                            sc.op("dve", lambda en: en.tensor_tensor(out=u_pad[:, fc, UP + t * TT:UP + (t + 1) * TT], in0=ps[p_][:], in1=sig[p_][:], op=ALU.mult),
                                  reads=[K("ps", p_), K("sig", p_)], writes=[K("u", fc, t)])
                    for gi_, (g, dstT, gname) in enumerate([(2, qT, "qg"), (3, kT, "kg")]):
                        slot = gi_ % 2
                        load_g(g, slot)
                        for fc in range(4):
                            for t in range(NT):
                                tl = slice(t * TT, (t + 1) * TT)
                                p_ = i_ % 2
                                i_ += 1
                                for kc in range(NCH):
                                    sc.op("pe", lambda en: en.matmul(ps[4 + p_][:], lhsT=wb[slot][:, kc, fc * 128:(fc + 1) * 128], rhs=hT[:, kc, tl], start=(kc == 0), stop=(kc == NCH - 1)),
                                          reads=[K("wb", slot), K("h", kc, t)], writes=[K("ps", 4 + p_)], inc=(kc == NCH - 1))
                                sc.op("act", lambda en: en.activation(out=qsq[p_][:], in_=ps[4 + p_][:], func=AF.Square), reads=[K("ps", 4 + p_)], writes=[K("qsq", p_)])
                                sc.op("pe", lambda en: en.matmul(ps[6 + p_][:], lhsT=bdon_bf[:], rhs=qsq[p_][:], start=True, stop=True),
                                      reads=[K("bdon"), K("qsq", p_)], writes=[K("ps", 6 + p_)])
                                sc.op("act", lambda en: en.activation(out=rq[p_][:], in_=ps[6 + p_][:], func=AF.Ln, scale=1.0 / 64.0, bias=EPS), reads=[K("ps", 6 + p_)], writes=[K("rq", p_)])
                                sc.op("act", lambda en: en.activation(out=rq[p_][:], in_=rq[p_][:], func=AF.Exp, scale=-0.5), reads=[K("rq", p_)], writes=[K("rq", p_)])
                                sc.op("dve", lambda en: en.scalar_tensor_tensor(out=dstT[:, fc, tl], in0=ps[4 + p_][:], scalar=pvs(gname), op0=ALU.mult, in1=rq[p_][:], op1=ALU.mult),
                                      reads=[K("ps", 4 + p_), K("rq", p_)], writes=[K(gname, fc, t)])
                    load_g(4, 0)
                    for tc in range(16):
                        p_ = tc % 2
                        for kc in range(NCH):
                            sc.op("pe", lambda en: en.matmul(ps[p_][:], lhsT=hT[:, kc, tc * 128:(tc + 1) * 128], rhs=wb[0][:, kc, :], start=(kc == 0), stop=(kc == NCH - 1)),
                                  reads=[K("wb", 0), K("h", kc, tc // 4)], writes=[K("ps", p_)], inc=(kc == NCH - 1))
                        if tc % 2 == 0:
                            sc.op("act", lambda en: en.activation(out=v_sb[:, tc, :], in_=ps[p_][:], func=AF.Identity), reads=[K("ps", p_)], writes=[K("v", tc)])
                        else:
                            sc.op("dve", lambda en: en.tensor_copy(out=v_sb[:, tc, :], in_=ps[p_][:]), reads=[K("ps", p_)], writes=[K("v", tc)])
                    barrier()
                with ExitStack() as esb:
                    def tb_(name, shape, dt=F32):
                        return esb.enter_context(SBT(name, list(shape), dt))
                    NZ, NQ, NW = 3, 5, 4
                    spf = [tb_(f"spf{i}", [128, TT]) for i in range(NZ)]
                    spb = [tb_(f"spb{i}", [128, TT], BF16) for i in range(NZ)]
                    q1 = [tb_(f"q1_{i}", [128, TT]) for i in range(NQ)]
                    wtb = [tb_(f"wtb{i}", [128, TT], BF16) for i in range(NW)]
                    Rb = [tb_(f"Rb{i}", [128, TT], BF16) for i in range(2)]
                    units = []
                    for hp in range(4):
                        for qt in range(NT):
                            nk = 4 * (qt + 1)
                            for sc_ in range(nk - 1, -1, -1):
                                for hh in range(2):
                                    units.append((hp, qt, sc_, hh, nk))

                    def S1(ui):
                        hp, qt, sc_, hh, nk = units[ui]
                        zb = ui % NZ
                        hr = slice(hh * 64, (hh + 1) * 64)
                        sc.op("pe", lambda en: en.matmul(ps[zb][:], lhsT=kT[hr, hp, sc_ * 128:(sc_ + 1) * 128], rhs=qT[hr, hp, qt * TT:(qt + 1) * TT], start=True, stop=True),
                              reads=[K("kg", hp, sc_ // 4), K("qg", hp, qt)], writes=[K("ps", zb)])

                    def S2(ui):
                        zb = ui % NZ
                        sc.op("act", lambda en: en.activation(out=spf[zb][:], in_=ps[zb][:], func=AF.Exp, scale=0.125), reads=[K("ps", zb)], writes=[K("spf", zb)])
                        sc.op("act", lambda en: en.activation(out=spf[zb][:], in_=spf[zb][:], func=AF.Ln, bias=1.0), reads=[K("spf", zb)], writes=[K("spf", zb)])

                    def S3(ui):
                        hp, qt, sc_, hh, nk = units[ui]
                        zb = ui % NZ
                        qb = ui % NQ
                        d = sc_ - 4 * qt
                        sc.op("dve", lambda en: en.scalar_tensor_tensor(out=q1[qb][:], in0=ps[zb][:], scalar=0.125, op0=ALU.mult, in1=spf[zb][:], op1=ALU.subtract),
                              reads=[K("ps", zb), K("spf", zb)], writes=[K("q1", qb)])
                        if d >= 0:
                            sc.op("dve", lambda en: en.tensor_tensor(out=spb[zb][:], in0=spf[zb][:], in1=mask_bf[:, d, :], op=ALU.mult),
                                  reads=[K("spf", zb), K("mask")], writes=[K("spb", zb)])
                        else:
                            sc.op("dve", lambda en: en.tensor_copy(out=spb[zb][:], in_=spf[zb][:]), reads=[K("spf", zb)], writes=[K("spb", zb)])

                    def S4(ui):
                        hp, qt, sc_, hh, nk = units[ui]
                        zb = ui % NZ
                        cb = 3 + ui % 3
                        d = sc_ - 4 * qt
                        first = (sc_ == nk - 1)
                        nmm = 1 + (0 if first else 1) + (1 if d >= 0 else 0)
                        k_ = 1
                        sc.op("pe", lambda en: en.matmul(ps[cb][:], lhsT=ustr_bf[:], rhs=spb[zb][:], start=True, stop=(k_ == nmm)),
                              reads=[K("ustr"), K("spb", zb)], writes=[K("ps", cb)], inc=(k_ == nmm))
                        if not first:
                            k_ += 1
                            sc.op("pe", lambda en: en.matmul(ps[cb][:], lhsT=ones_bf[:], rhs=Rb[hh][:], start=False, stop=(k_ == nmm)),
                                  reads=[K("ones"), K("Rb", hh)], writes=[K("ps", cb)], inc=(k_ == nmm))
                        if d >= 0:
                            k_ += 1
                            sc.op("pe", lambda en: en.matmul(ps[cb][:], lhsT=ident_bf[:], rhs=nmask_bf[:, d, :], start=False, stop=(k_ == nmm)),
                                  reads=[K("mask")], writes=[K("ps", cb)], inc=(k_ == nmm))
                        if sc_ > 0:
                            if first:
                                sc.op("pool", lambda en: en.tensor_copy(out=Rb[hh][:], in_=spb[zb][:]), reads=[K("spb", zb)], writes=[K("Rb", hh)])
                            else:
                                sc.op("pool", lambda en: en.tensor_tensor(out=Rb[hh][:], in0=Rb[hh][:], in1=spb[zb][:], op=ALU.add),
                                      reads=[K("spb", zb), K("Rb", hh)], writes=[K("Rb", hh)])

                    def S5(ui):
                        qb = ui % NQ
                        cb = 3 + ui % 3
                        sc.op("dve", lambda en: en.tensor_tensor(out=q1[qb][:], in0=q1[qb][:], in1=ps[cb][:], op=ALU.subtract),
                              reads=[K("ps", cb), K("q1", qb)], writes=[K("q1", qb)])

                    def S6(ui):
                        qb = ui % NQ
                        wb_ = ui % NW
                        sc.op("act", lambda en: en.activation(out=wtb[wb_][:], in_=q1[qb][:], func=AF.Exp), reads=[K("q1", qb)], writes=[K("wtb", wb_)])

                    def S8(ui):
                        hp, qt, sc_, hh, nk = units[ui]
                        wb_ = ui % NW
                        pvb = 6 + ((hp * NT + qt) % 2)
                        h = 2 * hp + hh
                        sc.op("pe", lambda en: en.matmul(ps[pvb][hh * 64:(hh + 1) * 64, :], lhsT=v_sb[:, sc_, h * 64:(h + 1) * 64], rhs=wtb[wb_][:],
                                                         start=(sc_ == nk - 1), stop=(sc_ == 0)),
                              reads=[K("v", sc_), K("wtb", wb_)], writes=[K("pv", pvb, hh)], inc=True)
                        if sc_ == 0 and hh == 1:
                            sc.op("act", lambda en: en.activation(out=hT[:, 4 + hp, qt * TT:(qt + 1) * TT], in_=ps[pvb][:], func=AF.Identity),
                                  reads=[K("pv", pvb, 0), K("pv", pvb, 1)], writes=[K("h", 4 + hp, qt)])

                    stages = [S1, S2, S3, S4, S5, S6, S8]
                    nu = len(units)
                    for it in range(nu + len(stages) - 1):
                        for si_, fn in enumerate(stages):
                            ui = it - si_
                            if 0 <= ui < nu:
                                fn(ui)
                    barrier()
                esq.close()
                with ExitStack() as esc:
                    def tc_(name, shape, dt=F32):
                        return esc.enter_context(SBT(name, list(shape), dt))
                    HS = S // 2
                    y = tc_("cv_y", [128, 4, HS])
                    ysq = [tc_(f"ysq{i}", [128, TT], BF16) for i in range(2)]
                    ybf = [tc_(f"ybf{i}", [128, TT], BF16) for i in range(2)]
                    mean = tc_("cv_mean", [128, TT])
                    msq = tc_("cv_msq", [128, TT])
                    rs = tc_("cv_rs", [128, TT])
                    t1 = [tc_(f"cv_t1{i}", [128, TT]) for i in range(2)]
                    odw, _ = PV_OFF["dww"]
                    for hf in range(2):
                        for fc in range(4):
                            rk = [K("upad")] + [K("u", fc, t) for t in range(0, hf * 2 + 2)]
                            sc.op("dve", lambda en: en.tensor_scalar(out=y[:, fc, :], in0=u_pad[:, fc, hf * HS:hf * HS + HS], scalar1=pv[:, odw + fc * 31:odw + fc * 31 + 1],
                                                                     scalar2=pvs("dwb", fc), op0=ALU.mult, op1=ALU.add),
                                  reads=rk + [K("y", fc, 0), K("y", fc, 1)], writes=[K("y", fc, 0), K("y", fc, 1)])
                            for k in range(1, 31):
                                sc.op("dve", lambda en: en.scalar_tensor_tensor(out=y[:, fc, :], in0=u_pad[:, fc, hf * HS + k:hf * HS + k + HS],
                                                                                scalar=pv[:, odw + fc * 31 + k:odw + fc * 31 + k + 1], op0=ALU.mult, in1=y[:, fc, :], op1=ALU.add),
                                      reads=[K("y", fc, 0), K("y", fc, 1)], writes=[K("y", fc, 0), K("y", fc, 1)])
                        for tt in range(2):
                            t = hf * 2 + tt
                            tl = slice(t * TT, (t + 1) * TT)
                            hl = slice(tt * TT, (tt + 1) * TT)
                            for fc in range(4):
                                s_ = fc % 2
                                sc.op("act", lambda en: en.activation(out=ysq[s_][:], in_=y[:, fc, hl], func=AF.Square), reads=[K("y", fc, tt)], writes=[K("ysq", s_)])
                                sc.op("pool", lambda en: en.tensor_copy(out=ybf[s_][:], in_=y[:, fc, hl]), reads=[K("y", fc, tt)], writes=[K("ybf", s_)])
                                sc.op("pe", lambda en: en.matmul(ps[0][:], lhsT=ones_bf[:], rhs=ybf[s_][:], start=(fc == 0), stop=(fc == 3)),
                                      reads=[K("ones"), K("ybf", s_)], writes=[K("ps", 0)], inc=True)
                                sc.op("pe", lambda en: en.matmul(ps[1][:], lhsT=ones_bf[:], rhs=ysq[s_][:], start=(fc == 0), stop=(fc == 3)),
                                      reads=[K("ones"), K("ysq", s_)], writes=[K("ps", 1)], inc=True)
                            sc.op("dve", lambda en: en.tensor_scalar(out=mean[:], in0=ps[0][:], scalar1=1.0 / 512.0, scalar2=None, op0=ALU.mult), reads=[K("ps", 0)], writes=[K("mean")])
                            sc.op("pool", lambda en: en.tensor_tensor(out=msq[:], in0=mean[:], in1=mean[:], op=ALU.mult), reads=[K("mean")], writes=[K("msq")])
                            sc.op("dve", lambda en: en.scalar_tensor_tensor(out=rs[:], in0=ps[1][:], scalar=1.0 / 512.0, op0=ALU.mult, in1=msq[:], op1=ALU.subtract),
                                  reads=[K("ps", 1), K("msq")], writes=[K("rs")])
                            sc.op("act", lambda en: en.activation(out=rs[:], in_=rs[:], func=AF.Ln, bias=EPS), reads=[K("rs")], writes=[K("rs")])
                            sc.op("act", lambda en: en.activation(out=rs[:], in_=rs[:], func=AF.Exp, scale=-0.5), reads=[K("rs")], writes=[K("rs")])
                            for fc in range(4):
                                s_ = fc % 2
                                sc.op("pool", lambda en: en.tensor_tensor(out=t1[s_][:], in0=y[:, fc, hl], in1=mean[:], op=ALU.subtract), reads=[K("y", fc, tt), K("mean")], writes=[K("t1", s_)])
                                sc.op("pool", lambda en: en.tensor_tensor(out=t1[s_][:], in0=t1[s_][:], in1=rs[:], op=ALU.mult), reads=[K("t1", s_), K("rs")], writes=[K("t1", s_)])
                                sc.op("act", lambda en: en.activation(out=hT[:, fc, tl], in_=t1[s_][:], func=AF.Silu, scale=pvs("lng", fc), bias=pvs("lnb", fc)),
                                      reads=[K("t1", s_)], writes=[K("h", fc, t)])
                    barrier()
            out_proj(l, b, evout_d, hT)

        for si in range(nseq):
            b = si
            for c in range(NCH):
                sc.dma("sp", f"d_x{c}", [(xT[:, c, :], xT_d[si, c * 128:(c + 1) * 128, :])], writes=[K("x", c, t) for t in range(NT)])
            for ph in phases:
                l = int(ph[1])
                if ph[0] == "e":
                    phase_moe(l, b)
                elif ph == "m0":
                    phase_m0(b)
                elif ph == "m1":
                    phase_m1(b)
            for c in range(NCH):
                sc.dma("sp", f"d_o{c}", [(outT_d[si, c * 128:(c + 1) * 128, :], xT[:, c, :])], reads=[K("x", c, t) for t in range(NT)])
        for e_ in ("sp",):
            sc.wait_all(e_)
        print(f"[build] instructions={sc.n_inst} waits={sc.n_wait} counts={ {k: v for k, v in sc.count.items() if k in ('pe','act','dve','pool')} }")
    return nc


def kernel(**inputs):
    inp = {k: np.asarray(v) for k, v in inputs.items()}
    x = inp["x"]
    B = x.shape[0]
    nseq = B // N_CORES
    nc = build_program(nseq=nseq)
    cst = make_consts()
    shared = {
        "cst": cst,
        "mod_w": np.ascontiguousarray(inp["mod_w"], np.float32),
        "ev_in_w": np.ascontiguousarray(inp["ev_in_w"][0], np.float32),
        "ev_out_w": np.ascontiguousarray(inp["ev_out_w"][0], np.float32),
        "od_in_w": np.ascontiguousarray(inp["od_in_w"][0], np.float32),
        "od_out_w": np.ascontiguousarray(inp["od_out_w"][0], np.float32),
        "od_rg_w": np.ascontiguousarray(inp["od_rg_w"][0], np.float32),
        "od_ig_w": np.ascontiguousarray(inp["od_ig_w"][0], np.float32),
        "router_w": np.ascontiguousarray(inp["router_w"], np.float32),
        "ex_w1": np.ascontiguousarray(inp["ex_w1"], np.float32),
        "ex_w3": np.ascontiguousarray(inp["ex_w3"], np.float32),
        "ex_w2": np.ascontiguousarray(inp["ex_w2"], np.float32),
    }
    in_maps = []
    for core in range(N_CORES):
        b0 = core * nseq
        m = dict(shared)
        m["xT"] = np.ascontiguousarray(np.transpose(x[b0:b0 + nseq], (0, 2, 1)))
        m["pvec"] = make_pvec(inp, b0, nseq)
        in_maps.append(m)
    res = run_bass_kernel_spmd(nc, in_maps, core_ids=list(range(N_CORES)))
    out = np.empty_like(x)
    for core in range(N_CORES):
        b0 = core * nseq
        out[b0:b0 + nseq] = np.transpose(res.results[core]["outT"], (0, 2, 1))
    return out
```

```python
import numpy as np
from contextlib import ExitStack
import concourse.bass as bass
import concourse.mybir as mybir
from concourse.bass_utils import run_bass_kernel_spmd

F32 = mybir.dt.float32
BF16 = mybir.dt.bfloat16
AF = mybir.ActivationFunctionType
ALU = mybir.AluOpType
AX = mybir.AxisListType

D = 1024
S = 2048
NCH = 8
TT = 512
NT = 4
NE = 16
EPS = 1e-6
N_CORES = 8
SEQ_PER_CORE = 4


def _fm(v):
    v = np.asarray(v, np.float32)
    return np.ascontiguousarray(v.reshape(-1, 128).T)


PV_FIELDS = [("mixg", 16), ("ffng", 16), ("modb", 96), ("dwb", 4), ("lng", 4), ("lnb", 4), ("dww", 124),
             ("ocw", 32), ("ocb", 8), ("rgb", 8), ("igb", 8), ("lam", 8), ("qg", 1), ("kg", 1), ("rb", 16), ("cT", 32)]
PV_OFF = {}
_o = 0
for _n, _w in PV_FIELDS:
    PV_OFF[_n] = (_o, _w)
    _o += _w
NV = _o

CST_FIELDS = [("ident", 128), ("ustrict", 128), ("bdones", 128), ("ones", 128), ("mask", 2048), ("sel", 2048), ("nmask", 2048)]
CST_OFF = {}
_o = 0
for _n, _w in CST_FIELDS:
    CST_OFF[_n] = (_o, _w)
    _o += _w
NCST = _o


def make_consts():
    c = np.zeros((128, NCST), np.float32)
    j = np.arange(128)[:, None]
    s = np.arange(128)[None, :]
    c[:, CST_OFF["ident"][0]:][:, :128] = (j == s)
    c[:, CST_OFF["ustrict"][0]:][:, :128] = (j > s)
    c[:, CST_OFF["bdones"][0]:][:, :128] = ((j // 64) == (s // 64))
    c[:, CST_OFF["ones"][0]:][:, :128] = 1.0
    t = np.arange(512)[None, :]
    for d in range(4):
        o = CST_OFF["mask"][0] + d * 512
        c[:, o:o + 512] = ((128 * d + j) < t)
    o = CST_OFF["sel"][0]
    for e in range(16):
        c[e, o + e * 128:o + (e + 1) * 128] = 1.0
    for d in range(4):
        o = CST_OFF["nmask"][0] + d * 512
        c[:, o:o + 512] = np.where((128 * d + j) < t, 0.0, 30000.0)
    return c


def make_pvec(inp, b0, nseq):
    pv = np.zeros((128, NV), np.float32)

    def put(name, arr):
        o, w = PV_OFF[name]
        assert arr.shape == (128, w), (name, arr.shape, w)
        pv[:, o:o + w] = arr

    put("mixg", np.concatenate([_fm(inp["mix_norm_g"][l]) for l in range(2)], axis=1))
    put("ffng", np.concatenate([_fm(inp["ffn_norm_g"][l]) for l in range(2)], axis=1))
    put("modb", np.concatenate([_fm(inp["mod_b"][l]) for l in range(2)], axis=1))
    put("dwb", _fm(inp["ev_dw_b"][0]))
    put("lng", _fm(inp["ev_ln_g"][0]))
    put("lnb", _fm(inp["ev_ln_b"][0]))
    dw = np.asarray(inp["ev_dw_w"][0], np.float32)
    put("dww", np.ascontiguousarray(dw.reshape(31, 4, 128).transpose(2, 1, 0).reshape(128, 124)))
    cw = np.asarray(inp["od_conv_w"][0], np.float32)
    put("ocw", np.ascontiguousarray(cw.reshape(4, 8, 128).transpose(2, 1, 0).reshape(128, 32)))
    put("ocb", _fm(inp["od_conv_b"][0]))
    put("rgb", _fm(inp["od_rg_b"][0]))
    put("igb", _fm(inp["od_ig_b"][0]))
    put("lam", _fm(inp["od_lam"][0]))
    put("qg", np.tile(np.asarray(inp["ev_q_g"][0], np.float32), 2)[:, None])
    put("kg", np.tile(np.asarray(inp["ev_k_g"][0], np.float32), 2)[:, None])
    put("rb", np.tile(np.asarray(inp["router_b"], np.float32)[None, :], (128, 1)))
    cc = np.zeros((4, 1024), np.float32)
    cc[:nseq] = np.asarray(inp["c"][b0:b0 + nseq], np.float32)
    put("cT", np.ascontiguousarray(cc.reshape(4, 8, 128).transpose(2, 1, 0).reshape(128, 32)))
    return pv


class Sched:
    def __init__(self, nc, es):
        self.nc = nc
        self.es = es
        self.engines = {"pe": nc.tensor, "act": nc.scalar, "dve": nc.vector, "pool": nc.gpsimd, "sp": nc.sync}
        self.sems = {}
        self.count = {}
        self.seen = {e: {} for e in self.engines}
        self.lastw = {}
        self.readers = {}
        self.n_inst = 0
        self.n_wait = 0
        for e in ("pe", "act", "dve", "pool"):
            self._sem(e)

    def _sem(self, name):
        if name not in self.sems:
            self.sems[name] = self.es.enter_context(self.nc.semaphore(name))
            self.count[name] = 0
        return self.sems[name]

    def _wait(self, eng, sem, val):
        if val <= 0 or self.seen[eng].get(sem, 0) >= val:
            return
        self.engines[eng].wait_ge(self.sems[sem], val)
        self.seen[eng][sem] = val
        self.n_wait += 1

    def _deps(self, eng, reads, writes):
        need = {}
        for k in reads:
            ev = self.lastw.get(k)
            if ev is not None and ev[1] > need.get(ev[0], 0):
                need[ev[0]] = ev[1]
        for k in writes:
            ev = self.lastw.get(k)
            if ev is not None and ev[1] > need.get(ev[0], 0):
                need[ev[0]] = ev[1]
            rd = self.readers.get(k)
            if rd:
                for s_, v_ in rd.items():
                    if v_ > need.get(s_, 0):
                        need[s_] = v_
        for s_, v_ in need.items():
            if s_ == eng:
                if v_ > self.count[eng]:
                    continue
                if eng == "pe":
                    continue
            self._wait(eng, s_, v_)

    def _record(self, ev, reads, writes):
        for k in reads:
            rd = self.readers.setdefault(k, {})
            if ev[1] > rd.get(ev[0], 0):
                rd[ev[0]] = ev[1]
        for k in writes:
            self.lastw[k] = ev
            self.readers[k] = {}

    def op(self, eng, fn, reads=(), writes=(), inc=True):
        self._deps(eng, reads, writes)
        inst = fn(self.engines[eng])
        self.n_inst += 1
        if inc:
            inst.then_inc(self.sems[eng], 1)
            self.count[eng] += 1
            ev = (eng, self.count[eng])
        else:
            ev = (eng, self.count[eng] + 1)
        self._record(ev, reads, writes)
        return inst

    def dma(self, q, sem, pairs, reads=(), writes=()):
        self._sem(sem)
        self._deps(q, reads, writes)
        self._wait(q, sem, self.count[sem])
        for out, in_ in pairs:
            self.engines[q].dma_start(out=out, in_=in_).then_inc(self.sems[sem], 16)
            self.count[sem] += 16
            self.n_inst += 1
        ev = (sem, self.count[sem])
        self._record(ev, reads, writes)

    def wait_all(self, eng):
        for s_, v_ in self.count.items():
            self._wait(eng, s_, v_)


def build_program(nseq=SEQ_PER_CORE, phases=("m0", "e0", "m1", "e1")):
    nc = bass.Bass("TRN2", target_bir_lowering=False)
    dr = {}

    def din(name, shape):
        dr[name] = nc.dram_tensor(name, list(shape), F32, kind="ExternalInput").ap()
        return dr[name]

    xT_d = din("xT", [nseq, D, S])
    pvec_d = din("pvec", [128, NV])
    cst_d = din("cst", [128, NCST])
    modw_d = din("mod_w", [2, D, 6 * D])
    evin_d = din("ev_in_w", [D, 2560])
    evout_d = din("ev_out_w", [D, D])
    odin_d = din("od_in_w", [D, 2048])
    odout_d = din("od_out_w", [D, D])
    rgw_d = din("od_rg_w", [8, 128, 128])
    igw_d = din("od_ig_w", [8, 128, 128])
    rw_d = din("router_w", [D, NE])
    w1_d = din("ex_w1", [2, NE, D, 512])
    w3_d = din("ex_w3", [2, NE, D, 512])
    w2_d = din("ex_w2", [2, NE, 512, D])
    outT_d = nc.dram_tensor("outT", [nseq, D, S], F32, kind="ExternalOutput").ap()

    _uid = [0]

    def SBT(name, shape, dt):
        _uid[0] += 1
        return nc.sbuf_tensor(f"{name}_{_uid[0]}", shape, dt)

    with ExitStack() as es:
        sc = Sched(nc, es)

        def sb(name, shape, dt):
            return es.enter_context(SBT(name, list(shape), dt))

        xT = sb("xT_s", [128, NCH, S], F32)
        hT = sb("hT_s", [128, NCH, S], BF16)
        pv = sb("pv_s", [128, NV], F32)
        ident = sb("ident", [128, 128], F32)
        ones_bf = sb("ones_bf", [128, 128], BF16)
        ustr_bf = sb("ustr_bf", [128, 128], BF16)
        bdon_bf = sb("bdon_bf", [128, 128], BF16)
        sel = sb("sel", [16, NE, 128], F32)
        rw = sb("rw", [128, NCH, NE], F32)
        modT = [sb(f"modT{l}", [128, 48, 4], F32) for l in range(2)]
        A1 = [sb(f"A1_{l}", [128, NCH, 4], F32) for l in range(2)]
        A2 = [sb(f"A2_{l}", [128, NCH, 4], F32) for l in range(2)]
        cl8 = sb("cl8", [128, NCH], F32)
        ps = [es.enter_context(nc.psum_tensor(f"ps{i}", [128, 512], F32)) for i in range(8)]

        def pvs(name, i=0, n=None):
            o, w = PV_OFF[name]
            n = 1 if n is None else n
            return pv[:, o + i:o + i + n]

        def K(*a):
            return a

        o_, _ = CST_OFF["ident"]
        sc.dma("sp", "d_c0", [(pv[:], pvec_d[:, :]), (ident[:], cst_d[:, o_:o_ + 128])], writes=[K("pv"), K("ident")])
        o_, _ = CST_OFF["sel"]
        sc.dma("sp", "d_c1", [(sel[:], cst_d[0:16, o_:o_ + 2048].rearrange("p (e m) -> p e m", e=NE)),
                              (rw[:], rw_d.rearrange("(c p) e -> p c e", p=128))], writes=[K("sel"), K("rw")])
        oo, ou, ob, om = CST_OFF["ones"][0], CST_OFF["ustrict"][0], CST_OFF["bdones"][0], CST_OFF["mask"][0]
        sc.dma("pool", "d_c2", [(ones_bf[:], cst_d[:, oo:oo + 128]), (ustr_bf[:], cst_d[:, ou:ou + 128]),
                                (bdon_bf[:], cst_d[:, ob:ob + 128])],
               writes=[K("ones"), K("ustr"), K("bdon")])

        with ExitStack() as es2:
            cond = es2.enter_context(SBT("cond", [128, NCH, 4], F32))
            mw = [es2.enter_context(SBT(f"mw{i}", [128, NCH, 512], F32)) for i in range(2)]
            o_, _ = PV_OFF["cT"]
            sc.op("act", lambda e: e.activation(out=cond[:].rearrange("p c b -> p (c b)"), in_=pv[:, o_:o_ + 32], func=AF.Silu),
                  reads=[K("pv")], writes=[K("cond")])
            gi = 0
            for l in range(2):
                for g in range(12):
                    slot = gi % 2
                    gi += 1
                    sc.dma("sp", f"d_mw{slot}", [(mw[slot][:], modw_d[l, :, g * 512:(g + 1) * 512].rearrange("(c p) f -> p c f", p=128))],
                           writes=[K("mw", slot)])
                    for fc in range(4):
                        f = g * 4 + fc
                        for kc in range(NCH):
                            sc.op("pe", lambda e: e.matmul(ps[0][:, f * 4:(f + 1) * 4], lhsT=mw[slot][:, kc, fc * 128:(fc + 1) * 128],
                                                           rhs=cond[:, kc, :], start=(kc == 0), stop=(kc == NCH - 1)),
                                  reads=[K("mw", slot), K("cond")], writes=[K("ps", 0)], inc=(kc == NCH - 1))
                o_, _ = PV_OFF["modb"]
                sc.op("dve", lambda e: e.tensor_tensor(out=modT[l][:], in0=ps[0][:, 0:192].rearrange("p (f b) -> p f b", b=4),
                                                       in1=pv[:, o_ + l * 48:o_ + (l + 1) * 48].unsqueeze(2).broadcast_to([128, 48, 4]),
                                                       op=ALU.add),
                      reads=[K("ps", 0), K("pv")], writes=[K("modT", l)])
                og, _ = PV_OFF["mixg"]
                sc.op("dve", lambda e: e.scalar_tensor_tensor(out=A1[l][:], in0=modT[l][:, 8:16, :], scalar=1.0, op0=ALU.add,
                                                              in1=pv[:, og + l * 8:og + (l + 1) * 8].unsqueeze(2).broadcast_to([128, 8, 4]),
                                                              op1=ALU.mult),
                      reads=[K("modT", l), K("pv")], writes=[K("A1", l)])
                og, _ = PV_OFF["ffng"]
                sc.op("dve", lambda e: e.scalar_tensor_tensor(out=A2[l][:], in0=modT[l][:, 32:40, :], scalar=1.0, op0=ALU.add,
                                                              in1=pv[:, og + l * 8:og + (l + 1) * 8].unsqueeze(2).broadcast_to([128, 8, 4]),
                                                              op1=ALU.mult),
                      reads=[K("modT", l), K("pv")], writes=[K("A2", l)])
            ol, _ = PV_OFF["lam"]
            sc.op("act", lambda e: e.activation(out=cl8[:], in_=pv[:, ol:ol + 8], func=AF.Exp, scale=-1.0), reads=[K("pv")], writes=[K("cl8")])
            sc.op("act", lambda e: e.activation(out=cl8[:], in_=cl8[:], func=AF.Ln, bias=1.0), reads=[K("cl8")], writes=[K("cl8")])
            sc.op("dve", lambda e: e.tensor_scalar(out=cl8[:], in0=cl8[:], scalar1=-8.0, scalar2=None, op0=ALU.mult),
                  reads=[K("cl8")], writes=[K("cl8")])
            for e_ in ("pe", "act", "dve", "pool", "sp"):
                sc.wait_all(e_)

        xkeys = [K("x", c, t) for c in range(NCH) for t in range(NT)]
        hkeys = [K("h", c, t) for c in range(NCH) for t in range(NT)]

        def phase_norm(b, Aap, Sap, router, lgT=None):
            with ExitStack() as esn:
                rstd = esn.enter_context(SBT("rstd", [128, NT, TT], F32))
                sqb = [esn.enter_context(SBT(f"sqb{i}", [128, TT], BF16)) for i in range(3)]
                if router:
                    tbuf = [esn.enter_context(SBT(f"tbuf{i}", [128, NCH, TT], F32)) for i in range(2)]
                    srw = esn.enter_context(SBT("srw", [16, 1], F32))
                else:
                    tmp = [esn.enter_context(SBT(f"ntmp{i}", [128, TT], F32)) for i in range(3)]
                i2 = 0
                for t in range(NT):
                    tl = slice(t * TT, (t + 1) * TT)
                    for c in range(NCH):
                        s_ = i2 % 3
                        i2 += 1
                        if c % 2 == 0:
                            sc.op("act", lambda e: e.activation(out=sqb[s_][:], in_=xT[:, c, tl], func=AF.Square),
                                  reads=[K("x", c, t)], writes=[K("sqb", s_)])
                        else:
                            sc.op("dve", lambda e: e.tensor_tensor(out=sqb[s_][:], in0=xT[:, c, tl], in1=xT[:, c, tl], op=ALU.mult),
                                  reads=[K("x", c, t)], writes=[K("sqb", s_)])
                        sc.op("pe", lambda e: e.matmul(ps[t][:], lhsT=ones_bf[:], rhs=sqb[s_][:], start=(c == 0), stop=(c == NCH - 1)),
                              reads=[K("ones"), K("sqb", s_)], writes=[K("ps", t)], inc=True)
                if router:
                    for c in range(NCH):
                        sc.op("pe", lambda e: e.matmul(ps[5][0:16, 0:1], lhsT=rw[:, c, :], rhs=Sap[:, c, b:b + 1], start=(c == 0), stop=(c == NCH - 1)),
                              reads=[K("rw")], writes=[K("ps", 5)], inc=(c == NCH - 1))
                    sc.op("dve", lambda e: e.tensor_copy(out=srw[:], in_=ps[5][0:16, 0:1]), reads=[K("ps", 5)], writes=[K("srw")])
                for t in range(NT):
                    sc.op("act", lambda e: e.activation(out=rstd[:, t, :], in_=ps[t][:], func=AF.Ln, scale=1.0 / D, bias=EPS),
                          reads=[K("ps", t)], writes=[K("rstd", t)])
                for t in range(NT):
                    sc.op("act", lambda e: e.activation(out=rstd[:, t, :], in_=rstd[:, t, :], func=AF.Exp, scale=-0.5),
                          reads=[K("rstd", t)], writes=[K("rstd", t)])
                i2 = 0
                for t in range(NT):
                    tl = slice(t * TT, (t + 1) * TT)
                    for c in range(NCH):
                        if router:
                            dst, dkey = tbuf[t % 2][:, c, :], K("tbuf", t % 2, c)
                        else:
                            s_ = i2 % 3
                            i2 += 1
                            dst, dkey = tmp[s_][:], K("ntmp", s_)
                        sc.op("dve", lambda e: e.scalar_tensor_tensor(out=dst, in0=xT[:, c, tl], scalar=Aap[:, c, b:b + 1], op0=ALU.mult, in1=rstd[:, t, :], op1=ALU.mult),
                              reads=[K("x", c, t), K("rstd", t)], writes=[dkey])
                        sc.op("act", lambda e: e.activation(out=hT[:, c, tl], in_=dst, func=AF.Identity, bias=Sap[:, c, b:b + 1]),
                              reads=[dkey], writes=[K("h", c, t)])
                    if router:
                        for c in range(NCH):
                            sc.op("pe", lambda e: e.matmul(ps[4][0:16, :], lhsT=rw[:, c, :], rhs=tbuf[t % 2][:, c, :], start=(c == 0), stop=(c == NCH - 1)),
                                  reads=[K("rw"), K("tbuf", t % 2, c)], writes=[K("ps", 4)], inc=(c == NCH - 1))
                        sc.op("act", lambda e: e.activation(out=lgT[:, tl], in_=ps[4][0:16, :], func=AF.Identity, bias=srw[:, 0:1]),
                              reads=[K("ps", 4), K("srw")], writes=[K("lgT", t)])
                for e_ in ("pe", "act", "dve", "pool", "sp"):
                    sc.wait_all(e_)

        def phase_route(lgT, gT):
            with ExitStack() as esr:
                def t_(name, shape):
                    return esr.enter_context(SBT(name, list(shape), F32))
                scr = t_("r_sc", [128, 256])
                bi = t_("r_bi", [128, 256])
                p6 = t_("r_p6", [128, 64, 6])
                gs = t_("r_gs", [128, 64])
                gmax = t_("r_gmax", [128, 16])
                goh = t_("r_goh", [128, 64])
                m1 = t_("r_m1", [128, 64])
                e1 = t_("r_e1", [128, 256])
                b2 = t_("r_b2", [128, 256])
                m2 = t_("r_m2", [128, 64])
                sl = t_("r_sl", [128, 256])
                den = t_("r_den", [128, 16])
                gates = t_("r_gates", [128, 256])
                for tc in range(16):
                    sc.op("pe", lambda e: e.transpose(out=ps[5][:, tc * 16:(tc + 1) * 16], in_=lgT[:, tc * 128:(tc + 1) * 128], identity=ident[0:16, 0:16]),
                          reads=[K("lgT", tc // 4), K("ident")], writes=[K("ps", 5)], inc=(tc == 15))
                sc.op("act", lambda e: e.activation(out=scr[:], in_=ps[5][:, 0:256], func=AF.Sigmoid), reads=[K("ps", 5)], writes=[K("r_sc")])
                o_, _ = PV_OFF["rb"]
                V = sc.op
                V("dve", lambda e: e.tensor_tensor(out=bi[:].rearrange("p (t e) -> p t e", e=16), in0=scr[:].rearrange("p (t e) -> p t e", e=16),
                                                   in1=pv[:, o_:o_ + 16].unsqueeze(1).broadcast_to([128, 16, 16]), op=ALU.add),
                  reads=[K("r_sc"), K("pv")], writes=[K("r_bi")])
                bi4 = bi[:].rearrange("p (g k) -> p g k", k=4)
                V("dve", lambda e: e.tensor_tensor(out=p6[:, :, 0:3], in0=bi4[:, :, 0:3], in1=bi4[:, :, 1:4], op=ALU.add), reads=[K("r_bi")], writes=[K("r_p6a")])
                V("dve", lambda e: e.tensor_tensor(out=p6[:, :, 3:5], in0=bi4[:, :, 0:2], in1=bi4[:, :, 2:4], op=ALU.add), reads=[K("r_bi")], writes=[K("r_p6b")])
                V("dve", lambda e: e.tensor_tensor(out=p6[:, :, 5:6], in0=bi4[:, :, 0:1], in1=bi4[:, :, 3:4], op=ALU.add), reads=[K("r_bi")], writes=[K("r_p6c")])
                V("dve", lambda e: e.tensor_reduce(out=gs[:], in_=p6[:], axis=AX.X, op=ALU.max), reads=[K("r_p6a"), K("r_p6b"), K("r_p6c")], writes=[K("r_gs")])
                V("dve", lambda e: e.tensor_reduce(out=gmax[:], in_=gs[:].rearrange("p (t g) -> p t g", g=4), axis=AX.X, op=ALU.max), reads=[K("r_gs")], writes=[K("r_gmax")])
                V("dve", lambda e: e.tensor_tensor(out=goh[:].rearrange("p (t g) -> p t g", g=4), in0=gs[:].rearrange("p (t g) -> p t g", g=4),
                                                   in1=gmax[:].unsqueeze(2).broadcast_to([128, 16, 4]), op=ALU.is_equal), reads=[K("r_gs"), K("r_gmax")], writes=[K("r_goh")])
                V("dve", lambda e: e.tensor_reduce(out=m1[:], in_=bi4, axis=AX.X, op=ALU.max), reads=[K("r_bi")], writes=[K("r_m1")])
                V("dve", lambda e: e.tensor_tensor(out=e1[:].rearrange("p (g k) -> p g k", k=4), in0=bi4, in1=m1[:].unsqueeze(2).broadcast_to([128, 64, 4]), op=ALU.is_equal),
                  reads=[K("r_bi"), K("r_m1")], writes=[K("r_e1")])
                V("dve", lambda e: e.scalar_tensor_tensor(out=b2[:], in0=e1[:], scalar=-1.0e9, op0=ALU.mult, in1=bi[:], op1=ALU.add), reads=[K("r_e1"), K("r_bi")], writes=[K("r_b2")])
                V("dve", lambda e: e.tensor_reduce(out=m2[:], in_=b2[:].rearrange("p (g k) -> p g k", k=4), axis=AX.X, op=ALU.max), reads=[K("r_b2")], writes=[K("r_m2")])
                V("dve", lambda e: e.tensor_tensor(out=sl[:].rearrange("p (g k) -> p g k", k=4), in0=bi4, in1=m2[:].unsqueeze(2).broadcast_to([128, 64, 4]), op=ALU.is_ge),
                  reads=[K("r_bi"), K("r_m2")], writes=[K("r_sl")])
                V("dve", lambda e: e.tensor_tensor(out=sl[:].rearrange("p (g k) -> p g k", k=4), in0=sl[:].rearrange("p (g k) -> p g k", k=4),
                                                   in1=goh[:].unsqueeze(2).broadcast_to([128, 64, 4]), op=ALU.mult), reads=[K("r_sl"), K("r_goh")], writes=[K("r_sl")])
                V("dve", lambda e: e.tensor_tensor(out=sl[:], in0=sl[:], in1=scr[:], op=ALU.mult), reads=[K("r_sl"), K("r_sc")], writes=[K("r_sl")])
                V("dve", lambda e: e.tensor_reduce(out=den[:], in_=sl[:].rearrange("p (t e) -> p t e", e=16), axis=AX.X, op=ALU.add), reads=[K("r_sl")], writes=[K("r_den")])
                V("dve", lambda e: e.reciprocal(out=den[:], in_=den[:]), reads=[K("r_den")], writes=[K("r_den")])
                V("dve", lambda e: e.tensor_tensor(out=gates[:].rearrange("p (t e) -> p t e", e=16), in0=sl[:].rearrange("p (t e) -> p t e", e=16),
                                                   in1=den[:].unsqueeze(2).broadcast_to([128, 16, 16]), op=ALU.mult), reads=[K("r_sl"), K("r_den")], writes=[K("r_gates")])
                for tc in range(16):
                    bk = tc // 4
                    sc.op("pe", lambda e: e.transpose(out=ps[bk][0:16, (tc % 4) * 128:(tc % 4 + 1) * 128], in_=gates[:, tc * 16:(tc + 1) * 16], identity=ident[:]),
                          reads=[K("r_gates"), K("ident")], writes=[K("ps", bk)], inc=(tc % 4 == 3))
                for bk in range(4):
                    sc.op("act", lambda e: e.activation(out=gT[:, bk * TT:(bk + 1) * TT], in_=ps[bk][0:16, :], func=AF.Identity),
                          reads=[K("ps", bk)], writes=[K("gT", bk)])
                for e_ in ("pe", "act", "dve", "pool", "sp"):
                    sc.wait_all(e_)

        def phase_moe(l, b):
            with ExitStack() as esm:
                lgT = esm.enter_context(SBT("lgT", [16, S], F32))
                gT = esm.enter_context(SBT("gT", [16, S], F32))
                phase_norm(b, A2[l], modT[l][:, 24:32, :], True, lgT)
                phase_route(lgT, gT)
                w1s = [esm.enter_context(SBT(f"w1s{i}", [128, NCH, 512], BF16)) for i in range(2)]
                w3s = [esm.enter_context(SBT(f"w3s{i}", [128, NCH, 512], BF16)) for i in range(2)]
                w2s = [esm.enter_context(SBT(f"w2s{i}", [128, 4, D], BF16)) for i in range(2)]
                gbc = [esm.enter_context(SBT(f"gbc{i}", [128, TT], F32)) for i in range(2)]
                s1 = [esm.enter_context(SBT(f"s1_{i}", [128, TT], F32)) for i in range(2)]
                sg = [esm.enter_context(SBT(f"sg_{i}", [128, TT], F32)) for i in range(2)]
                actT = [esm.enter_context(SBT(f"actT{i}", [128, 4, TT], BF16)) for i in range(2)]

                def load_w(e):
                    s_ = e % 2
                    sc.dma("pool", f"d_w{s_}",
                           [(w1s[s_][:], w1_d[l, e].rearrange("(c p) f -> p c f", p=128)),
                            (w3s[s_][:], w3_d[l, e].rearrange("(c p) f -> p c f", p=128)),
                            (w2s[s_][:], w2_d[l, e].rearrange("(c p) f -> p c f", p=128))],
                           writes=[K("wexp", s_)])

                load_w(0)
                it = 0
                fi = 0
                for e in range(NE):
                    ws = e % 2
                    if e + 1 < NE:
                        load_w(e + 1)
                    for t in range(NT):
                        tl = slice(t * TT, (t + 1) * TT)
                        a_ = it % 2
                        it += 1
                        sc.op("pe", lambda en: en.matmul(ps[6][:], lhsT=sel[:, e, :], rhs=gT[:, tl], start=True, stop=True),
                              reads=[K("sel"), K("gT", t)], writes=[K("ps", 6)])
                        sc.op("act", lambda en: en.activation(out=gbc[a_][:], in_=ps[6][:], func=AF.Identity), reads=[K("ps", 6)], writes=[K("gbc", a_)])
                        for fc in range(4):
                            f_ = fi % 2
                            fi += 1
                            for kc in range(NCH):
                                sc.op("pe", lambda en: en.matmul(ps[0 + f_][:], lhsT=w1s[ws][:, kc, fc * 128:(fc + 1) * 128], rhs=hT[:, kc, tl],
                                                                 start=(kc == 0), stop=(kc == NCH - 1)),
                                      reads=[K("wexp", ws), K("h", kc, t)], writes=[K("ps", 0 + f_)], inc=(kc == NCH - 1))
                            for kc in range(NCH):
                                sc.op("pe", lambda en: en.matmul(ps[2 + f_][:], lhsT=w3s[ws][:, kc, fc * 128:(fc + 1) * 128], rhs=hT[:, kc, tl],
                                                                 start=(kc == 0), stop=(kc == NCH - 1)),
                                      reads=[K("wexp", ws), K("h", kc, t)], writes=[K("ps", 2 + f_)], inc=(kc == NCH - 1))
                            sc.op("act", lambda en: en.activation(out=s1[f_][:], in_=ps[0 + f_][:], func=AF.Silu), reads=[K("ps", 0 + f_)], writes=[K("s1", f_)])
                            sc.op("pool", lambda en: en.tensor_tensor(out=sg[f_][:], in0=s1[f_][:], in1=gbc[a_][:], op=ALU.mult),
                                  reads=[K("s1", f_), K("gbc", a_)], writes=[K("sg", f_)])
                            sc.op("dve", lambda en: en.tensor_tensor(out=actT[a_][:, fc, :], in0=ps[2 + f_][:], in1=sg[f_][:], op=ALU.mult),
                                  reads=[K("ps", 2 + f_), K("sg", f_)], writes=[K("actT", a_, fc)])
                        for dc in range(NCH):
                            o_ = 4 + dc % 2
                            for fc in range(4):
                                sc.op("pe", lambda en: en.matmul(ps[o_][:], lhsT=w2s[ws][:, fc, dc * 128:(dc + 1) * 128], rhs=actT[a_][:, fc, :],
                                                                 start=(fc == 0), stop=(fc == 3)),
                                      reads=[K("wexp", ws), K("actT", a_, fc)], writes=[K("ps", o_)], inc=(fc == 3))
                            sc.op("dve", lambda en: en.scalar_tensor_tensor(out=xT[:, dc, tl], in0=ps[o_][:], scalar=modT[l][:, 40 + dc, b:b + 1], op0=ALU.mult,
                                                                            in1=xT[:, dc, tl], op1=ALU.add),
                                  reads=[K("ps", o_), K("x", dc, t), K("modall")], writes=[K("x", dc, t)])
                for e_ in ("pe", "act", "dve", "pool", "sp"):
                    sc.wait_all(e_)

        def barrier():
            for e_ in ("pe", "act", "dve", "pool", "sp"):
                sc.wait_all(e_)

        def out_proj(l, b, w_d, zT_):
            with ExitStack() as eso:
                ow = eso.enter_context(SBT("ow", [128, NCH, D], BF16))
                sc.dma("pool", "d_ow", [(ow[:, 0:4, :], w_d[0:512, :].rearrange("(c p) f -> p c f", p=128)),
                                        (ow[:, 4:8, :], w_d[512:1024, :].rearrange("(c p) f -> p c f", p=128))], writes=[K("ow")])
                i_ = 0
                for dc in range(NCH):
                    for t in range(NT):
                        tl = slice(t * TT, (t + 1) * TT)
                        o_ = i_ % 4
                        i_ += 1
                        for kc in range(NCH):
                            sc.op("pe", lambda en: en.matmul(ps[o_][:], lhsT=ow[:, kc, dc * 128:(dc + 1) * 128], rhs=zT_[:, kc, tl],
                                                             start=(kc == 0), stop=(kc == NCH - 1)),
                                  reads=[K("ow"), K("h", kc, t)], writes=[K("ps", o_)], inc=(kc == NCH - 1))
                        sc.op("dve", lambda en: en.scalar_tensor_tensor(out=xT[:, dc, tl], in0=ps[o_][:], scalar=modT[l][:, 16 + dc, b:b + 1], op0=ALU.mult,
                                                                        in1=xT[:, dc, tl], op1=ALU.add),
                              reads=[K("ps", o_), K("x", dc, t)], writes=[K("x", dc, t)])
                barrier()

        def phase_m1(b):
            l = 1
            phase_norm(b, A1[l], modT[l][:, 0:8, :], False)
            HS = S // 2
            with ExitStack() as esz:
                zT = esz.enter_context(SBT("zT", [128, NCH, S], BF16))
                with ExitStack() as es1:
                    def t_(name, shape, dt=F32):
                        return es1.enter_context(SBT(name, list(shape), dt))
                    wyx = [t_(f"wyx{i}", [128, NCH, 256], BF16) for i in range(2)]
                    rgw = t_("rgw", [128, NCH, 128], BF16)
                    igw = t_("igw", [128, NCH, 128], BF16)
                    xbr = t_("xbr", [128, 3 + S])
                    Bgy = t_("Bgy", [128, HS])
                    Bxc = t_("Bxc", [128, HS])
                    Br = t_("Br", [128, HS])
                    Bi = t_("Bi", [128, HS])
                    Bm = t_("Bm", [128, HS])
                    xcb = t_("xcb", [128, HS], BF16)
                    carry = t_("carry", [128, 1])
                    sc.dma("pool", "d_gw", [(rgw[:], rgw_d.rearrange("n k j -> k n j")), (igw[:], igw_d.rearrange("n k j -> k n j"))], writes=[K("gw")])
                    sc.op("pool", lambda en: en.memset(xbr[:, 0:3], 0.0), writes=[K("xbrpad")])
                    ocw, _ = PV_OFF["ocw"]

                    def load_wyx(c):
                        s_ = c % 2
                        sc.dma("pool", f"d_wyx{s_}", [(wyx[s_][:, :, 0:128], odin_d[:, c * 128:(c + 1) * 128].rearrange("(k p) f -> p k f", p=128)),
                                                      (wyx[s_][:, :, 128:256], odin_d[:, 1024 + c * 128:1024 + (c + 1) * 128].rearrange("(k p) f -> p k f", p=128))],
                               writes=[K("wyx", s_)])
                    load_wyx(0)
                    for c in range(NCH):
                        ws = c % 2
                        if c + 1 < NCH:
                            load_wyx(c + 1)
                        for hf in range(2):
                            for tt in range(2):
                                t = hf * 2 + tt
                                tl = slice(t * TT, (t + 1) * TT)
                                hl = slice(tt * TT, (tt + 1) * TT)
                                for kc in range(NCH):
                                    sc.op("pe", lambda en: en.matmul(ps[tt][:], lhsT=wyx[ws][:, kc, 0:128], rhs=hT[:, kc, tl], start=(kc == 0), stop=(kc == NCH - 1)),
                                          reads=[K("wyx", ws), K("h", kc, t)], writes=[K("ps", tt)], inc=(kc == NCH - 1))
                                for kc in range(NCH):
                                    sc.op("pe", lambda en: en.matmul(ps[2 + tt][:], lhsT=wyx[ws][:, kc, 128:256], rhs=hT[:, kc, tl], start=(kc == 0), stop=(kc == NCH - 1)),
                                          reads=[K("wyx", ws), K("h", kc, t)], writes=[K("ps", 2 + tt)], inc=(kc == NCH - 1))
                                sc.op("act", lambda en: en.activation(out=Bgy[:, hl], in_=ps[tt][:], func=AF.Gelu_apprx_tanh), reads=[K("ps", tt)], writes=[K("Bgy", tt)])
                                sc.op("act", lambda en: en.activation(out=xbr[:, 3 + t * TT:3 + (t + 1) * TT], in_=ps[2 + tt][:], func=AF.Identity),
                                      reads=[K("ps", 2 + tt)], writes=[K("xbr", t)])
                            for tt in range(2):
                                t = hf * 2 + tt
                                hl = slice(tt * TT, (tt + 1) * TT)
                                rk = [K("xbr", t), K("xbrpad")] + ([K("xbr", t - 1)] if t > 0 else [])
                                sc.op("dve", lambda en: en.tensor_scalar(out=Bxc[:, hl], in0=xbr[:, t * TT + 3:t * TT + 3 + TT], scalar1=pv[:, ocw + c * 4 + 3:ocw + c * 4 + 4],
                                                                         scalar2=pvs("ocb", c), op0=ALU.mult, op1=ALU.add),
                                      reads=rk, writes=[K("Bxc", tt)])
                                for k in range(3):
                                    sc.op("dve", lambda en: en.scalar_tensor_tensor(out=Bxc[:, hl], in0=xbr[:, t * TT + k:t * TT + k + TT], scalar=pv[:, ocw + c * 4 + k:ocw + c * 4 + k + 1],
                                                                                    op0=ALU.mult, in1=Bxc[:, hl], op1=ALU.add),
                                          reads=rk + [K("Bxc", tt)], writes=[K("Bxc", tt)])
                                sc.op("pool", lambda en: en.tensor_copy(out=xcb[:, hl], in_=Bxc[:, hl]), reads=[K("Bxc", tt)], writes=[K("xcb", tt)])
                                sc.op("pe", lambda en: en.matmul(ps[4 + tt][:], lhsT=rgw[:, c, :], rhs=xcb[:, hl], start=True, stop=True),
                                      reads=[K("gw"), K("xcb", tt)], writes=[K("ps", 4 + tt)])
                                sc.op("pe", lambda en: en.matmul(ps[6 + tt][:], lhsT=igw[:, c, :], rhs=xcb[:, hl], start=True, stop=True),
                                      reads=[K("gw"), K("xcb", tt)], writes=[K("ps", 6 + tt)])
                            for tt in range(2):
                                hl = slice(tt * TT, (tt + 1) * TT)
                                sc.op("act", lambda en: en.activation(out=Br[:, hl], in_=ps[4 + tt][:], func=AF.Sigmoid, bias=pvs("rgb", c)), reads=[K("ps", 4 + tt)], writes=[K("Br", tt)])
                                sc.op("act", lambda en: en.activation(out=Bi[:, hl], in_=ps[6 + tt][:], func=AF.Sigmoid, bias=pvs("igb", c)), reads=[K("ps", 6 + tt)], writes=[K("Bi", tt)])
                            for tt in range(2):
                                hl = slice(tt * TT, (tt + 1) * TT)
                                sc.op("act", lambda en: en.activation(out=Br[:, hl], in_=Br[:, hl], func=AF.Exp, scale=cl8[:, c:c + 1]), reads=[K("Br", tt)], writes=[K("Br", tt)])
                                sc.op("pool", lambda en: en.tensor_tensor(out=Bm[:, hl], in0=Br[:, hl], in1=Br[:, hl], op=ALU.mult), reads=[K("Br", tt)], writes=[K("Bm", tt)])
                                sc.op("pool", lambda en: en.tensor_tensor(out=Bi[:, hl], in0=Bi[:, hl], in1=Bxc[:, hl], op=ALU.mult), reads=[K("Bi", tt), K("Bxc", tt)], writes=[K("Bi", tt)])
                            for tt in range(2):
                                hl = slice(tt * TT, (tt + 1) * TT)
                                sc.op("act", lambda en: en.activation(out=Bm[:, hl], in_=Bm[:, hl], func=AF.Ln, scale=-1.0, bias=1.0), reads=[K("Bm", tt)], writes=[K("Bm", tt)])
                            for tt in range(2):
                                hl = slice(tt * TT, (tt + 1) * TT)
                                sc.op("act", lambda en: en.activation(out=Bm[:, hl], in_=Bm[:, hl], func=AF.Exp, scale=0.5), reads=[K("Bm", tt)], writes=[K("Bm", tt)])
                            for tt in range(2):
                                t = hf * 2 + tt
                                tl = slice(t * TT, (t + 1) * TT)
                                hl = slice(tt * TT, (tt + 1) * TT)
                                sc.op("pool", lambda en: en.tensor_tensor(out=Bi[:, hl], in0=Bi[:, hl], in1=Bm[:, hl], op=ALU.mult), reads=[K("Bi", tt), K("Bm", tt)], writes=[K("Bi", tt)])
                                if t == 0:
                                    sc.op("dve", lambda en: en.tensor_tensor_scan(out=Bxc[:, hl], data0=Br[:, hl], data1=Bi[:, hl], initial=0.0, op0=ALU.mult, op1=ALU.add),
                                          reads=[K("Br", tt), K("Bi", tt), K("Bxc", tt)], writes=[K("Bxc", tt)])
                                else:
                                    sc.op("dve", lambda en: en.tensor_tensor_scan(out=Bxc[:, hl], data0=Br[:, hl], data1=Bi[:, hl], initial=carry[:, 0:1], op0=ALU.mult, op1=ALU.add),
                                          reads=[K("Br", tt), K("Bi", tt), K("Bxc", tt), K("carry")], writes=[K("Bxc", tt)])
                                sc.op("dve", lambda en: en.tensor_copy(out=carry[:, 0:1], in_=Bxc[:, tt * TT + TT - 1:tt * TT + TT]), reads=[K("Bxc", tt)], writes=[K("carry")])
                                sc.op("pool", lambda en: en.tensor_tensor(out=zT[:, c, tl], in0=Bgy[:, hl], in1=Bxc[:, hl], op=ALU.mult),
                                      reads=[K("Bgy", tt), K("Bxc", tt)], writes=[K("z", c, t)])
                    barrier()
                out_proj(l, b, odout_d, zT)

        def phase_m0(b):
            l = 0
            phase_norm(b, A1[l], modT[l][:, 0:8, :], False)
            UP = 30
            with ExitStack() as es0:
                def t0_(name, shape, dt=F32):
                    return es0.enter_context(SBT(name, list(shape), dt))
                u_pad = t0_("u_pad", [128, 4, UP + S], BF16)
                mask_bf = t0_("mask_bf", [128, 4, 512], BF16)
                om, ou, oo = CST_OFF["mask"][0], CST_OFF["ustrict"][0], CST_OFF["ones"][0]
                nmask_bf = t0_("nmask_bf", [128, 4, 512], BF16)
                ident_bf = t0_("ident_bf", [128, 128], BF16)
                onm, oid = CST_OFF["nmask"][0], CST_OFF["ident"][0]
                sc.dma("pool", "d_c3", [(mask_bf[:], cst_d[:, om:om + 2048].rearrange("p (d t) -> p d t", d=4)),
                                        (nmask_bf[:], cst_d[:, onm:onm + 2048].rearrange("p (d t) -> p d t", d=4)),
                                        (ident_bf[:], cst_d[:, oid:oid + 128])], writes=[K("mask")])
                sc.op("pool", lambda en: en.memset(u_pad[:, :, 0:UP], 0.0), writes=[K("upad")])
                esq = ExitStack()
                qT = esq.enter_context(SBT("qT", [128, 4, S], BF16))
                kT = esq.enter_context(SBT("kT", [128, 4, S], BF16))
                v_sb = esq.enter_context(SBT("v_sb", [128, 16, 512], BF16))
                with ExitStack() as esa:
                    def ta_(name, shape, dt=F32):
                        return esa.enter_context(SBT(name, list(shape), dt))
                    wb = [ta_(f"wb{i}", [128, NCH, 512], BF16) for i in range(2)]
                    sig = [ta_(f"sig{i}", [128, TT]) for i in range(2)]
                    qsq = [ta_(f"qsq{i}", [128, TT], BF16) for i in range(2)]
                    rq = [ta_(f"rq{i}", [128, TT]) for i in range(2)]

                    def load_g(g, slot):
                        sc.dma("pool", f"d_wb{slot}", [(wb[slot][:], evin_d[:, g * 512:(g + 1) * 512].rearrange("(k p) f -> p k f", p=128))], writes=[K("wb", slot)])
                    load_g(0, 0)
                    load_g(1, 1)
                    i_ = 0
                    for fc in range(4):
                        for t in range(NT):
                            tl = slice(t * TT, (t + 1) * TT)
                            p_ = i_ % 2
                            i_ += 1
                            for kc in range(NCH):
                                sc.op("pe", lambda en: en.matmul(ps[p_][:], lhsT=wb[0][:, kc, fc * 128:(fc + 1) * 128], rhs=hT[:, kc, tl], start=(kc == 0), stop=(kc == NCH - 1)),
                                      reads=[K("wb", 0), K("h", kc, t)], writes=[K("ps", p_)], inc=(kc == NCH - 1))
                            for kc in range(NCH):
                                sc.op("pe", lambda en: en.matmul(ps[2 + p_][:], lhsT=wb[1][:, kc, fc * 128:(fc + 1) * 128], rhs=hT[:, kc, tl], start=(kc == 0), stop=(kc == NCH - 1)),
                                      reads=[K("wb", 1), K("h", kc, t)], writes=[K("ps", 2 + p_)], inc=(kc == NCH - 1))
                            sc.op("act", lambda en: en.activation(out=sig[p_][:], in_=ps[2 + p_][:], func=AF.Sigmoid), reads=[K("ps", 2 + p_)], writes=[K("sig", p_)])
                            sc.op("dve", lambda en: en.tensor_tensor(out=u_pad[:, fc, UP + t * TT:UP + (t + 1) * TT], in0=ps[p_][:], in1=sig[p_][:], op=ALU.mult),
                                  reads=[K("ps", p_), K("sig", p_)], writes=[K("u", fc, t)])
                    for gi_, (g, dstT, gname) in enumerate([(2, qT, "qg"), (3, kT, "kg")]):
                        slot = gi_ % 2
                        load_g(g, slot)
                        for fc in range(4):
                            for t in range(NT):
                                tl = slice(t * TT, (t + 1) * TT)
                                p_ = i_ % 2
                                i_ += 1
                                for kc in range(NCH):
                                    sc.op("pe", lambda en: en.matmul(ps[4 + p_][:], lhsT=wb[slot][:, kc, fc * 128:(fc + 1) * 128], rhs=hT[:, kc, tl], start=(kc == 0), stop=(kc == NCH - 1)),
                                          reads=[K("wb", slot), K("h", kc, t)], writes=[K("ps", 4 + p_)], inc=(kc == NCH - 1))
                                sc.op("act", lambda en: en.activation(out=qsq[p_][:], in_=ps[4 + p_][:], func=AF.Square), reads=[K("ps", 4 + p_)], writes=[K("qsq", p_)])
                                sc.op("pe", lambda en: en.matmul(ps[6 + p_][:], lhsT=bdon_bf[:], rhs=qsq[p_][:], start=True, stop=True),
                                      reads=[K("bdon"), K("qsq", p_)], writes=[K("ps", 6 + p_)])
                                sc.op("act", lambda en: en.activation(out=rq[p_][:], in_=ps[6 + p_][:], func=AF.Ln, scale=1.0 / 64.0, bias=EPS), reads=[K("ps", 6 + p_)], writes=[K("rq", p_)])
                                sc.op("act", lambda en: en.activation(out=rq[p_][:], in_=rq[p_][:], func=AF.Exp, scale=-0.5), reads=[K("rq", p_)], writes=[K("rq", p_)])
                                sc.op("dve", lambda en: en.scalar_tensor_tensor(out=dstT[:, fc, tl], in0=ps[4 + p_][:], scalar=pvs(gname), op0=ALU.mult, in1=rq[p_][:], op1=ALU.mult),
                                      reads=[K("ps", 4 + p_), K("rq", p_)], writes=[K(gname, fc, t)])
                    load_g(4, 0)
                    for tc in range(16):
                        p_ = tc % 2
                        for kc in range(NCH):
                            sc.op("pe", lambda en: en.matmul(ps[p_][:], lhsT=hT[:, kc, tc * 128:(tc + 1) * 128], rhs=wb[0][:, kc, :], start=(kc == 0), stop=(kc == NCH - 1)),
                                  reads=[K("wb", 0), K("h", kc, tc // 4)], writes=[K("ps", p_)], inc=(kc == NCH - 1))
                        if tc % 2 == 0:
                            sc.op("act", lambda en: en.activation(out=v_sb[:, tc, :], in_=ps[p_][:], func=AF.Identity), reads=[K("ps", p_)], writes=[K("v", tc)])
                        else:
                            sc.op("dve", lambda en: en.tensor_copy(out=v_sb[:, tc, :], in_=ps[p_][:]), reads=[K("ps", p_)], writes=[K("v", tc)])
                    barrier()
                with ExitStack() as esb:
                    def tb_(name, shape, dt=F32):
                        return esb.enter_context(SBT(name, list(shape), dt))
                    NZ, NQ, NW = 3, 5, 4
                    spf = [tb_(f"spf{i}", [128, TT]) for i in range(NZ)]
                    spb = [tb_(f"spb{i}", [128, TT], BF16) for i in range(NZ)]
                    q1 = [tb_(f"q1_{i}", [128, TT]) for i in range(NQ)]
                    wtb = [tb_(f"wtb{i}", [128, TT], BF16) for i in range(NW)]
                    Rb = [tb_(f"Rb{i}", [128, TT], BF16) for i in range(2)]
                    units = []
                    for hp in range(4):
                        for qt in range(NT):
                            nk = 4 * (qt + 1)
                            for sc_ in range(nk - 1, -1, -1):
                                for hh in range(2):
                                    units.append((hp, qt, sc_, hh, nk))

                    def S1(ui):
                        hp, qt, sc_, hh, nk = units[ui]
                        zb = ui % NZ
                        hr = slice(hh * 64, (hh + 1) * 64)
                        sc.op("pe", lambda en: en.matmul(ps[zb][:], lhsT=kT[hr, hp, sc_ * 128:(sc_ + 1) * 128], rhs=qT[hr, hp, qt * TT:(qt + 1) * TT], start=True, stop=True),
                              reads=[K("kg", hp, sc_ // 4), K("qg", hp, qt)], writes=[K("ps", zb)])

                    def S2(ui):
                        zb = ui % NZ
                        sc.op("act", lambda en: en.activation(out=spf[zb][:], in_=ps[zb][:], func=AF.Exp, scale=0.125), reads=[K("ps", zb)], writes=[K("spf", zb)])
                        sc.op("act", lambda en: en.activation(out=spf[zb][:], in_=spf[zb][:], func=AF.Ln, bias=1.0), reads=[K("spf", zb)], writes=[K("spf", zb)])

                    def S3(ui):
                        hp, qt, sc_, hh, nk = units[ui]
                        zb = ui % NZ
                        qb = ui % NQ
                        d = sc_ - 4 * qt
                        sc.op("dve", lambda en: en.scalar_tensor_tensor(out=q1[qb][:], in0=ps[zb][:], scalar=0.125, op0=ALU.mult, in1=spf[zb][:], op1=ALU.subtract),
                              reads=[K("ps", zb), K("spf", zb)], writes=[K("q1", qb)])
                        if d >= 0:
                            sc.op("dve", lambda en: en.tensor_tensor(out=spb[zb][:], in0=spf[zb][:], in1=mask_bf[:, d, :], op=ALU.mult),
                                  reads=[K("spf", zb), K("mask")], writes=[K("spb", zb)])
                        else:
                            sc.op("dve", lambda en: en.tensor_copy(out=spb[zb][:], in_=spf[zb][:]), reads=[K("spf", zb)], writes=[K("spb", zb)])

                    def S4(ui):
                        hp, qt, sc_, hh, nk = units[ui]
                        zb = ui % NZ
                        cb = 3 + ui % 3
                        d = sc_ - 4 * qt
                        first = (sc_ == nk - 1)
                        nmm = 1 + (0 if first else 1) + (1 if d >= 0 else 0)
                        k_ = 1
                        sc.op("pe", lambda en: en.matmul(ps[cb][:], lhsT=ustr_bf[:], rhs=spb[zb][:], start=True, stop=(k_ == nmm)),
                              reads=[K("ustr"), K("spb", zb)], writes=[K("ps", cb)], inc=(k_ == nmm))
                        if not first:
                            k_ += 1
                            sc.op("pe", lambda en: en.matmul(ps[cb][:], lhsT=ones_bf[:], rhs=Rb[hh][:], start=False, stop=(k_ == nmm)),
                                  reads=[K("ones"), K("Rb", hh)], writes=[K("ps", cb)], inc=(k_ == nmm))
                        if d >= 0:
                            k_ += 1
                            sc.op("pe", lambda en: en.matmul(ps[cb][:], lhsT=ident_bf[:], rhs=nmask_bf[:, d, :], start=False, stop=(k_ == nmm)),
                                  reads=[K("mask")], writes=[K("ps", cb)], inc=(k_ == nmm))
                        if sc_ > 0:
                            if first:
                                sc.op("pool", lambda en: en.tensor_copy(out=Rb[hh][:], in_=spb[zb][:]), reads=[K("spb", zb)], writes=[K("Rb", hh)])
                            else:
                                sc.op("pool", lambda en: en.tensor_tensor(out=Rb[hh][:], in0=Rb[hh][:], in1=spb[zb][:], op=ALU.add),
                                      reads=[K("spb", zb), K("Rb", hh)], writes=[K("Rb", hh)])

                    def S5(ui):
                        qb = ui % NQ
                        cb = 3 + ui % 3
                        sc.op("dve", lambda en: en.tensor_tensor(out=q1[qb][:], in0=q1[qb][:], in1=ps[cb][:], op=ALU.subtract),
                              reads=[K("ps", cb), K("q1", qb)], writes=[K("q1", qb)])

                    def S6(ui):
                        qb = ui % NQ
                        wb_ = ui % NW
                        sc.op("act", lambda en: en.activation(out=wtb[wb_][:], in_=q1[qb][:], func=AF.Exp), reads=[K("q1", qb)], writes=[K("wtb", wb_)])

                    def S8(ui):
                        hp, qt, sc_, hh, nk = units[ui]
                        wb_ = ui % NW
                        pvb = 6 + ((hp * NT + qt) % 2)
                        h = 2 * hp + hh
                        sc.op("pe", lambda en: en.matmul(ps[pvb][hh * 64:(hh + 1) * 64, :], lhsT=v_sb[:, sc_, h * 64:(h + 1) * 64], rhs=wtb[wb_][:],
                                                         start=(sc_ == nk - 1), stop=(sc_ == 0)),
                              reads=[K("v", sc_), K("wtb", wb_)], writes=[K("pv", pvb, hh)], inc=True)
                        if sc_ == 0 and hh == 1:
                            sc.op("act", lambda en: en.activation(out=hT[:, 4 + hp, qt * TT:(qt + 1) * TT], in_=ps[pvb][:], func=AF.Identity),
                                  reads=[K("pv", pvb, 0), K("pv", pvb, 1)], writes=[K("h", 4 + hp, qt)])

                    stages = [S1, S2, S3, S4, S5, S6, S8]
                    nu = len(units)
                    for it in range(nu + len(stages) - 1):
                        for si_, fn in enumerate(stages):
                            ui = it - si_
                            if 0 <= ui < nu:
                                fn(ui)
                    barrier()
                esq.close()
                with ExitStack() as esc:
                    def tc_(name, shape, dt=F32):
                        return esc.enter_context(SBT(name, list(shape), dt))
                    Dg = tc_("cv_Dg", [128, 124, 128], BF16)
                    y = [tc_(f"cv_y{i}", [128, 4, TT]) for i in range(2)]
                    ysq = [tc_(f"ysq{i}", [128, TT], BF16) for i in range(4)]
                    ybf = [tc_(f"ybf{i}", [128, TT], BF16) for i in range(4)]
                    mean = tc_("cv_mean", [128, TT])
                    msq = tc_("cv_msq", [128, TT])
                    rs = tc_("cv_rs", [128, TT])
                    t1 = [tc_(f"cv_t1{i}", [128, TT]) for i in range(2)]
                    odw, _ = PV_OFF["dww"]
                    for idx in range(124):
                        sc.op("dve", lambda en: en.tensor_scalar(out=Dg[:, idx, :], in0=ident[:], scalar1=pv[:, odw + idx:odw + idx + 1], scalar2=None, op0=ALU.mult),
                              reads=[K("ident")], writes=[K("Dg", idx)])
                    gi_ = 0
                    for t in range(NT):
                        tl = slice(t * TT, (t + 1) * TT)
                        yb = y[t % 2]
                        for fc in range(4):
                            bk = 2 + gi_ % 4
                            gi_ += 1
                            rk = [K("upad"), K("u", fc, t)] + ([K("u", fc, t - 1)] if t > 0 else [])
                            for k in range(31):
                                sc.op("pe", lambda en: en.matmul(ps[bk][:], lhsT=Dg[:, fc * 31 + k, :], rhs=u_pad[:, fc, t * TT + k:t * TT + k + TT], start=(k == 0), stop=(k == 30)),
                                      reads=rk + [K("Dg", fc * 31 + k)], writes=[K("ps", bk)], inc=(k == 30))
                            sc.op("act", lambda en: en.activation(out=yb[:, fc, :], in_=ps[bk][:], func=AF.Identity, bias=pvs("dwb", fc)), reads=[K("ps", bk)], writes=[K("y", t % 2, fc)])
                            s_ = fc
                            sc.op("act", lambda en: en.activation(out=ysq[s_][:], in_=yb[:, fc, :], func=AF.Square), reads=[K("y", t % 2, fc)], writes=[K("ysq", s_)])
                            sc.op("pool", lambda en: en.tensor_copy(out=ybf[s_][:], in_=yb[:, fc, :]), reads=[K("y", t % 2, fc)], writes=[K("ybf", s_)])
                        for fc in range(4):
                            s_ = fc
                            sc.op("pe", lambda en: en.matmul(ps[0][:], lhsT=ones_bf[:], rhs=ybf[s_][:], start=(fc == 0), stop=(fc == 3)),
                                  reads=[K("ones"), K("ybf", s_)], writes=[K("ps", 0)], inc=(fc == 3))
                        for fc in range(4):
                            s_ = fc
                            sc.op("pe", lambda en: en.matmul(ps[1][:], lhsT=ones_bf[:], rhs=ysq[s_][:], start=(fc == 0), stop=(fc == 3)),
                                  reads=[K("ones"), K("ysq", s_)], writes=[K("ps", 1)], inc=(fc == 3))
                        sc.op("dve", lambda en: en.tensor_scalar(out=mean[:], in0=ps[0][:], scalar1=1.0 / 512.0, scalar2=None, op0=ALU.mult), reads=[K("ps", 0)], writes=[K("mean")])
                        sc.op("pool", lambda en: en.tensor_tensor(out=msq[:], in0=mean[:], in1=mean[:], op=ALU.mult), reads=[K("mean")], writes=[K("msq")])
                        sc.op("dve", lambda en: en.scalar_tensor_tensor(out=rs[:], in0=ps[1][:], scalar=1.0 / 512.0, op0=ALU.mult, in1=msq[:], op1=ALU.subtract),
                              reads=[K("ps", 1), K("msq")], writes=[K("rs")])
                        sc.op("act", lambda en: en.activation(out=rs[:], in_=rs[:], func=AF.Ln, bias=EPS), reads=[K("rs")], writes=[K("rs")])
                        sc.op("act", lambda en: en.activation(out=rs[:], in_=rs[:], func=AF.Exp, scale=-0.5), reads=[K("rs")], writes=[K("rs")])
                        for fc in range(4):
                            s_ = fc % 2
                            sc.op("dve", lambda en: en.tensor_tensor(out=t1[s_][:], in0=yb[:, fc, :], in1=mean[:], op=ALU.subtract), reads=[K("y", t % 2, fc), K("mean")], writes=[K("t1", s_)])
                            sc.op("dve", lambda en: en.tensor_tensor(out=t1[s_][:], in0=t1[s_][:], in1=rs[:], op=ALU.mult), reads=[K("t1", s_), K("rs")], writes=[K("t1", s_)])
                            sc.op("act", lambda en: en.activation(out=hT[:, fc, tl], in_=t1[s_][:], func=AF.Silu, scale=pvs("lng", fc), bias=pvs("lnb", fc)),
                                  reads=[K("t1", s_)], writes=[K("h", fc, t)])
                    barrier()
            out_proj(l, b, evout_d, hT)

        for si in range(nseq):
            b = si
            for c in range(NCH):
                sc.dma("sp", f"d_x{c}", [(xT[:, c, :], xT_d[si, c * 128:(c + 1) * 128, :])], writes=[K("x", c, t) for t in range(NT)])
            for ph in phases:
                l = int(ph[1])
                if ph[0] == "e":
                    phase_moe(l, b)
                elif ph == "m0":
                    phase_m0(b)
                elif ph == "m1":
                    phase_m1(b)
            for c in range(NCH):
                sc.dma("sp", f"d_o{c}", [(outT_d[si, c * 128:(c + 1) * 128, :], xT[:, c, :])], reads=[K("x", c, t) for t in range(NT)])
        for e_ in ("sp",):
            sc.wait_all(e_)
        print(f"[build] instructions={sc.n_inst} waits={sc.n_wait} counts={ {k: v for k, v in sc.count.items() if k in ('pe','act','dve','pool')} }")
    return nc


def kernel(**inputs):
    inp = {k: np.asarray(v) for k, v in inputs.items()}
    x = inp["x"]
    B = x.shape[0]
    nseq = B // N_CORES
    nc = build_program(nseq=nseq)
    cst = make_consts()
    shared = {
        "cst": cst,
        "mod_w": np.ascontiguousarray(inp["mod_w"], np.float32),
        "ev_in_w": np.ascontiguousarray(inp["ev_in_w"][0], np.float32),
        "ev_out_w": np.ascontiguousarray(inp["ev_out_w"][0], np.float32),
        "od_in_w": np.ascontiguousarray(inp["od_in_w"][0], np.float32),
        "od_out_w": np.ascontiguousarray(inp["od_out_w"][0], np.float32),
        "od_rg_w": np.ascontiguousarray(inp["od_rg_w"][0], np.float32),
        "od_ig_w": np.ascontiguousarray(inp["od_ig_w"][0], np.float32),
        "router_w": np.ascontiguousarray(inp["router_w"], np.float32),
        "ex_w1": np.ascontiguousarray(inp["ex_w1"], np.float32),
        "ex_w3": np.ascontiguousarray(inp["ex_w3"], np.float32),
        "ex_w2": np.ascontiguousarray(inp["ex_w2"], np.float32),
    }
    in_maps = []
    for core in range(N_CORES):
        b0 = core * nseq
        m = dict(shared)
        m["xT"] = np.ascontiguousarray(np.transpose(x[b0:b0 + nseq], (0, 2, 1)))
        m["pvec"] = make_pvec(inp, b0, nseq)
        in_maps.append(m)
    res = run_bass_kernel_spmd(nc, in_maps, core_ids=list(range(N_CORES)))
    out = np.empty_like(x)
    for core in range(N_CORES):
        b0 = core * nseq
        out[b0:b0 + nseq] = np.transpose(res.results[core]["outT"], (0, 2, 1))
    return out
```

```python
import numpy as np
from contextlib import ExitStack
import concourse.bass as bass
import concourse.mybir as mybir
from concourse.bass_utils import run_bass_kernel_spmd

F32 = mybir.dt.float32
BF16 = mybir.dt.bfloat16
AF = mybir.ActivationFunctionType
ALU = mybir.AluOpType
AX = mybir.AxisListType

D = 1024
S = 2048
NCH = 8
TT = 512
NT = 4
NE = 16
EPS = 1e-6
N_CORES = 8
SEQ_PER_CORE = 4


def _fm(v):
    v = np.asarray(v, np.float32)
    return np.ascontiguousarray(v.reshape(-1, 128).T)


PV_FIELDS = [("mixg", 16), ("ffng", 16), ("modb", 96), ("dwb", 4), ("lng", 4), ("lnb", 4), ("dww", 124),
             ("ocw", 32), ("ocb", 8), ("rgb", 8), ("igb", 8), ("lam", 8), ("qg", 1), ("kg", 1), ("rb", 16), ("cT", 32)]
PV_OFF = {}
_o = 0
for _n, _w in PV_FIELDS:
    PV_OFF[_n] = (_o, _w)
    _o += _w
NV = _o

CST_FIELDS = [("ident", 128), ("ustrict", 128), ("bdones", 128), ("ones", 128), ("mask", 2048), ("sel", 2048), ("nmask", 2048)]
CST_OFF = {}
_o = 0
for _n, _w in CST_FIELDS:
    CST_OFF[_n] = (_o, _w)
    _o += _w
NCST = _o


def make_consts():
    c = np.zeros((128, NCST), np.float32)
    j = np.arange(128)[:, None]
    s = np.arange(128)[None, :]
    c[:, CST_OFF["ident"][0]:][:, :128] = (j == s)
    c[:, CST_OFF["ustrict"][0]:][:, :128] = (j > s)
    c[:, CST_OFF["bdones"][0]:][:, :128] = ((j // 64) == (s // 64))
    c[:, CST_OFF["ones"][0]:][:, :128] = 1.0
    t = np.arange(512)[None, :]
    for d in range(4):
        o = CST_OFF["mask"][0] + d * 512
        c[:, o:o + 512] = ((128 * d + j) < t)
    o = CST_OFF["sel"][0]
    for e in range(16):
        c[e, o + e * 128:o + (e + 1) * 128] = 1.0
    for d in range(4):
        o = CST_OFF["nmask"][0] + d * 512
        c[:, o:o + 512] = np.where((128 * d + j) < t, 0.0, 30000.0)
    return c


def make_pvec(inp, b0, nseq):
    pv = np.zeros((128, NV), np.float32)

    def put(name, arr):
        o, w = PV_OFF[name]
        assert arr.shape == (128, w), (name, arr.shape, w)
        pv[:, o:o + w] = arr

    put("mixg", np.concatenate([_fm(inp["mix_norm_g"][l]) for l in range(2)], axis=1))
    put("ffng", np.concatenate([_fm(inp["ffn_norm_g"][l]) for l in range(2)], axis=1))
    put("modb", np.concatenate([_fm(inp["mod_b"][l]) for l in range(2)], axis=1))
    put("dwb", _fm(inp["ev_dw_b"][0]))
    put("lng", _fm(inp["ev_ln_g"][0]))
    put("lnb", _fm(inp["ev_ln_b"][0]))
    dw = np.asarray(inp["ev_dw_w"][0], np.float32)
    put("dww", np.ascontiguousarray(dw.reshape(31, 4, 128).transpose(2, 1, 0).reshape(128, 124)))
    cw = np.asarray(inp["od_conv_w"][0], np.float32)
    put("ocw", np.ascontiguousarray(cw.reshape(4, 8, 128).transpose(2, 1, 0).reshape(128, 32)))
    put("ocb", _fm(inp["od_conv_b"][0]))
    put("rgb", _fm(inp["od_rg_b"][0]))
    put("igb", _fm(inp["od_ig_b"][0]))
    put("lam", _fm(inp["od_lam"][0]))
    put("qg", np.tile(np.asarray(inp["ev_q_g"][0], np.float32), 2)[:, None])
    put("kg", np.tile(np.asarray(inp["ev_k_g"][0], np.float32), 2)[:, None])
    put("rb", np.tile(np.asarray(inp["router_b"], np.float32)[None, :], (128, 1)))
    cc = np.zeros((4, 1024), np.float32)
    cc[:nseq] = np.asarray(inp["c"][b0:b0 + nseq], np.float32)
    put("cT", np.ascontiguousarray(cc.reshape(4, 8, 128).transpose(2, 1, 0).reshape(128, 32)))
    return pv


class Sched:
    def __init__(self, nc, es):
        self.nc = nc
        self.es = es
        self.engines = {"pe": nc.tensor, "act": nc.scalar, "dve": nc.vector, "pool": nc.gpsimd, "sp": nc.sync}
        self.sems = {}
        self.count = {}
        self.seen = {e: {} for e in self.engines}
        self.lastw = {}
        self.readers = {}
        self.n_inst = 0
        self.n_wait = 0
        for e in ("pe", "act", "dve", "pool"):
            self._sem(e)

    def _sem(self, name):
        if name not in self.sems:
            self.sems[name] = self.es.enter_context(self.nc.semaphore(name))
            self.count[name] = 0
        return self.sems[name]

    def _wait(self, eng, sem, val):
        if val <= 0 or self.seen[eng].get(sem, 0) >= val:
            return
        self.engines[eng].wait_ge(self.sems[sem], val)
        self.seen[eng][sem] = val
        self.n_wait += 1

    def _deps(self, eng, reads, writes):
        need = {}
        for k in reads:
            ev = self.lastw.get(k)
            if ev is not None and ev[1] > need.get(ev[0], 0):
                need[ev[0]] = ev[1]
        for k in writes:
            ev = self.lastw.get(k)
            if ev is not None and ev[1] > need.get(ev[0], 0):
                need[ev[0]] = ev[1]
            rd = self.readers.get(k)
            if rd:
                for s_, v_ in rd.items():
                    if v_ > need.get(s_, 0):
                        need[s_] = v_
        for s_, v_ in need.items():
            if s_ == eng:
                if v_ > self.count[eng]:
                    continue
                if eng == "pe":
                    continue
            self._wait(eng, s_, v_)

    def _record(self, ev, reads, writes):
        for k in reads:
            rd = self.readers.setdefault(k, {})
            if ev[1] > rd.get(ev[0], 0):
                rd[ev[0]] = ev[1]
        for k in writes:
            self.lastw[k] = ev
            self.readers[k] = {}

    def op(self, eng, fn, reads=(), writes=(), inc=True):
        self._deps(eng, reads, writes)
        inst = fn(self.engines[eng])
        self.n_inst += 1
        if inc:
            inst.then_inc(self.sems[eng], 1)
            self.count[eng] += 1
            ev = (eng, self.count[eng])
        else:
            ev = (eng, self.count[eng] + 1)
        self._record(ev, reads, writes)
        return inst

    def dma(self, q, sem, pairs, reads=(), writes=()):
        self._sem(sem)
        self._deps(q, reads, writes)
        self._wait(q, sem, self.count[sem])
        for out, in_ in pairs:
            self.engines[q].dma_start(out=out, in_=in_).then_inc(self.sems[sem], 16)
            self.count[sem] += 16
            self.n_inst += 1
        ev = (sem, self.count[sem])
        self._record(ev, reads, writes)

    def wait_all(self, eng):
        for s_, v_ in self.count.items():
            self._wait(eng, s_, v_)


def build_program(nseq=SEQ_PER_CORE, phases=("m0", "e0", "m1", "e1")):
    nc = bass.Bass("TRN2", target_bir_lowering=False)
    dr = {}

    def din(name, shape):
        dr[name] = nc.dram_tensor(name, list(shape), F32, kind="ExternalInput").ap()
        return dr[name]

    xT_d = din("xT", [nseq, D, S])
    pvec_d = din("pvec", [128, NV])
    cst_d = din("cst", [128, NCST])
    modw_d = din("mod_w", [2, D, 6 * D])
    evin_d = din("ev_in_w", [D, 2560])
    evout_d = din("ev_out_w", [D, D])
    odin_d = din("od_in_w", [D, 2048])
    odout_d = din("od_out_w", [D, D])
    rgw_d = din("od_rg_w", [8, 128, 128])
    igw_d = din("od_ig_w", [8, 128, 128])
    rw_d = din("router_w", [D, NE])
    w1_d = din("ex_w1", [2, NE, D, 512])
    w3_d = din("ex_w3", [2, NE, D, 512])
    w2_d = din("ex_w2", [2, NE, 512, D])
    outT_d = nc.dram_tensor("outT", [nseq, D, S], F32, kind="ExternalOutput").ap()

    _uid = [0]

    def SBT(name, shape, dt):
        _uid[0] += 1
        return nc.sbuf_tensor(f"{name}_{_uid[0]}", shape, dt)

    with ExitStack() as es:
        sc = Sched(nc, es)

        def sb(name, shape, dt):
            return es.enter_context(SBT(name, list(shape), dt))

        xT = sb("xT_s", [128, NCH, S], F32)
        hT = sb("hT_s", [128, NCH, S], BF16)
        pv = sb("pv_s", [128, NV], F32)
        ident = sb("ident", [128, 128], F32)
        ones_bf = sb("ones_bf", [128, 128], BF16)
        ustr_bf = sb("ustr_bf", [128, 128], BF16)
        bdon_bf = sb("bdon_bf", [128, 128], BF16)
        sel = sb("sel", [16, NE, 128], F32)
        rw = sb("rw", [128, NCH, NE], F32)
        modT = [sb(f"modT{l}", [128, 48, 4], F32) for l in range(2)]
        A1 = [sb(f"A1_{l}", [128, NCH, 4], F32) for l in range(2)]
        A2 = [sb(f"A2_{l}", [128, NCH, 4], F32) for l in range(2)]
        cl8 = sb("cl8", [128, NCH], F32)
        ps = [es.enter_context(nc.psum_tensor(f"ps{i}", [128, 512], F32)) for i in range(8)]

        def pvs(name, i=0, n=None):
            o, w = PV_OFF[name]
            n = 1 if n is None else n
            return pv[:, o + i:o + i + n]

        def K(*a):
            return a

        o_, _ = CST_OFF["ident"]
        sc.dma("sp", "d_c0", [(pv[:], pvec_d[:, :]), (ident[:], cst_d[:, o_:o_ + 128])], writes=[K("pv"), K("ident")])
        o_, _ = CST_OFF["sel"]
        sc.dma("sp", "d_c1", [(sel[:], cst_d[0:16, o_:o_ + 2048].rearrange("p (e m) -> p e m", e=NE)),
                              (rw[:], rw_d.rearrange("(c p) e -> p c e", p=128))], writes=[K("sel"), K("rw")])
        oo, ou, ob, om = CST_OFF["ones"][0], CST_OFF["ustrict"][0], CST_OFF["bdones"][0], CST_OFF["mask"][0]
        sc.dma("pool", "d_c2", [(ones_bf[:], cst_d[:, oo:oo + 128]), (ustr_bf[:], cst_d[:, ou:ou + 128]),
                                (bdon_bf[:], cst_d[:, ob:ob + 128])],
               writes=[K("ones"), K("ustr"), K("bdon")])

        with ExitStack() as es2:
            cond = es2.enter_context(SBT("cond", [128, NCH, 4], F32))
            mw = [es2.enter_context(SBT(f"mw{i}", [128, NCH, 512], F32)) for i in range(2)]
            o_, _ = PV_OFF["cT"]
            sc.op("act", lambda e: e.activation(out=cond[:].rearrange("p c b -> p (c b)"), in_=pv[:, o_:o_ + 32], func=AF.Silu),
                  reads=[K("pv")], writes=[K("cond")])
            gi = 0
            for l in range(2):
                for g in range(12):
                    slot = gi % 2
                    gi += 1
                    sc.dma("sp", f"d_mw{slot}", [(mw[slot][:], modw_d[l, :, g * 512:(g + 1) * 512].rearrange("(c p) f -> p c f", p=128))],
                           writes=[K("mw", slot)])
                    for fc in range(4):
                        f = g * 4 + fc
                        for kc in range(NCH):
                            sc.op("pe", lambda e: e.matmul(ps[0][:, f * 4:(f + 1) * 4], lhsT=mw[slot][:, kc, fc * 128:(fc + 1) * 128],
                                                           rhs=cond[:, kc, :], start=(kc == 0), stop=(kc == NCH - 1)),
                                  reads=[K("mw", slot), K("cond")], writes=[K("ps", 0)], inc=(kc == NCH - 1))
                o_, _ = PV_OFF["modb"]
                sc.op("dve", lambda e: e.tensor_tensor(out=modT[l][:], in0=ps[0][:, 0:192].rearrange("p (f b) -> p f b", b=4),
                                                       in1=pv[:, o_ + l * 48:o_ + (l + 1) * 48].unsqueeze(2).broadcast_to([128, 48, 4]),
                                                       op=ALU.add),
                      reads=[K("ps", 0), K("pv")], writes=[K("modT", l)])
                og, _ = PV_OFF["mixg"]
                sc.op("dve", lambda e: e.scalar_tensor_tensor(out=A1[l][:], in0=modT[l][:, 8:16, :], scalar=1.0, op0=ALU.add,
                                                              in1=pv[:, og + l * 8:og + (l + 1) * 8].unsqueeze(2).broadcast_to([128, 8, 4]),
                                                              op1=ALU.mult),
                      reads=[K("modT", l), K("pv")], writes=[K("A1", l)])
                og, _ = PV_OFF["ffng"]
                sc.op("dve", lambda e: e.scalar_tensor_tensor(out=A2[l][:], in0=modT[l][:, 32:40, :], scalar=1.0, op0=ALU.add,
                                                              in1=pv[:, og + l * 8:og + (l + 1) * 8].unsqueeze(2).broadcast_to([128, 8, 4]),
                                                              op1=ALU.mult),
                      reads=[K("modT", l), K("pv")], writes=[K("A2", l)])
            ol, _ = PV_OFF["lam"]
            sc.op("act", lambda e: e.activation(out=cl8[:], in_=pv[:, ol:ol + 8], func=AF.Exp, scale=-1.0), reads=[K("pv")], writes=[K("cl8")])
            sc.op("act", lambda e: e.activation(out=cl8[:], in_=cl8[:], func=AF.Ln, bias=1.0), reads=[K("cl8")], writes=[K("cl8")])
            sc.op("dve", lambda e: e.tensor_scalar(out=cl8[:], in0=cl8[:], scalar1=-8.0, scalar2=None, op0=ALU.mult),
                  reads=[K("cl8")], writes=[K("cl8")])
            for e_ in ("pe", "act", "dve", "pool", "sp"):
                sc.wait_all(e_)

        xkeys = [K("x", c, t) for c in range(NCH) for t in range(NT)]
        hkeys = [K("h", c, t) for c in range(NCH) for t in range(NT)]

        def phase_norm(b, Aap, Sap, router, lgT=None):
            with ExitStack() as esn:
                rstd = esn.enter_context(SBT("rstd", [128, NT, TT], F32))
                sqb = [esn.enter_context(SBT(f"sqb{i}", [128, TT], BF16)) for i in range(3)]
                if router:
                    tbuf = [esn.enter_context(SBT(f"tbuf{i}", [128, NCH, TT], F32)) for i in range(2)]
                    srw = esn.enter_context(SBT("srw", [16, 1], F32))
                else:
                    tmp = [esn.enter_context(SBT(f"ntmp{i}", [128, TT], F32)) for i in range(3)]
                i2 = 0
                for t in range(NT):
                    tl = slice(t * TT, (t + 1) * TT)
                    for c in range(NCH):
                        s_ = i2 % 3
                        i2 += 1
                        if c % 2 == 0:
                            sc.op("act", lambda e: e.activation(out=sqb[s_][:], in_=xT[:, c, tl], func=AF.Square),
                                  reads=[K("x", c, t)], writes=[K("sqb", s_)])
                        else:
                            sc.op("dve", lambda e: e.tensor_tensor(out=sqb[s_][:], in0=xT[:, c, tl], in1=xT[:, c, tl], op=ALU.mult),
                                  reads=[K("x", c, t)], writes=[K("sqb", s_)])
                        sc.op("pe", lambda e: e.matmul(ps[t][:], lhsT=ones_bf[:], rhs=sqb[s_][:], start=(c == 0), stop=(c == NCH - 1)),
                              reads=[K("ones"), K("sqb", s_)], writes=[K("ps", t)], inc=True)
                if router:
                    for c in range(NCH):
                        sc.op("pe", lambda e: e.matmul(ps[5][0:16, 0:1], lhsT=rw[:, c, :], rhs=Sap[:, c, b:b + 1], start=(c == 0), stop=(c == NCH - 1)),
                              reads=[K("rw")], writes=[K("ps", 5)], inc=(c == NCH - 1))
                    sc.op("dve", lambda e: e.tensor_copy(out=srw[:], in_=ps[5][0:16, 0:1]), reads=[K("ps", 5)], writes=[K("srw")])
                for t in range(NT):
                    sc.op("act", lambda e: e.activation(out=rstd[:, t, :], in_=ps[t][:], func=AF.Ln, scale=1.0 / D, bias=EPS),
                          reads=[K("ps", t)], writes=[K("rstd", t)])
                for t in range(NT):
                    sc.op("act", lambda e: e.activation(out=rstd[:, t, :], in_=rstd[:, t, :], func=AF.Exp, scale=-0.5),
                          reads=[K("rstd", t)], writes=[K("rstd", t)])
                i2 = 0
                for t in range(NT):
                    tl = slice(t * TT, (t + 1) * TT)
                    for c in range(NCH):
                        if router:
                            dst, dkey = tbuf[t % 2][:, c, :], K("tbuf", t % 2, c)
                        else:
                            s_ = i2 % 3
                            i2 += 1
                            dst, dkey = tmp[s_][:], K("ntmp", s_)
                        sc.op("dve", lambda e: e.scalar_tensor_tensor(out=dst, in0=xT[:, c, tl], scalar=Aap[:, c, b:b + 1], op0=ALU.mult, in1=rstd[:, t, :], op1=ALU.mult),
                              reads=[K("x", c, t), K("rstd", t)], writes=[dkey])
                        sc.op("act", lambda e: e.activation(out=hT[:, c, tl], in_=dst, func=AF.Identity, bias=Sap[:, c, b:b + 1]),
                              reads=[dkey], writes=[K("h", c, t)])
                    if router:
                        for c in range(NCH):
                            sc.op("pe", lambda e: e.matmul(ps[4][0:16, :], lhsT=rw[:, c, :], rhs=tbuf[t % 2][:, c, :], start=(c == 0), stop=(c == NCH - 1)),
                                  reads=[K("rw"), K("tbuf", t % 2, c)], writes=[K("ps", 4)], inc=(c == NCH - 1))
                        sc.op("act", lambda e: e.activation(out=lgT[:, tl], in_=ps[4][0:16, :], func=AF.Identity, bias=srw[:, 0:1]),
                              reads=[K("ps", 4), K("srw")], writes=[K("lgT", t)])
                for e_ in ("pe", "act", "dve", "pool", "sp"):
                    sc.wait_all(e_)

        def phase_route(lgT, gT):
            with ExitStack() as esr:
                def t_(name, shape):
                    return esr.enter_context(SBT(name, list(shape), F32))
                scr = t_("r_sc", [128, 256])
                bi = t_("r_bi", [128, 256])
                p6 = t_("r_p6", [128, 64, 6])
                gs = t_("r_gs", [128, 64])
                gmax = t_("r_gmax", [128, 16])
                goh = t_("r_goh", [128, 64])
                m1 = t_("r_m1", [128, 64])
                e1 = t_("r_e1", [128, 256])
                b2 = t_("r_b2", [128, 256])
                m2 = t_("r_m2", [128, 64])
                sl = t_("r_sl", [128, 256])
                den = t_("r_den", [128, 16])
                gates = t_("r_gates", [128, 256])
                for tc in range(16):
                    sc.op("pe", lambda e: e.transpose(out=ps[5][:, tc * 16:(tc + 1) * 16], in_=lgT[:, tc * 128:(tc + 1) * 128], identity=ident[0:16, 0:16]),
                          reads=[K("lgT", tc // 4), K("ident")], writes=[K("ps", 5)], inc=(tc == 15))
                sc.op("act", lambda e: e.activation(out=scr[:], in_=ps[5][:, 0:256], func=AF.Sigmoid), reads=[K("ps", 5)], writes=[K("r_sc")])
                o_, _ = PV_OFF["rb"]
                V = sc.op
                V("dve", lambda e: e.tensor_tensor(out=bi[:].rearrange("p (t e) -> p t e", e=16), in0=scr[:].rearrange("p (t e) -> p t e", e=16),
                                                   in1=pv[:, o_:o_ + 16].unsqueeze(1).broadcast_to([128, 16, 16]), op=ALU.add),
                  reads=[K("r_sc"), K("pv")], writes=[K("r_bi")])
                bi4 = bi[:].rearrange("p (g k) -> p g k", k=4)
                V("dve", lambda e: e.tensor_tensor(out=p6[:, :, 0:3], in0=bi4[:, :, 0:3], in1=bi4[:, :, 1:4], op=ALU.add), reads=[K("r_bi")], writes=[K("r_p6a")])
                V("dve", lambda e: e.tensor_tensor(out=p6[:, :, 3:5], in0=bi4[:, :, 0:2], in1=bi4[:, :, 2:4], op=ALU.add), reads=[K("r_bi")], writes=[K("r_p6b")])
                V("dve", lambda e: e.tensor_tensor(out=p6[:, :, 5:6], in0=bi4[:, :, 0:1], in1=bi4[:, :, 3:4], op=ALU.add), reads=[K("r_bi")], writes=[K("r_p6c")])
                V("dve", lambda e: e.tensor_reduce(out=gs[:], in_=p6[:], axis=AX.X, op=ALU.max), reads=[K("r_p6a"), K("r_p6b"), K("r_p6c")], writes=[K("r_gs")])
                V("dve", lambda e: e.tensor_reduce(out=gmax[:], in_=gs[:].rearrange("p (t g) -> p t g", g=4), axis=AX.X, op=ALU.max), reads=[K("r_gs")], writes=[K("r_gmax")])
                V("dve", lambda e: e.tensor_tensor(out=goh[:].rearrange("p (t g) -> p t g", g=4), in0=gs[:].rearrange("p (t g) -> p t g", g=4),
                                                   in1=gmax[:].unsqueeze(2).broadcast_to([128, 16, 4]), op=ALU.is_equal), reads=[K("r_gs"), K("r_gmax")], writes=[K("r_goh")])
                V("dve", lambda e: e.tensor_reduce(out=m1[:], in_=bi4, axis=AX.X, op=ALU.max), reads=[K("r_bi")], writes=[K("r_m1")])
                V("dve", lambda e: e.tensor_tensor(out=e1[:].rearrange("p (g k) -> p g k", k=4), in0=bi4, in1=m1[:].unsqueeze(2).broadcast_to([128, 64, 4]), op=ALU.is_equal),
                  reads=[K("r_bi"), K("r_m1")], writes=[K("r_e1")])
                V("dve", lambda e: e.scalar_tensor_tensor(out=b2[:], in0=e1[:], scalar=-1.0e9, op0=ALU.mult, in1=bi[:], op1=ALU.add), reads=[K("r_e1"), K("r_bi")], writes=[K("r_b2")])
                V("dve", lambda e: e.tensor_reduce(out=m2[:], in_=b2[:].rearrange("p (g k) -> p g k", k=4), axis=AX.X, op=ALU.max), reads=[K("r_b2")], writes=[K("r_m2")])
                V("dve", lambda e: e.tensor_tensor(out=sl[:].rearrange("p (g k) -> p g k", k=4), in0=bi4, in1=m2[:].unsqueeze(2).broadcast_to([128, 64, 4]), op=ALU.is_ge),
                  reads=[K("r_bi"), K("r_m2")], writes=[K("r_sl")])
                V("dve", lambda e: e.tensor_tensor(out=sl[:].rearrange("p (g k) -> p g k", k=4), in0=sl[:].rearrange("p (g k) -> p g k", k=4),
                                                   in1=goh[:].unsqueeze(2).broadcast_to([128, 64, 4]), op=ALU.mult), reads=[K("r_sl"), K("r_goh")], writes=[K("r_sl")])
                V("dve", lambda e: e.tensor_tensor(out=sl[:], in0=sl[:], in1=scr[:], op=ALU.mult), reads=[K("r_sl"), K("r_sc")], writes=[K("r_sl")])
                V("dve", lambda e: e.tensor_reduce(out=den[:], in_=sl[:].rearrange("p (t e) -> p t e", e=16), axis=AX.X, op=ALU.add), reads=[K("r_sl")], writes=[K("r_den")])
                V("dve", lambda e: e.reciprocal(out=den[:], in_=den[:]), reads=[K("r_den")], writes=[K("r_den")])
                V("dve", lambda e: e.tensor_tensor(out=gates[:].rearrange("p (t e) -> p t e", e=16), in0=sl[:].rearrange("p (t e) -> p t e", e=16),
                                                   in1=den[:].unsqueeze(2).broadcast_to([128, 16, 16]), op=ALU.mult), reads=[K("r_sl"), K("r_den")], writes=[K("r_gates")])
                for tc in range(16):
                    bk = tc // 4
                    sc.op("pe", lambda e: e.transpose(out=ps[bk][0:16, (tc % 4) * 128:(tc % 4 + 1) * 128], in_=gates[:, tc * 16:(tc + 1) * 16], identity=ident[:]),
                          reads=[K("r_gates"), K("ident")], writes=[K("ps", bk)], inc=(tc % 4 == 3))
                for bk in range(4):
                    sc.op("act", lambda e: e.activation(out=gT[:, bk * TT:(bk + 1) * TT], in_=ps[bk][0:16, :], func=AF.Identity),
                          reads=[K("ps", bk)], writes=[K("gT", bk)])
                for e_ in ("pe", "act", "dve", "pool", "sp"):
                    sc.wait_all(e_)

        def phase_moe(l, b):
            with ExitStack() as esm:
                lgT = esm.enter_context(SBT("lgT", [16, S], F32))
                gT = esm.enter_context(SBT("gT", [16, S], F32))
                phase_norm(b, A2[l], modT[l][:, 24:32, :], True, lgT)
                phase_route(lgT, gT)
                w1s = [esm.enter_context(SBT(f"w1s{i}", [128, NCH, 512], BF16)) for i in range(2)]
                w3s = [esm.enter_context(SBT(f"w3s{i}", [128, NCH, 512], BF16)) for i in range(2)]
                w2s = [esm.enter_context(SBT(f"w2s{i}", [128, 4, D], BF16)) for i in range(2)]
                gbc = [esm.enter_context(SBT(f"gbc{i}", [128, TT], F32)) for i in range(2)]
                s1 = [esm.enter_context(SBT(f"s1_{i}", [128, TT], F32)) for i in range(2)]
                sg = [esm.enter_context(SBT(f"sg_{i}", [128, TT], F32)) for i in range(2)]
                actT = [esm.enter_context(SBT(f"actT{i}", [128, 4, TT], BF16)) for i in range(2)]

                def load_w(e):
                    s_ = e % 2
                    sc.dma("pool", f"d_w{s_}",
                           [(w1s[s_][:], w1_d[l, e].rearrange("(c p) f -> p c f", p=128)),
                            (w3s[s_][:], w3_d[l, e].rearrange("(c p) f -> p c f", p=128)),
                            (w2s[s_][:], w2_d[l, e].rearrange("(c p) f -> p c f", p=128))],
                           writes=[K("wexp", s_)])

                load_w(0)
                it = 0
                fi = 0
                for e in range(NE):
                    ws = e % 2
                    if e + 1 < NE:
                        load_w(e + 1)
                    for t in range(NT):
                        tl = slice(t * TT, (t + 1) * TT)
                        a_ = it % 2
                        it += 1
                        sc.op("pe", lambda en: en.matmul(ps[6][:], lhsT=sel[:, e, :], rhs=gT[:, tl], start=True, stop=True),
                              reads=[K("sel"), K("gT", t)], writes=[K("ps", 6)])
                        sc.op("act", lambda en: en.activation(out=gbc[a_][:], in_=ps[6][:], func=AF.Identity), reads=[K("ps", 6)], writes=[K("gbc", a_)])
                        for fc in range(4):
                            f_ = fi % 2
                            fi += 1
                            for kc in range(NCH):
                                sc.op("pe", lambda en: en.matmul(ps[0 + f_][:], lhsT=w1s[ws][:, kc, fc * 128:(fc + 1) * 128], rhs=hT[:, kc, tl],
                                                                 start=(kc == 0), stop=(kc == NCH - 1)),
                                      reads=[K("wexp", ws), K("h", kc, t)], writes=[K("ps", 0 + f_)], inc=(kc == NCH - 1))
                            for kc in range(NCH):
                                sc.op("pe", lambda en: en.matmul(ps[2 + f_][:], lhsT=w3s[ws][:, kc, fc * 128:(fc + 1) * 128], rhs=hT[:, kc, tl],
                                                                 start=(kc == 0), stop=(kc == NCH - 1)),
                                      reads=[K("wexp", ws), K("h", kc, t)], writes=[K("ps", 2 + f_)], inc=(kc == NCH - 1))
                            sc.op("act", lambda en: en.activation(out=s1[f_][:], in_=ps[0 + f_][:], func=AF.Silu), reads=[K("ps", 0 + f_)], writes=[K("s1", f_)])
                            sc.op("pool", lambda en: en.tensor_tensor(out=sg[f_][:], in0=s1[f_][:], in1=gbc[a_][:], op=ALU.mult),
                                  reads=[K("s1", f_), K("gbc", a_)], writes=[K("sg", f_)])
                            sc.op("dve", lambda en: en.tensor_tensor(out=actT[a_][:, fc, :], in0=ps[2 + f_][:], in1=sg[f_][:], op=ALU.mult),
                                  reads=[K("ps", 2 + f_), K("sg", f_)], writes=[K("actT", a_, fc)])
                        for dc in range(NCH):
                            o_ = 4 + dc % 2
                            for fc in range(4):
                                sc.op("pe", lambda en: en.matmul(ps[o_][:], lhsT=w2s[ws][:, fc, dc * 128:(dc + 1) * 128], rhs=actT[a_][:, fc, :],
                                                                 start=(fc == 0), stop=(fc == 3)),
                                      reads=[K("wexp", ws), K("actT", a_, fc)], writes=[K("ps", o_)], inc=(fc == 3))
                            sc.op("dve", lambda en: en.scalar_tensor_tensor(out=xT[:, dc, tl], in0=ps[o_][:], scalar=modT[l][:, 40 + dc, b:b + 1], op0=ALU.mult,
                                                                            in1=xT[:, dc, tl], op1=ALU.add),
                                  reads=[K("ps", o_), K("x", dc, t), K("modall")], writes=[K("x", dc, t)])
                for e_ in ("pe", "act", "dve", "pool", "sp"):
                    sc.wait_all(e_)

        def barrier():
            for e_ in ("pe", "act", "dve", "pool", "sp"):
                sc.wait_all(e_)

        def out_proj(l, b, w_d, zT_):
            with ExitStack() as eso:
                ow = eso.enter_context(SBT("ow", [128, NCH, D], BF16))
                sc.dma("pool", "d_ow", [(ow[:, 0:4, :], w_d[0:512, :].rearrange("(c p) f -> p c f", p=128)),
                                        (ow[:, 4:8, :], w_d[512:1024, :].rearrange("(c p) f -> p c f", p=128))], writes=[K("ow")])
                i_ = 0
                for dc in range(NCH):
                    for t in range(NT):
                        tl = slice(t * TT, (t + 1) * TT)
                        o_ = i_ % 4
                        i_ += 1
                        for kc in range(NCH):
                            sc.op("pe", lambda en: en.matmul(ps[o_][:], lhsT=ow[:, kc, dc * 128:(dc + 1) * 128], rhs=zT_[:, kc, tl],
                                                             start=(kc == 0), stop=(kc == NCH - 1)),
                                  reads=[K("ow"), K("h", kc, t)], writes=[K("ps", o_)], inc=(kc == NCH - 1))
                        sc.op("dve", lambda en: en.scalar_tensor_tensor(out=xT[:, dc, tl], in0=ps[o_][:], scalar=modT[l][:, 16 + dc, b:b + 1], op0=ALU.mult,
                                                                        in1=xT[:, dc, tl], op1=ALU.add),
                              reads=[K("ps", o_), K("x", dc, t)], writes=[K("x", dc, t)])
                barrier()

        def phase_m1(b):
            l = 1
            phase_norm(b, A1[l], modT[l][:, 0:8, :], False)
            HS = S // 2
            with ExitStack() as esz:
                zT = esz.enter_context(SBT("zT", [128, NCH, S], BF16))
                with ExitStack() as es1:
                    def t_(name, shape, dt=F32):
                        return es1.enter_context(SBT(name, list(shape), dt))
                    wyx = [t_(f"wyx{i}", [128, NCH, 256], BF16) for i in range(2)]
                    rgw = t_("rgw", [128, NCH, 128], BF16)
                    igw = t_("igw", [128, NCH, 128], BF16)
                    xbrs = [t_("xbr0", [128, 3 + S]), t_("xbr1", [128, 3 + S])]
                    Bgy = t_("Bgy", [128, HS])
                    Bxc = t_("Bxc", [128, HS])
                    Br = t_("Br", [128, HS])
                    Bi = t_("Bi", [128, HS])
                    Bm = t_("Bm", [128, HS])
                    xcb = t_("xcb", [128, HS], BF16)
                    carry = t_("carry", [128, 1])
                    sc.dma("pool", "d_gw", [(rgw[:], rgw_d.rearrange("n k j -> k n j")), (igw[:], igw_d.rearrange("n k j -> k n j"))], writes=[K("gw")])
                    sc.op("pool", lambda en: en.memset(xbrs[0][:, 0:3], 0.0), writes=[K("xbrpad")])
                    sc.op("pool", lambda en: en.memset(xbrs[1][:, 0:3], 0.0), writes=[K("xbrpad2")])
                    ocw, _ = PV_OFF["ocw"]

                    def load_wyx(c):
                        s_ = c % 2
                        sc.dma("pool", f"d_wyx{s_}", [(wyx[s_][:, :, 0:128], odin_d[:, c * 128:(c + 1) * 128].rearrange("(k p) f -> p k f", p=128)),
                                                      (wyx[s_][:, :, 128:256], odin_d[:, 1024 + c * 128:1024 + (c + 1) * 128].rearrange("(k p) f -> p k f", p=128))],
                               writes=[K("wyx", s_)])
                    Bgy2 = t_("Bgy2", [128, HS])
                    Bgys = [Bgy, Bgy2]
                    m1units = [(c, hf) for c in range(NCH) for hf in range(2)]

                    def m1_stage1(j):
                        c, hf = m1units[j]
                        ws = c % 2
                        xbr = xbrs[c % 2]
                        Bg = Bgys[j % 2]
                        if hf == 0 and c + 1 < NCH:
                            load_wyx(c + 1)
                        for tt in range(2):
                            t = hf * 2 + tt
                            tl = slice(t * TT, (t + 1) * TT)
                            hl = slice(tt * TT, (tt + 1) * TT)
                            for kc in range(NCH):
                                sc.op("pe", lambda en: en.matmul(ps[tt][:], lhsT=wyx[ws][:, kc, 0:128], rhs=hT[:, kc, tl], start=(kc == 0), stop=(kc == NCH - 1)),
                                      reads=[K("wyx", ws), K("h", kc, t)], writes=[K("ps", tt)], inc=(kc == NCH - 1))
                            for kc in range(NCH):
                                sc.op("pe", lambda en: en.matmul(ps[2 + tt][:], lhsT=wyx[ws][:, kc, 128:256], rhs=hT[:, kc, tl], start=(kc == 0), stop=(kc == NCH - 1)),
                                      reads=[K("wyx", ws), K("h", kc, t)], writes=[K("ps", 2 + tt)], inc=(kc == NCH - 1))
                            sc.op("act", lambda en: en.activation(out=Bg[:, hl], in_=ps[tt][:], func=AF.Gelu_apprx_tanh), reads=[K("ps", tt)], writes=[K("Bgy", j % 2, tt)])
                            sc.op("act", lambda en: en.activation(out=xbr[:, 3 + t * TT:3 + (t + 1) * TT], in_=ps[2 + tt][:], func=AF.Identity),
                                  reads=[K("ps", 2 + tt)], writes=[K("xbr", c % 2, t)])

                    def m1_rest(j):
                        c, hf = m1units[j]
                        xbr = xbrs[c % 2]
                        Bg = Bgys[j % 2]
                        for tt in range(2):
                            t = hf * 2 + tt
                            hl = slice(tt * TT, (tt + 1) * TT)
                            rk = [K("xbr", c % 2, t), K("xbrpad"), K("xbrpad2")] + ([K("xbr", c % 2, t - 1)] if t > 0 else [])
                            sc.op("dve", lambda en: en.tensor_scalar(out=Bxc[:, hl], in0=xbr[:, t * TT + 3:t * TT + 3 + TT], scalar1=pv[:, ocw + c * 4 + 3:ocw + c * 4 + 4],
                                                                     scalar2=pvs("ocb", c), op0=ALU.mult, op1=ALU.add),
                                  reads=rk, writes=[K("Bxc", tt)])
                            for k in range(3):
                                sc.op("dve", lambda en: en.scalar_tensor_tensor(out=Bxc[:, hl], in0=xbr[:, t * TT + k:t * TT + k + TT], scalar=pv[:, ocw + c * 4 + k:ocw + c * 4 + k + 1],
                                                                                op0=ALU.mult, in1=Bxc[:, hl], op1=ALU.add),
                                      reads=rk + [K("Bxc", tt)], writes=[K("Bxc", tt)])
                            sc.op("pool", lambda en: en.tensor_copy(out=xcb[:, hl], in_=Bxc[:, hl]), reads=[K("Bxc", tt)], writes=[K("xcb", tt)])
                            sc.op("pe", lambda en: en.matmul(ps[4 + tt][:], lhsT=rgw[:, c, :], rhs=xcb[:, hl], start=True, stop=True),
                                  reads=[K("gw"), K("xcb", tt)], writes=[K("ps", 4 + tt)])
                            sc.op("pe", lambda en: en.matmul(ps[6 + tt][:], lhsT=igw[:, c, :], rhs=xcb[:, hl], start=True, stop=True),
                                  reads=[K("gw"), K("xcb", tt)], writes=[K("ps", 6 + tt)])
                        for tt in range(2):
                            hl = slice(tt * TT, (tt + 1) * TT)
                            sc.op("act", lambda en: en.activation(out=Br[:, hl], in_=ps[4 + tt][:], func=AF.Sigmoid, bias=pvs("rgb", c)), reads=[K("ps", 4 + tt)], writes=[K("Br", tt)])
                            sc.op("act", lambda en: en.activation(out=Bi[:, hl], in_=ps[6 + tt][:], func=AF.Sigmoid, bias=pvs("igb", c)), reads=[K("ps", 6 + tt)], writes=[K("Bi", tt)])
                        for tt in range(2):
                            hl = slice(tt * TT, (tt + 1) * TT)
                            sc.op("act", lambda en: en.activation(out=Br[:, hl], in_=Br[:, hl], func=AF.Exp, scale=cl8[:, c:c + 1]), reads=[K("Br", tt)], writes=[K("Br", tt)])
                            sc.op("dve", lambda en: en.tensor_tensor(out=Bm[:, hl], in0=Br[:, hl], in1=Br[:, hl], op=ALU.mult), reads=[K("Br", tt)], writes=[K("Bm", tt)])
                            sc.op("pool", lambda en: en.tensor_tensor(out=Bi[:, hl], in0=Bi[:, hl], in1=Bxc[:, hl], op=ALU.mult), reads=[K("Bi", tt), K("Bxc", tt)], writes=[K("Bi", tt)])
                        for tt in range(2):
                            hl = slice(tt * TT, (tt + 1) * TT)
                            sc.op("act", lambda en: en.activation(out=Bm[:, hl], in_=Bm[:, hl], func=AF.Ln, scale=-1.0, bias=1.0), reads=[K("Bm", tt)], writes=[K("Bm", tt)])
                        for tt in range(2):
                            hl = slice(tt * TT, (tt + 1) * TT)
                            sc.op("act", lambda en: en.activation(out=Bm[:, hl], in_=Bm[:, hl], func=AF.Exp, scale=0.5), reads=[K("Bm", tt)], writes=[K("Bm", tt)])
                        for tt in range(2):
                            t = hf * 2 + tt
                            tl = slice(t * TT, (t + 1) * TT)
                            hl = slice(tt * TT, (tt + 1) * TT)
                            sc.op("dve", lambda en: en.tensor_tensor(out=Bi[:, hl], in0=Bi[:, hl], in1=Bm[:, hl], op=ALU.mult), reads=[K("Bi", tt), K("Bm", tt)], writes=[K("Bi", tt)])
                            if t == 0:
                                sc.op("dve", lambda en: en.tensor_tensor_scan(out=Bxc[:, hl], data0=Br[:, hl], data1=Bi[:, hl], initial=0.0, op0=ALU.mult, op1=ALU.add),
                                      reads=[K("Br", tt), K("Bi", tt), K("Bxc", tt)], writes=[K("Bxc", tt)])
                            else:
                                sc.op("dve", lambda en: en.tensor_tensor_scan(out=Bxc[:, hl], data0=Br[:, hl], data1=Bi[:, hl], initial=carry[:, 0:1], op0=ALU.mult, op1=ALU.add),
                                      reads=[K("Br", tt), K("Bi", tt), K("Bxc", tt), K("carry")], writes=[K("Bxc", tt)])
                            sc.op("dve", lambda en: en.tensor_copy(out=carry[:, 0:1], in_=Bxc[:, tt * TT + TT - 1:tt * TT + TT]), reads=[K("Bxc", tt)], writes=[K("carry")])
                            sc.op("pool", lambda en: en.tensor_tensor(out=zT[:, c, tl], in0=Bg[:, hl], in1=Bxc[:, hl], op=ALU.mult),
                                  reads=[K("Bgy", j % 2, tt), K("Bxc", tt)], writes=[K("z", c, t)])

                    load_wyx(0)
                    m1_stage1(0)
                    for j in range(len(m1units)):
                        if j + 1 < len(m1units):
                            m1_stage1(j + 1)
                        m1_rest(j)
                    barrier()
                out_proj(l, b, odout_d, zT)

        def phase_m0(b):
            l = 0
            phase_norm(b, A1[l], modT[l][:, 0:8, :], False)
            UP = 30
            with ExitStack() as es0:
                def t0_(name, shape, dt=F32):
                    return es0.enter_context(SBT(name, list(shape), dt))
                u_pad = t0_("u_pad", [128, 4, UP + S], BF16)
                mask_bf = t0_("mask_bf", [128, 4, 512], BF16)
                om, ou, oo = CST_OFF["mask"][0], CST_OFF["ustrict"][0], CST_OFF["ones"][0]
                nmask_bf = t0_("nmask_bf", [128, 4, 512], BF16)
                ident_bf = t0_("ident_bf", [128, 128], BF16)
                onm, oid = CST_OFF["nmask"][0], CST_OFF["ident"][0]
                sc.dma("pool", "d_c3", [(mask_bf[:], cst_d[:, om:om + 2048].rearrange("p (d t) -> p d t", d=4)),
                                        (nmask_bf[:], cst_d[:, onm:onm + 2048].rearrange("p (d t) -> p d t", d=4)),
                                        (ident_bf[:], cst_d[:, oid:oid + 128])], writes=[K("mask")])
                sc.op("pool", lambda en: en.memset(u_pad[:, :, 0:UP], 0.0), writes=[K("upad")])
                esq = ExitStack()
                qT = esq.enter_context(SBT("qT", [128, 4, S], BF16))
                kT = esq.enter_context(SBT("kT", [128, 4, S], BF16))
                v_sb = esq.enter_context(SBT("v_sb", [128, 16, 512], BF16))
                with ExitStack() as esa:
                    def ta_(name, shape, dt=F32):
                        return esa.enter_context(SBT(name, list(shape), dt))
                    wb = [ta_(f"wb{i}", [128, NCH, 512], BF16) for i in range(2)]
                    sig = [ta_(f"sig{i}", [128, TT]) for i in range(2)]
                    qsq = [ta_(f"qsq{i}", [128, TT], BF16) for i in range(2)]
                    rq = [ta_(f"rq{i}", [128, TT]) for i in range(2)]

                    def load_g(g, slot):
                        sc.dma("pool", f"d_wb{slot}", [(wb[slot][:], evin_d[:, g * 512:(g + 1) * 512].rearrange("(k p) f -> p k f", p=128))], writes=[K("wb", slot)])
                    load_g(0, 0)
                    load_g(1, 1)
                    i_ = 0
                    for fc in range(4):
                        for t in range(NT):
                            tl = slice(t * TT, (t + 1) * TT)
                            p_ = i_ % 2
                            i_ += 1
                            for kc in range(NCH):
                                sc.op("pe", lambda en: en.matmul(ps[p_][:], lhsT=wb[0][:, kc, fc * 128:(fc + 1) * 128], rhs=hT[:, kc, tl], start=(kc == 0), stop=(kc == NCH - 1)),
                                      reads=[K("wb", 0), K("h", kc, t)], writes=[K("ps", p_)], inc=(kc == NCH - 1))
                            for kc in range(NCH):
                                sc.op("pe", lambda en: en.matmul(ps[2 + p_][:], lhsT=wb[1][:, kc, fc * 128:(fc + 1) * 128], rhs=hT[:, kc, tl], start=(kc == 0), stop=(kc == NCH - 1)),
                                      reads=[K("wb", 1), K("h", kc, t)], writes=[K("ps", 2 + p_)], inc=(kc == NCH - 1))
                            sc.op("act", lambda en: en.activation(out=sig[p_][:], in_=ps[2 + p_][:], func=AF.Sigmoid), reads=[K("ps", 2 + p_)], writes=[K("sig", p_)])
                            sc.op("dve", lambda en: en.tensor_tensor(out=u_pad[:, fc, UP + t * TT:UP + (t + 1) * TT], in0=ps[p_][:], in1=sig[p_][:], op=ALU.mult),
                                  reads=[K("ps", p_), K("sig", p_)], writes=[K("u", fc, t)])
                    for gi_, (g, dstT, gname) in enumerate([(2, qT, "qg"), (3, kT, "kg")]):
                        slot = gi_ % 2
                        load_g(g, slot)
                        for fc in range(4):
                            for t in range(NT):
                                tl = slice(t * TT, (t + 1) * TT)
                                p_ = i_ % 2
                                i_ += 1
                                for kc in range(NCH):
                                    sc.op("pe", lambda en: en.matmul(ps[4 + p_][:], lhsT=wb[slot][:, kc, fc * 128:(fc + 1) * 128], rhs=hT[:, kc, tl], start=(kc == 0), stop=(kc == NCH - 1)),
                                          reads=[K("wb", slot), K("h", kc, t)], writes=[K("ps", 4 + p_)], inc=(kc == NCH - 1))
                                sc.op("act", lambda en: en.activation(out=qsq[p_][:], in_=ps[4 + p_][:], func=AF.Square), reads=[K("ps", 4 + p_)], writes=[K("qsq", p_)])
                                sc.op("pe", lambda en: en.matmul(ps[6 + p_][:], lhsT=bdon_bf[:], rhs=qsq[p_][:], start=True, stop=True),
                                      reads=[K("bdon"), K("qsq", p_)], writes=[K("ps", 6 + p_)])
                                sc.op("act", lambda en: en.activation(out=rq[p_][:], in_=ps[6 + p_][:], func=AF.Ln, scale=1.0 / 64.0, bias=EPS), reads=[K("ps", 6 + p_)], writes=[K("rq", p_)])
                                sc.op("act", lambda en: en.activation(out=rq[p_][:], in_=rq[p_][:], func=AF.Exp, scale=-0.5), reads=[K("rq", p_)], writes=[K("rq", p_)])
                                sc.op("dve", lambda en: en.scalar_tensor_tensor(out=dstT[:, fc, tl], in0=ps[4 + p_][:], scalar=pvs(gname), op0=ALU.mult, in1=rq[p_][:], op1=ALU.mult),
                                      reads=[K("ps", 4 + p_), K("rq", p_)], writes=[K(gname, fc, t)])
                    load_g(4, 0)
                    for tc in range(16):
                        p_ = tc % 2
                        for kc in range(NCH):
                            sc.op("pe", lambda en: en.matmul(ps[p_][:], lhsT=hT[:, kc, tc * 128:(tc + 1) * 128], rhs=wb[0][:, kc, :], start=(kc == 0), stop=(kc == NCH - 1)),
                                  reads=[K("wb", 0), K("h", kc, tc // 4)], writes=[K("ps", p_)], inc=(kc == NCH - 1))
                        if tc % 2 == 0:
                            sc.op("act", lambda en: en.activation(out=v_sb[:, tc, :], in_=ps[p_][:], func=AF.Identity), reads=[K("ps", p_)], writes=[K("v", tc)])
                        else:
                            sc.op("dve", lambda en: en.tensor_copy(out=v_sb[:, tc, :], in_=ps[p_][:]), reads=[K("ps", p_)], writes=[K("v", tc)])
                    barrier()
                with ExitStack() as esb:
                    def tb_(name, shape, dt=F32):
                        return esb.enter_context(SBT(name, list(shape), dt))
                    NZ, NQ, NW = 3, 5, 4
                    spf = [tb_(f"spf{i}", [128, TT]) for i in range(NZ)]
                    spb = [tb_(f"spb{i}", [128, TT], BF16) for i in range(NZ)]
                    q1 = [tb_(f"q1_{i}", [128, TT]) for i in range(NQ)]
                    wtb = [tb_(f"wtb{i}", [128, TT], BF16) for i in range(NW)]
                    Rb = [tb_(f"Rb{i}", [128, TT], BF16) for i in range(2)]
                    units = []
                    for hp in range(4):
                        for qt in range(NT):
                            nk = 4 * (qt + 1)
                            for sc_ in range(nk - 1, -1, -1):
                                for hh in range(2):
                                    units.append((hp, qt, sc_, hh, nk))

                    def S1(ui):
                        hp, qt, sc_, hh, nk = units[ui]
                        zb = ui % NZ
                        hr = slice(hh * 64, (hh + 1) * 64)
                        sc.op("pe", lambda en: en.matmul(ps[zb][:], lhsT=kT[hr, hp, sc_ * 128:(sc_ + 1) * 128], rhs=qT[hr, hp, qt * TT:(qt + 1) * TT], start=True, stop=True),
                              reads=[K("kg", hp, sc_ // 4), K("qg", hp, qt)], writes=[K("ps", zb)])

                    def S2(ui):
                        zb = ui % NZ
                        sc.op("act", lambda en: en.activation(out=spf[zb][:], in_=ps[zb][:], func=AF.Exp, scale=0.125), reads=[K("ps", zb)], writes=[K("spf", zb)])
                        sc.op("act", lambda en: en.activation(out=spf[zb][:], in_=spf[zb][:], func=AF.Ln, bias=1.0), reads=[K("spf", zb)], writes=[K("spf", zb)])

                    def S3(ui):
                        hp, qt, sc_, hh, nk = units[ui]
                        zb = ui % NZ
                        qb = ui % NQ
                        d = sc_ - 4 * qt
                        sc.op("dve", lambda en: en.scalar_tensor_tensor(out=q1[qb][:], in0=ps[zb][:], scalar=0.125, op0=ALU.mult, in1=spf[zb][:], op1=ALU.subtract),
                              reads=[K("ps", zb), K("spf", zb)], writes=[K("q1", qb)])
                        if d >= 0:
                            sc.op("dve", lambda en: en.tensor_tensor(out=spb[zb][:], in0=spf[zb][:], in1=mask_bf[:, d, :], op=ALU.mult),
                                  reads=[K("spf", zb), K("mask")], writes=[K("spb", zb)])
                        else:
                            sc.op("dve", lambda en: en.tensor_copy(out=spb[zb][:], in_=spf[zb][:]), reads=[K("spf", zb)], writes=[K("spb", zb)])

                    def S4(ui):
                        hp, qt, sc_, hh, nk = units[ui]
                        zb = ui % NZ
                        cb = 3 + ui % 3
                        d = sc_ - 4 * qt
                        first = (sc_ == nk - 1)
                        nmm = 1 + (0 if first else 1) + (1 if d >= 0 else 0)
                        k_ = 1
                        sc.op("pe", lambda en: en.matmul(ps[cb][:], lhsT=ustr_bf[:], rhs=spb[zb][:], start=True, stop=(k_ == nmm)),
                              reads=[K("ustr"), K("spb", zb)], writes=[K("ps", cb)], inc=(k_ == nmm))
                        if not first:
                            k_ += 1
                            sc.op("pe", lambda en: en.matmul(ps[cb][:], lhsT=ones_bf[:], rhs=Rb[hh][:], start=False, stop=(k_ == nmm)),
                                  reads=[K("ones"), K("Rb", hh)], writes=[K("ps", cb)], inc=(k_ == nmm))
                        if d >= 0:
                            k_ += 1
                            sc.op("pe", lambda en: en.matmul(ps[cb][:], lhsT=ident_bf[:], rhs=nmask_bf[:, d, :], start=False, stop=(k_ == nmm)),
                                  reads=[K("mask")], writes=[K("ps", cb)], inc=(k_ == nmm))
                        if sc_ > 0:
                            if first:
                                sc.op("pool", lambda en: en.tensor_copy(out=Rb[hh][:], in_=spb[zb][:]), reads=[K("spb", zb)], writes=[K("Rb", hh)])
                            else:
                                sc.op("pool", lambda en: en.tensor_tensor(out=Rb[hh][:], in0=Rb[hh][:], in1=spb[zb][:], op=ALU.add),
                                      reads=[K("spb", zb), K("Rb", hh)], writes=[K("Rb", hh)])

                    def S5(ui):
                        qb = ui % NQ
                        cb = 3 + ui % 3
                        sc.op("dve", lambda en: en.tensor_tensor(out=q1[qb][:], in0=q1[qb][:], in1=ps[cb][:], op=ALU.subtract),
                              reads=[K("ps", cb), K("q1", qb)], writes=[K("q1", qb)])

                    def S6(ui):
                        qb = ui % NQ
                        wb_ = ui % NW
                        sc.op("act", lambda en: en.activation(out=wtb[wb_][:], in_=q1[qb][:], func=AF.Exp), reads=[K("q1", qb)], writes=[K("wtb", wb_)])

                    def S8(ui):
                        hp, qt, sc_, hh, nk = units[ui]
                        wb_ = ui % NW
                        pvb = 6 + ((hp * NT + qt) % 2)
                        h = 2 * hp + hh
                        sc.op("pe", lambda en: en.matmul(ps[pvb][hh * 64:(hh + 1) * 64, :], lhsT=v_sb[:, sc_, h * 64:(h + 1) * 64], rhs=wtb[wb_][:],
                                                         start=(sc_ == nk - 1), stop=(sc_ == 0)),
                              reads=[K("v", sc_), K("wtb", wb_)], writes=[K("pv", pvb, hh)], inc=True)
                        if sc_ == 0 and hh == 1:
                            sc.op("act", lambda en: en.activation(out=hT[:, 4 + hp, qt * TT:(qt + 1) * TT], in_=ps[pvb][:], func=AF.Identity),
                                  reads=[K("pv", pvb, 0), K("pv", pvb, 1)], writes=[K("h", 4 + hp, qt)])

                    stages = [S1, S2, S3, S4, S5, S6, S8]
                    nu = len(units)
                    for it in range(nu + len(stages) - 1):
                        for si_, fn in enumerate(stages):
                            ui = it - si_
                            if 0 <= ui < nu:
                                fn(ui)
                    barrier()
                esq.close()
                with ExitStack() as esc:
                    def tc_(name, shape, dt=F32):
                        return esc.enter_context(SBT(name, list(shape), dt))
                    Dg = tc_("cv_Dg", [128, 124, 128], BF16)
                    y = [tc_(f"cv_y{i}", [128, 4, TT]) for i in range(2)]
                    ysq = [tc_(f"ysq{i}", [128, TT], BF16) for i in range(4)]
                    ybf = [tc_(f"ybf{i}", [128, TT], BF16) for i in range(4)]
                    mean = tc_("cv_mean", [128, TT])
                    msq = tc_("cv_msq", [128, TT])
                    rs = tc_("cv_rs", [128, TT])
                    t1 = [tc_(f"cv_t1{i}", [128, TT]) for i in range(2)]
                    odw, _ = PV_OFF["dww"]
                    for idx in range(124):
                        sc.op("dve", lambda en: en.tensor_scalar(out=Dg[:, idx, :], in0=ident[:], scalar1=pv[:, odw + idx:odw + idx + 1], scalar2=None, op0=ALU.mult),
                              reads=[K("ident")], writes=[K("Dg", idx)])
                    gi_ = 0
                    for t in range(NT):
                        tl = slice(t * TT, (t + 1) * TT)
                        yb = y[t % 2]
                        for fc in range(4):
                            bk = 2 + gi_ % 4
                            gi_ += 1
                            rk = [K("upad"), K("u", fc, t)] + ([K("u", fc, t - 1)] if t > 0 else [])
                            for k in range(31):
                                sc.op("pe", lambda en: en.matmul(ps[bk][:], lhsT=Dg[:, fc * 31 + k, :], rhs=u_pad[:, fc, t * TT + k:t * TT + k + TT], start=(k == 0), stop=(k == 30)),
                                      reads=rk + [K("Dg", fc * 31 + k)], writes=[K("ps", bk)], inc=(k == 30))
                            sc.op("act", lambda en: en.activation(out=yb[:, fc, :], in_=ps[bk][:], func=AF.Identity, bias=pvs("dwb", fc)), reads=[K("ps", bk)], writes=[K("y", t % 2, fc)])
                            s_ = fc
                            sc.op("act", lambda en: en.activation(out=ysq[s_][:], in_=yb[:, fc, :], func=AF.Square), reads=[K("y", t % 2, fc)], writes=[K("ysq", s_)])
                            sc.op("pool", lambda en: en.tensor_copy(out=ybf[s_][:], in_=yb[:, fc, :]), reads=[K("y", t % 2, fc)], writes=[K("ybf", s_)])
                        for fc in range(4):
                            s_ = fc
                            sc.op("pe", lambda en: en.matmul(ps[0][:], lhsT=ones_bf[:], rhs=ybf[s_][:], start=(fc == 0), stop=(fc == 3)),
                                  reads=[K("ones"), K("ybf", s_)], writes=[K("ps", 0)], inc=(fc == 3))
                        for fc in range(4):
                            s_ = fc
                            sc.op("pe", lambda en: en.matmul(ps[1][:], lhsT=ones_bf[:], rhs=ysq[s_][:], start=(fc == 0), stop=(fc == 3)),
                                  reads=[K("ones"), K("ysq", s_)], writes=[K("ps", 1)], inc=(fc == 3))
                        sc.op("dve", lambda en: en.tensor_scalar(out=mean[:], in0=ps[0][:], scalar1=1.0 / 512.0, scalar2=None, op0=ALU.mult), reads=[K("ps", 0)], writes=[K("mean")])
                        sc.op("pool", lambda en: en.tensor_tensor(out=msq[:], in0=mean[:], in1=mean[:], op=ALU.mult), reads=[K("mean")], writes=[K("msq")])
                        sc.op("dve", lambda en: en.scalar_tensor_tensor(out=rs[:], in0=ps[1][:], scalar=1.0 / 512.0, op0=ALU.mult, in1=msq[:], op1=ALU.subtract),
                              reads=[K("ps", 1), K("msq")], writes=[K("rs")])
                        sc.op("act", lambda en: en.activation(out=rs[:], in_=rs[:], func=AF.Ln, bias=EPS), reads=[K("rs")], writes=[K("rs")])
                        sc.op("act", lambda en: en.activation(out=rs[:], in_=rs[:], func=AF.Exp, scale=-0.5), reads=[K("rs")], writes=[K("rs")])
                        for fc in range(4):
                            s_ = fc % 2
                            sc.op("dve", lambda en: en.tensor_tensor(out=t1[s_][:], in0=yb[:, fc, :], in1=mean[:], op=ALU.subtract), reads=[K("y", t % 2, fc), K("mean")], writes=[K("t1", s_)])
                            sc.op("dve", lambda en: en.tensor_tensor(out=t1[s_][:], in0=t1[s_][:], in1=rs[:], op=ALU.mult), reads=[K("t1", s_), K("rs")], writes=[K("t1", s_)])
                            sc.op("act", lambda en: en.activation(out=hT[:, fc, tl], in_=t1[s_][:], func=AF.Silu, scale=pvs("lng", fc), bias=pvs("lnb", fc)),
                                  reads=[K("t1", s_)], writes=[K("h", fc, t)])
                    barrier()
            out_proj(l, b, evout_d, hT)

        for si in range(nseq):
            b = si
            for c in range(NCH):
                sc.dma("sp", f"d_x{c}", [(xT[:, c, :], xT_d[si, c * 128:(c + 1) * 128, :])], writes=[K("x", c, t) for t in range(NT)])
            for ph in phases:
                l = int(ph[1])
                if ph[0] == "e":
                    phase_moe(l, b)
                elif ph == "m0":
                    phase_m0(b)
                elif ph == "m1":
                    phase_m1(b)
            for c in range(NCH):
                sc.dma("sp", f"d_o{c}", [(outT_d[si, c * 128:(c + 1) * 128, :], xT[:, c, :])], reads=[K("x", c, t) for t in range(NT)])
        for e_ in ("sp",):
            sc.wait_all(e_)
        print(f"[build] instructions={sc.n_inst} waits={sc.n_wait} counts={ {k: v for k, v in sc.count.items() if k in ('pe','act','dve','pool')} }")
    return nc


def kernel(**inputs):
    inp = {k: np.asarray(v) for k, v in inputs.items()}
    x = inp["x"]
    B = x.shape[0]
    nseq = B // N_CORES
    nc = build_program(nseq=nseq)
    cst = make_consts()
    shared = {
        "cst": cst,
        "mod_w": np.ascontiguousarray(inp["mod_w"], np.float32),
        "ev_in_w": np.ascontiguousarray(inp["ev_in_w"][0], np.float32),
        "ev_out_w": np.ascontiguousarray(inp["ev_out_w"][0], np.float32),
        "od_in_w": np.ascontiguousarray(inp["od_in_w"][0], np.float32),
        "od_out_w": np.ascontiguousarray(inp["od_out_w"][0], np.float32),
        "od_rg_w": np.ascontiguousarray(inp["od_rg_w"][0], np.float32),
        "od_ig_w": np.ascontiguousarray(inp["od_ig_w"][0], np.float32),
        "router_w": np.ascontiguousarray(inp["router_w"], np.float32),
        "ex_w1": np.ascontiguousarray(inp["ex_w1"], np.float32),
        "ex_w3": np.ascontiguousarray(inp["ex_w3"], np.float32),
        "ex_w2": np.ascontiguousarray(inp["ex_w2"], np.float32),
    }
    in_maps = []
    for core in range(N_CORES):
        b0 = core * nseq
        m = dict(shared)
        m["xT"] = np.ascontiguousarray(np.transpose(x[b0:b0 + nseq], (0, 2, 1)))
        m["pvec"] = make_pvec(inp, b0, nseq)
        in_maps.append(m)
    res = run_bass_kernel_spmd(nc, in_maps, core_ids=list(range(N_CORES)))
    out = np.empty_like(x)
    for core in range(N_CORES):
        b0 = core * nseq
        out[b0:b0 + nseq] = np.transpose(res.results[core]["outT"], (0, 2, 1))
    return out
```
